# Optimizing a Trainium2 kernel written in Bass

```python
import jax, jax.numpy as jnp
from jax import lax
import numpy as np

D_MODEL = 1024
BATCH = 16
SEQ = 2048
DEPTH = 1

SSM_EXPAND = 2
SSM_D_INNER = SSM_EXPAND * D_MODEL
SSM_HEAD_DIM = 64
SSM_HEADS = SSM_D_INNER // SSM_HEAD_DIM
SSM_GROUPS = 8
SSM_STATE = 128
SSM_CONV = 4
SSM_CHUNK = 128
SSM_CONV_DIM = SSM_D_INNER + 2 * SSM_GROUPS * SSM_STATE

ATTN_HEADS = 16
ATTN_HEAD_DIM = 64
ATTN_WIDTH = ATTN_HEADS * ATTN_HEAD_DIM
IDX_HEADS = 8
IDX_HEAD_DIM = 64
TOPK_MAX = 256
Q_BLOCK = 128

EPS = 1e-6

IN_SIZES = (SSM_D_INNER, SSM_CONV_DIM, SSM_HEADS,
            ATTN_WIDTH, ATTN_HEAD_DIM, ATTN_HEAD_DIM, ATTN_WIDTH,
            IDX_HEADS * IDX_HEAD_DIM, IDX_HEAD_DIM, IDX_HEADS,
            2 * D_MODEL)
IN_TOTAL = 10984

kernel_name = "hybrid_ssd_dsa_gated_block"


def _split_offsets(sizes):
    offs, acc = [], 0
    for s in sizes[:-1]:
        acc += s
        offs.append(acc)
    return offs


def rmsnorm(x, w):
    xf = x.astype(jnp.float32)
    y = xf * lax.rsqrt(jnp.mean(xf * xf, axis=-1, keepdims=True) + EPS)
    return (y * w.astype(jnp.float32)).astype(x.dtype)


def layernorm(x, w, b):
    xf = x.astype(jnp.float32)
    mu = jnp.mean(xf, axis=-1, keepdims=True)
    var = jnp.mean(jnp.square(xf - mu), axis=-1, keepdims=True)
    y = (xf - mu) * lax.rsqrt(var + EPS)
    return (y * w.astype(jnp.float32) + b.astype(jnp.float32)).astype(x.dtype)


def causal_depthwise_conv(u, w, b):
    out = lax.conv_general_dilated(
        u, w[:, None, :].astype(u.dtype), window_strides=(1,),
        padding=[(SSM_CONV - 1, 0)], dimension_numbers=("NWC", "WIO", "NWC"),
        feature_group_count=u.shape[-1])
    return out + b.astype(u.dtype)


def ssd_chunked(xs, dt, A, Bm, Cm):
    b, L, h, p = xs.shape
    g, n = Bm.shape[2], Bm.shape[3]
    j = h // g
    c = L // SSM_CHUNK
    q = SSM_CHUNK
    xc = (xs * dt[..., None]).reshape(b, c, q, g, j, p)
    ac = (dt * A).reshape(b, c, q, g, j)
    Bc = Bm.reshape(b, c, q, g, n)
    Cc = Cm.reshape(b, c, q, g, n)
    acs = jnp.cumsum(ac, axis=2)
    acs_t = jnp.transpose(acs, (0, 1, 3, 4, 2))
    seg = acs_t[..., :, None] - acs_t[..., None, :]
    tril = jnp.tril(jnp.ones((q, q), dtype=bool))
    Lmat = jnp.exp(jnp.where(tril, seg, -jnp.inf))
    CB = jnp.einsum('bclgn,bcsgn->bcgls', Cc, Bc)
    y_diag = jnp.einsum('bcgjls,bcsgjp->bclgjp', CB[:, :, :, None] * Lmat, xc)
    decay_states = jnp.exp(acs[:, :, -1:] - acs)
    states = jnp.einsum('bclgn,bclgjp->bcgjpn', Bc, xc * decay_states[..., None])
    chunk_decay = jnp.exp(acs[:, :, -1])

    def step(hprev, inp):
        st, dec = inp
        return hprev * dec[..., None, None] + st, hprev

    h0 = jnp.zeros((b, g, j, p, n), dtype=xs.dtype)
    _, prev = lax.scan(step, h0, (jnp.moveaxis(states, 1, 0), jnp.moveaxis(chunk_decay, 1, 0)))
    prev = jnp.moveaxis(prev, 0, 1)
    y_off = jnp.einsum('bclgn,bcgjpn->bclgjp', Cc, prev) * jnp.exp(acs)[..., None]
    return (y_diag + y_off).reshape(b, L, h, p)


def ssm_branch(z, xbc, dt_raw, conv_w, conv_b, dt_bias, a_log, d_skip, norm_w):
    bsz, L, _ = xbc.shape
    xbc = jax.nn.silu(causal_depthwise_conv(xbc, conv_w, conv_b))
    xs, Bm, Cm = jnp.split(xbc, [SSM_D_INNER, SSM_D_INNER + SSM_GROUPS * SSM_STATE], axis=-1)
    xs = xs.reshape(bsz, L, SSM_HEADS, SSM_HEAD_DIM).astype(jnp.float32)
    Bm = Bm.reshape(bsz, L, SSM_GROUPS, SSM_STATE).astype(jnp.float32)
    Cm = Cm.reshape(bsz, L, SSM_GROUPS, SSM_STATE).astype(jnp.float32)
    dt = jax.nn.softplus(dt_raw.astype(jnp.float32) + dt_bias.astype(jnp.float32))
    A = -jnp.exp(a_log.astype(jnp.float32))
    y = ssd_chunked(xs, dt, A, Bm, Cm)
    y = y + d_skip.astype(jnp.float32)[:, None] * xs
    y = y.reshape(bsz, L, SSM_D_INNER) * jax.nn.silu(z.astype(jnp.float32))
    yg = y.reshape(bsz, L, SSM_GROUPS, SSM_D_INNER // SSM_GROUPS)
    yg = yg * lax.rsqrt(jnp.mean(yg * yg, axis=-1, keepdims=True) + EPS)
    y = yg.reshape(bsz, L, SSM_D_INNER) * norm_w.astype(jnp.float32)
    return y.astype(z.dtype)


def dsa_branch(q, k, v, zg, qi, ki, wi, ki_norm_w, ki_norm_b):
    bsz, L, _ = q.shape
    n_sel = min(TOPK_MAX, L // 4)
    nb = L // Q_BLOCK
    scale = ATTN_HEAD_DIM ** -0.5
    idx_scale = (IDX_HEADS ** -0.5) * (IDX_HEAD_DIM ** -0.5)
    slopes = jnp.exp2(-8.0 * jnp.arange(1, ATTN_HEADS + 1, dtype=jnp.float32) / ATTN_HEADS)
    ki = layernorm(ki, ki_norm_w, ki_norm_b).astype(jnp.float32)
    s_pos = jnp.arange(L, dtype=jnp.int32)

    def to_blocks(a, tail):
        return jnp.moveaxis(a.reshape((bsz, nb, Q_BLOCK) + tail), 1, 0)

    qb_all = to_blocks(q, (ATTN_HEADS, ATTN_HEAD_DIM))
    qib_all = to_blocks(qi, (IDX_HEADS, IDX_HEAD_DIM))
    wb_all = to_blocks(wi, (IDX_HEADS,))
    t0_all = jnp.arange(nb, dtype=jnp.int32) * Q_BLOCK

    def gather_rows(a, idx):
        return jax.vmap(lambda aa, ii: aa[ii])(a, idx)

    def block(args):
        qb, qib, wb, t0 = args
        t = t0 + jnp.arange(Q_BLOCK, dtype=jnp.int32)
        isc = jax.nn.relu(jnp.einsum('bqhd,bsd->bqhs', qib.astype(jnp.float32), ki))
        iscore = jnp.einsum('bqhs,bqh->bqs', isc, wb.astype(jnp.float32) * idx_scale)
        causal = s_pos[None, :] <= t[:, None]
        iscore = jnp.where(causal[None], iscore, -jnp.inf)
        _, sel = lax.top_k(iscore, n_sel)
        valid = sel <= t[None, :, None]
        k_sel = gather_rows(k, sel)
        v_sel = gather_rows(v, sel)
        logits = jnp.einsum('bqhd,bqkd->bqhk', qb, k_sel).astype(jnp.float32) * scale
        dist = (t[None, :, None] - sel).astype(jnp.float32)
        logits = logits - slopes[None, None, :, None] * dist[:, :, None, :]
        logits = jnp.where(valid[:, :, None, :], logits, -jnp.inf)
        probs = jax.nn.softmax(logits, axis=-1).astype(v.dtype)
        return jnp.einsum('bqhk,bqkd->bqhd', probs, v_sel)

    o = lax.map(block, (qb_all, qib_all, wb_all, t0_all))
    o = jnp.moveaxis(o, 0, 1).reshape(bsz, L, ATTN_WIDTH)
    return o * jax.nn.silu(zg)


def setup_inputs(seed: int = 0) -> dict:
    key = jax.random.key(seed)
    ks = jax.random.split(key, 20)
    f32 = jnp.float32
    nrm = lambda k, shape, s: jax.random.normal(k, shape, f32) * s
    x = jax.random.normal(ks[0], (BATCH, SEQ, D_MODEL), f32)
    norm_w = 1.0 + nrm(ks[1], (DEPTH, D_MODEL), 0.02)
    w_in = nrm(ks[2], (DEPTH, D_MODEL, IN_TOTAL), D_MODEL ** -0.5)
    conv_w = nrm(ks[3], (DEPTH, SSM_CONV, SSM_CONV_DIM), SSM_CONV ** -0.5)
    conv_b = nrm(ks[4], (DEPTH, SSM_CONV_DIM), 0.01)
    u = jax.random.uniform(ks[5], (DEPTH, SSM_HEADS), f32)
    dt0 = jnp.exp(u * (jnp.log(0.1) - jnp.log(0.001)) + jnp.log(0.001))
    dt_bias = dt0 + jnp.log(-jnp.expm1(-dt0))
    a_log = jnp.log(jax.random.uniform(ks[6], (DEPTH, SSM_HEADS), f32, 1.0, 16.0))
    d_skip = 1.0 + nrm(ks[7], (DEPTH, SSM_HEADS), 0.1)
    ssm_norm_w = 1.0 + nrm(ks[8], (DEPTH, SSM_D_INNER), 0.02)
    idx_k_norm_w = 1.0 + nrm(ks[9], (DEPTH, IDX_HEAD_DIM), 0.02)
    idx_k_norm_b = nrm(ks[10], (DEPTH, IDX_HEAD_DIM), 0.01)
    gate_b = nrm(ks[11], (DEPTH, 2 * D_MODEL), 0.01)
    w_ssm_out = nrm(ks[12], (DEPTH, SSM_D_INNER, D_MODEL), SSM_D_INNER ** -0.5)
    w_attn_out = nrm(ks[13], (DEPTH, ATTN_WIDTH, D_MODEL), ATTN_WIDTH ** -0.5)
    w_out = nrm(ks[14], (DEPTH, D_MODEL, D_MODEL), D_MODEL ** -0.5)
    final_norm_w = 1.0 + nrm(ks[15], (D_MODEL,), 0.02)
    return {"x": x, "norm_w": norm_w, "w_in": w_in, "conv_w": conv_w, "conv_b": conv_b,
            "dt_bias": dt_bias, "a_log": a_log, "d_skip": d_skip, "ssm_norm_w": ssm_norm_w,
            "idx_k_norm_w": idx_k_norm_w, "idx_k_norm_b": idx_k_norm_b, "gate_b": gate_b,
            "w_ssm_out": w_ssm_out, "w_attn_out": w_attn_out, "w_out": w_out,
            "final_norm_w": final_norm_w}


def reference(x, norm_w, w_in, conv_w, conv_b, dt_bias, a_log, d_skip, ssm_norm_w,
              idx_k_norm_w, idx_k_norm_b, gate_b, w_ssm_out, w_attn_out, w_out, final_norm_w):
    bsz, L, _ = x.shape
    offs = _split_offsets(IN_SIZES)
    for l in range(DEPTH):
        h = rmsnorm(x, norm_w[l])
        proj = jnp.einsum('bld,de->ble', h, w_in[l])
        (ssm_z, ssm_xbc, ssm_dt, q, k, v, attn_z, qi, ki, wi, gate) = jnp.split(proj, offs, axis=-1)
        y_ssm = ssm_branch(ssm_z, ssm_xbc, ssm_dt, conv_w[l], conv_b[l], dt_bias[l],
                           a_log[l], d_skip[l], ssm_norm_w[l])
        y_ssm = jnp.einsum('ble,ed->bld', y_ssm, w_ssm_out[l])
        y_attn = dsa_branch(q, k, v, attn_z, qi, ki, wi, idx_k_norm_w[l], idx_k_norm_b[l])
        y_attn = jnp.einsum('ble,ed->bld', y_attn, w_attn_out[l])
        g = jax.nn.sigmoid(gate + gate_b[l])
        g_ssm, g_attn = jnp.split(g, 2, axis=-1)
        merged = g_ssm * y_ssm + g_attn * y_attn
        x = x + jnp.einsum('bld,de->ble', merged, w_out[l])
    return rmsnorm(x, final_norm_w)
```

```python
import numpy as np
import ml_dtypes
from contextlib import ExitStack
import concourse.bass as bass
import concourse.mybir as mybir
from concourse.bass_utils import run_bass_kernel_spmd

F32 = mybir.dt.float32
BF16 = mybir.dt.bfloat16
U32 = mybir.dt.uint32
AF = mybir.ActivationFunctionType
ALU = mybir.AluOpType
AX = mybir.AxisListType

D_MODEL = 1024
SEQ = 2048
IN_TOTAL = 10984
NIT = 16
TOPK = 256
EPS = 1e-6
IDX_SCALE = (8 ** -0.5) * (64 ** -0.5)

NW, SNW, DTB, ALOG, DSK, KIW, KIB, NRES, GB, FNW, NPB = 0, 1024, 3072, 3104, 3136, 3168, 3232, 3296, 3296, 5344, 6368
IDENT, TRI, USTR, NEGM, ONES, NEGT, NPK = 0, 128, 256, 384, 512, 640, 768
C_SMALL, C_GRP, C_Q, C_QI, C_AZ, C_GATE = 0, 232, 6376, 7400, 7912, 8936


class Dep:
    __slots__ = ("name", "w", "r")

    def __init__(self, name=""):
        self.name = name
        self.w = None
        self.r = {}


class Sched:
    ENGS = ("pe", "act", "dve", "pool", "sp")

    def __init__(self, nc, stack, n_dma_sems=24):
        self.nc = nc
        self.lists = {e: [] for e in self.ENGS}
        self.sems = {}
        self.cnt = {}
        for e in ("pe", "act", "dve", "pool"):
            self.sems[e] = stack.enter_context(nc.semaphore("s_" + e))
            self.cnt[e] = 0
        self.dma_pool = {}
        for q, n in (("sp", n_dma_sems), ("pool", 8)):
            keys = []
            for i in range(n):
                k = "d_%s_%d" % (q, i)
                self.sems[k] = stack.enter_context(nc.semaphore(k))
                self.cnt[k] = 0
                keys.append(k)
            self.dma_pool[q] = [keys, 0]
        self.seen = {e: {} for e in self.ENGS}
        self.n_ops = 0

    def _needs(self, eng, reads, writes):
        needs = {}

        def add(k, v):
            if v > needs.get(k, 0):
                needs[k] = v
        for d in reads:
            if d.w is not None and not (eng == "pe" and d.w[0] == "pe"):
                add(*d.w)
        for d in writes:
            if d.w is not None and not (eng == "pe" and d.w[0] == "pe"):
                add(*d.w)
            for k, v in d.r.items():
                if not (eng == "pe" and k == "pe"):
                    add(k, v)
        out = []
        seen = self.seen[eng]
        for k, v in needs.items():
            if seen.get(k, 0) >= v:
                continue
            seen[k] = v
            out.append((k, v))
        return out

    def op(self, eng, fn, reads=(), writes=()):
        reads = [getattr(d, "dep", d) for d in reads]
        writes = [getattr(d, "dep", d) for d in writes]
        waits = self._needs(eng, reads, writes)
        self.cnt[eng] += 1
        v = self.cnt[eng]
        self.lists[eng].append((waits, fn, eng, 1))
        for d in reads:
            d.r[eng] = v
        for d in writes:
            d.w = (eng, v)
            d.r = {}
        self.n_ops += 1

    def dma(self, q, fn, reads=(), writes=()):
        reads = [getattr(d, "dep", d) for d in reads]
        writes = [getattr(d, "dep", d) for d in writes]
        keys, idx = self.dma_pool[q]
        k = keys[idx % len(keys)]
        self.dma_pool[q][1] = idx + 1
        waits = self._needs(q, reads, writes)
        prev = self.cnt[k]
        if prev > 0 and self.seen[q].get(k, 0) < prev:
            self.seen[q][k] = prev
            waits.append((k, prev))
        self.cnt[k] += 16
        v = self.cnt[k]
        self.lists[q].append((waits, fn, k, 16))
        for d in reads:
            d.r[k] = v
        for d in writes:
            d.w = (k, v)
            d.r = {}
        self.n_ops += 1

    def barrier(self, engs=("pe", "act", "dve", "sp")):
        for e in engs:
            waits = []
            for k, v in self.cnt.items():
                if k == e or k.startswith("d_pool") or v == 0:
                    continue
                if self.seen[e].get(k, 0) < v:
                    self.seen[e][k] = v
                    waits.append((k, v))
            if waits:
                self.lists[e].append((waits, None, None, 0))

    def final_wait(self, eng):
        waits = []
        for k, v in self.cnt.items():
            if k == eng or v == 0:
                continue
            if self.seen[eng].get(k, 0) < v:
                self.seen[eng][k] = v
                waits.append((k, v))
        self.lists[eng].append((waits, None, None, 0))

    def emit(self):
        nc = self.nc
        sems = self.sems
        lists = self.lists

        def replay(e, lst):
            for waits, fn, k, inc in lst:
                for (wk, wv) in waits:
                    e.wait_ge(sems[wk], wv)
                if fn is not None:
                    fn(e).then_inc(sems[k], inc)

        with nc.Block() as block:
            @block.tensor
            def _(e):
                replay(e, lists["pe"])

            @block.scalar
            def _(e):
                replay(e, lists["act"])

            @block.vector
            def _(e):
                replay(e, lists["dve"])

            @block.gpsimd
            def _(e):
                replay(e, lists["pool"])

            @block.sync
            def _(e):
                replay(e, lists["sp"])


def build_program(n_seq=2, dbg=None):
    nc = bass.Bass("TRN2", target_bir_lowering=False)

    def dram(name, shape, dt=F32, kind="ExternalInput"):
        return nc.dram_tensor(name, shape, dt, kind=kind).ap()

    x_d = dram("x", [n_seq, SEQ, D_MODEL])
    win_d = dram("win", [D_MODEL, IN_TOTAL])
    wso_d = dram("wso", [2048, 1024])
    wao_d = dram("wao", [1024, 1024])
    wout_d = dram("wout", [1024, 1024])
    pb_d = dram("pb", [128, NPB])
    pc_d = dram("pc", [128, 160])
    pk_d = dram("pk", [128, NPK])
    kaug_d = dram("kaug", [5, SEQ], BF16)
    qaug_d = dram("qaug", [5, 16, SEQ], BF16)
    out_d = dram("out", [n_seq, SEQ, D_MODEL], kind="ExternalOutput")
    dbg_outs = {}

    win_v = win_d.rearrange("(kc p) n -> p kc n", p=128)
    wso_v = wso_d.rearrange("(kc p) n -> p kc n", p=128)
    wao_v = wao_d.rearrange("(kc p) n -> p kc n", p=128)
    wout_v = wout_d.rearrange("(kc p) n -> p kc n", p=128)

    with ExitStack() as st0:
        S = Sched(nc, st0)
        uid = [0]

        class T:
            def __init__(self, name, shape, dt, psum=False, stack=st0):
                uid[0] += 1
                nm = "%s_%d" % (name, uid[0])
                alloc = nc.psum_tensor if psum else nc.sbuf_tensor
                self.t = stack.enter_context(alloc(nm, list(shape), dt))
                self.dep = Dep(nm)
                self.row = int(np.prod(shape[1:]))

            def __getitem__(self, k):
                return self.t[k]

            def ap(self, col0, dims, p0=0, np_=128):
                return bass.AP(self.t, p0 * self.row + col0, [[self.row, np_]] + [list(d) for d in dims])

        class Ring:
            def __init__(self, tiles):
                self.tiles = tiles
                self.i = 0

            def next(self):
                t = self.tiles[self.i % len(self.tiles)]
                self.i += 1
                return t

        def mm(out, lhsT, rhs, start=True, stop=True, r=(), w=()):
            S.op("pe", lambda e: e.matmul(out, lhsT=lhsT, rhs=rhs, start=start, stop=stop), r, w)

        def tr(out, in_, r=(), w=()):
            S.op("pe", lambda e: e.transpose(out=out, in_=in_, identity=identb[:]), list(r) + [identb], w)

        def act(out, in_, func, r=(), w=(), bias=None, scale=None, accum=None):
            kw = {}
            if bias is not None:
                kw["bias"] = bias
            if scale is not None:
                kw["scale"] = scale
            if accum is not None:
                kw["accum_out"] = accum
            S.op("act", lambda e: e.activation(out=out, in_=in_, func=func, **kw), r, w)

        def acopy(out, in_, r=(), w=()):
            S.op("act", lambda e: e.copy(out=out, in_=in_), r, w)

        def tt(out, a, b, op, r=(), w=(), eng="dve"):
            S.op(eng, lambda e: e.tensor_tensor(out=out, in0=a, in1=b, op=op), r, w)

        def ts(out, a, s1, op0, r=(), w=(), s2=None, op1=None, accum=None):
            kw = {}
            if op1 is not None:
                kw["op1"] = op1
            if accum is not None:
                kw["accum_out"] = accum
            S.op("dve", lambda e: e.tensor_scalar(out=out, in0=a, scalar1=s1, scalar2=s2, op0=op0, **kw), r, w)

        def stt(out, a, s, b, op0, op1, r=(), w=()):
            S.op("dve", lambda e: e.scalar_tensor_tensor(out=out, in0=a, scalar=s, in1=b, op0=op0, op1=op1), r, w)

        def vcopy(out, in_, r=(), w=()):
            S.op("dve", lambda e: e.tensor_copy(out=out, in_=in_), r, w)

        def memset(ap, val, w=()):
            S.op("dve", lambda e: e.memset(ap, val), (), w)

        def recip(out, in_, r=(), w=()):
            S.op("dve", lambda e: e.reciprocal(out=out, in_=in_), r, w)

        def dma(q, out, in_, r=(), w=()):
            S.dma(q, lambda e: e.dma_start(out=out, in_=in_), r, w)

        def rstd_col(out_col, ssq_col, scale, eps_col, tile):
            act(ssq_col, ssq_col, AF.Sqrt, r=[tile, cc], w=[tile], bias=eps_col, scale=scale)
            recip(out_col, ssq_col, r=[tile], w=[tile])

        def interleave(*gens):
            alive = list(gens)
            while alive:
                for g_ in list(alive):
                    try:
                        next(g_)
                    except StopIteration:
                        alive.remove(g_)

        def dump(name, tile, ap, shape):
            if dbg is None or name not in dbg:
                return
            d = nc.dram_tensor("dbg_" + name, list(shape), tile.t.dtype, kind="ExternalOutput").ap()
            dbg_outs[name] = "dbg_" + name
            dma("sp", d, ap, r=[tile], w=[])

        pb = T("pb", [128, NRES], F32)
        pc = T("pc", [128, 160], F32)
        pk = T("pk", [128, NPK], F32)
        identb = T("identb", [128, 128], BF16)
        cc = T("cc", [128, 8], F32)
        Abc = T("Abc", [128, 32], F32)
        Wsm = T("Wsm", [128, 8, 232], BF16)
        KA = T("KA", [128, SEQ], BF16)
        KIN = T("KIN", [128, SEQ], BF16)
        VA = T("VA", [128, 16, 66], BF16)
        St = T("St", [128, 8, 256], F32)
        Sb = T("Sb", [128, 8, 256], BF16)
        St_deps = [Dep("St%d" % g) for g in range(8)]
        Sb_deps = [Dep("Sb%d" % g) for g in range(8)]
        halo = T("halo", [128, 32, 3], F32)
        halo_deps = [Dep("halo%d" % g) for g in range(8)]
        xring = Ring([T("xt%d" % i, [128, 1024], F32) for i in range(2)])
        hb = T("hb", [128, 1024], BF16)
        hT = T("hT", [128, 8, 512], BF16)
        sm = T("sm", [128, 16], F32)
        dt4 = T("dt4", [128, 4, 32], F32)
        a4 = T("a4", [128, 4, 32], F32)
        eacs4 = T("eacs4", [128, 4, 32], F32)
        cdb4 = T("cdb4", [128, 4, 32], F32)
        dtd4 = T("dtd4", [128, 4, 32], F32)
        wis = T("wis", [128, 4, 8], F32)
        s32 = [T("s32_%d" % i, [128, 32], F32) for i in range(4)]
        kvb = T("kvb", [128, 128], BF16)
        kif = T("kif", [128, 64], F32)
        wring = Ring([T("wb%d" % i, [128, 6144], BF16) for i in range(2)])
        yssm = T("yssm", [128, 4, 1024], BF16)
        yattn = T("yattn", [128, 4, 1024], BF16)

        accR = Ring([T("acc%d" % i, [128, 512], F32, psum=True) for i in range(2)])
        ptr = T("ptr", [128, 1024], BF16, psum=True)
        bk3 = T("bk3", [128, 512], F32, psum=True)
        bk4 = T("bk4", [128, 512], F32, psum=True)
        bk5 = T("bk5", [128, 512], F32, psum=True)
        bk6 = T("bk6", [128, 512], F32, psum=True)
        bk7 = T("bk7", [128, 512], F32, psum=True)
        cb_dep = acs_dep = bk4.dep
        sts_dep = yoff_dep = bk6.dep

        dma("sp", pb[:], pb_d[:, 0:NRES], w=[pb])
        dma("sp", pc[:], pc_d, w=[pc])
        dma("sp", pk[:], pk_d, w=[pk])
        vcopy(identb[:], pk[:, IDENT:IDENT + 128], r=[pk], w=[identb])
        memset(cc[:, 0:1], EPS, w=[cc])
        memset(cc[:, 1:2], 1.0, w=[cc])
        memset(cc[:, 2:3], -1e29, w=[cc])
        memset(cc[:, 3:4], 4.0 * EPS, w=[cc])
        ts(pc[:], pc[:], 0.5, ALU.mult, r=[pc], w=[pc])
        act(Abc[:], pb[:, ALOG:ALOG + 32], AF.Exp, r=[pb], w=[Abc])
        ts(Abc[:], Abc[:], -1.0, ALU.mult, r=[Abc], w=[Abc])
        memset(KA[:], 0.0, w=[KA])
        dma("sp", KA.ap(0, [[1, SEQ]], p0=64, np_=5), kaug_d, w=[KA])
        memset(VA[:], 2.0, w=[VA])
        dma("pool", Wsm[:], win_v[:, :, C_SMALL:C_SMALL + 232], w=[Wsm])

        def load_w(src_v, c0, n, nkc=8):
            wb = wring.next()
            dma("pool", wb.ap(0, [[n, nkc], [1, n]]), src_v[:, :, c0:c0 + n], w=[wb])
            return wb

        def wslice(wb, n, kc, a, b):
            return wb.ap(kc * n + a, [[1, b - a]])

        for seq in range(n_seq):
            memset(St[:], 0.0, w=St_deps)
            memset(Sb[:], 0.0, w=Sb_deps)
            memset(halo[:], 0.0, w=halo_deps)
            for stl in range(4):
                t0 = stl * 512
                pre_w = [load_w(win_v, C_GRP, 768)]
                for tt_ in range(4):
                    xt = xring.next()
                    dma("sp", xt[:], x_d[seq, t0 + tt_ * 128:t0 + (tt_ + 1) * 128, :], w=[xt])
                    act(hb[:], xt[:], AF.Square, r=[xt], w=[hb, sm], accum=sm[:, 0:1])
                    rstd_col(sm[:, 2:3], sm[:, 0:1], 1.0 / D_MODEL, cc[:, 0:1], sm)
                    stt(hb[:], xt[:], sm[:, 2:3], pb[:, NW:NW + 1024], ALU.mult, ALU.mult, r=[xt, sm, pb], w=[hb])
                    for kc in range(8):
                        tr(ptr.ap(kc * 128, [[1, 128]]), hb[:, kc * 128:(kc + 1) * 128], r=[hb], w=[ptr])
                    acopy(hT.ap(tt_ * 128, [[512, 8], [1, 128]]), ptr.ap(0, [[128, 8], [1, 128]]), r=[ptr], w=[hT])
                if seq == 0 and stl == 0:
                    dump("hT", hT, hT[:], [128, 8, 512])

                for tt_ in range(4):
                    gt = stl * 4 + tt_
                    acc = accR.next()
                    for kc in range(8):
                        mm(acc[:, 0:232], hT.ap(kc * 512 + tt_ * 128, [[1, 128]]), Wsm.ap(kc * 232, [[1, 232]]),
                           start=(kc == 0), stop=(kc == 7), r=[hT, Wsm], w=[acc])
                    x32, e32, acs_sb, dd = s32
                    tt(x32[:], acc[:, 0:32], pb[:, DTB:DTB + 32], ALU.add, r=[acc, pb], w=[x32])
                    act(e32[:], x32[:], AF.Exp, r=[x32], w=[e32])
                    act(dt4.ap(tt_ * 32, [[1, 32]]), e32[:], AF.Ln, r=[e32, cc], w=[dt4], bias=cc[:, 1:2], scale=1.0)
                    tt(a4.ap(tt_ * 32, [[1, 32]]), dt4.ap(tt_ * 32, [[1, 32]]), Abc[:], ALU.mult, r=[dt4, Abc], w=[a4])
                    acopy(kvb[:, 0:64], acc[:, 32:96], r=[acc], w=[kvb])
                    acopy(VA.ap(gt * 66, [[1, 64]]), acc[:, 96:160], r=[acc], w=[VA])
                    S.op("dve", lambda e, acc=acc: e.bn_stats(out=sm[:, 4:10], in_=acc[:, 160:224]), [acc], [sm])
                    S.op("dve", lambda e: e.bn_aggr(out=sm[:, 10:12], in_=sm[:, 4:10]), [sm], [sm])
                    rstd_col(sm[:, 13:14], sm[:, 11:12], 1.0, cc[:, 0:1], sm)
                    ts(kif[:], acc[:, 160:224], sm[:, 10:11], ALU.subtract, r=[acc, sm], w=[kif], s2=sm[:, 13:14], op1=ALU.mult)
                    tt(kif[:], kif[:], pb[:, KIW:KIW + 64], ALU.mult, r=[kif, pb], w=[kif])
                    tt(kvb[:, 64:128], kif[:], pb[:, KIB:KIB + 64], ALU.add, r=[kif, pb], w=[kvb])
                    ts(wis.ap(tt_ * 8, [[1, 8]]), acc[:, 224:232], IDX_SCALE, ALU.mult, r=[acc], w=[wis])
                    tr(ptr.ap(0, [[1, 128]], np_=64), kvb[:, 0:64], r=[kvb], w=[ptr])
                    tr(ptr.ap(128, [[1, 128]], np_=64), kvb[:, 64:128], r=[kvb], w=[ptr])
                    acopy(KA.ap(gt * 128, [[1, 128]], np_=64), ptr.ap(0, [[1, 128]], np_=64), r=[ptr], w=[KA])
                    acopy(KIN.ap(gt * 128, [[1, 128]], np_=64), ptr.ap(128, [[1, 128]], np_=64), r=[ptr], w=[KIN])
                    mm(bk4[:, 128:160], pk[:, TRI:TRI + 128], a4.ap(tt_ * 32, [[1, 32]]), r=[pk, a4], w=[acs_dep])
                    mm(bk4[:, 160:192], pk[:, ONES:ONES + 128], a4.ap(tt_ * 32, [[1, 32]]), r=[pk, a4], w=[acs_dep])
                    acopy(acs_sb[:], bk4[:, 128:160], r=[acs_dep], w=[acs_sb])
                    act(eacs4.ap(tt_ * 32, [[1, 32]]), bk4[:, 128:160], AF.Exp, r=[acs_dep], w=[eacs4])
                    act(cdb4.ap(tt_ * 32, [[1, 32]]), bk4[:, 160:192], AF.Exp, r=[acs_dep], w=[cdb4])
                    tt(dd[:], bk4[:, 160:192], acs_sb[:], ALU.subtract, r=[acs_dep, acs_sb], w=[dd])
                    act(dd[:], dd[:], AF.Exp, r=[dd], w=[dd])
                    tt(dtd4.ap(tt_ * 32, [[1, 32]]), dt4.ap(tt_ * 32, [[1, 32]]), dd[:], ALU.mult, r=[dt4, dd], w=[dtd4])
                if seq == 0 and stl == 0:
                    dump("dt4", dt4, dt4[:], [128, 4, 32])
                    dump("KA", KA, KA[:], [128, SEQ])
                    dump("KIN", KIN, KIN[:], [128, SEQ])
                    dump("VA", VA, VA[:], [128, 16, 66])
                    dump("wis", wis, wis[:], [128, 4, 8])
                    dump("eacs4", eacs4, eacs4[:], [128, 4, 32])
                    dump("dtd4", dtd4, dtd4[:], [128, 4, 32])

                S.barrier(("pe", "act", "dve", "sp", "pool"))
                with ExitStack() as ph:
                    Upre = T("Upre", [128, 4, 515], F32, stack=ph)
                    Upre_d = [Dep("Upre%d" % i) for i in range(4)]
                    cv = T("cv", [128, 4, 512], F32, stack=ph)
                    cv_d = [Dep("cv%d" % i) for i in range(4)]
                    thR = Ring([T("th%d" % i, [128, 512], F32, stack=ph) for i in range(2)])
                    xbP = [T("xb%d" % i, [128, 4, 512], BF16, stack=ph) for i in range(2)]
                    xb_d = [[Dep("xb%d_%d" % (i, f)) for f in range(4)] for i in range(2)]
                    zsP = [T("zs%d" % i, [128, 4, 256], F32, stack=ph) for i in range(2)]
                    xtkP = [T("xtk%d" % i, [128, 4, 384], BF16, stack=ph) for i in range(2)]
                    rhsAR = Ring([T("rhsA%d" % i, [128, 512], F32, stack=ph) for i in range(2)])
                    CBmR = Ring([T("CBm%d" % i, [128, 128], F32, stack=ph) for i in range(2)])
                    EsegR = Ring([T("Eseg%d" % i, [128, 512], F32, stack=ph) for i in range(2)])
                    MTR = Ring([T("MT%d" % i, [128, 512], BF16, stack=ph) for i in range(2)])
                    xcR = Ring([T("xc%d" % i, [128, 256], BF16, stack=ph) for i in range(2)])
                    xcdR = Ring([T("xcd%d" % i, [128, 256], BF16, stack=ph) for i in range(2)])
                    xsDR = Ring([T("xsD%d" % i, [128, 256], F32, stack=ph) for i in range(2)])
                    t1R = Ring([T("t1%d" % i, [128, 256], F32, stack=ph) for i in range(2)])
                    yzR = Ring([T("yz%d" % i, [128, 256], F32, stack=ph) for i in range(4)])
                    smgP = [T("smg%d" % i, [128, 12], F32, stack=ph) for i in range(2)]
                    yzs = {}
                    yjk = T("yjk", [128, 256], BF16, stack=ph)
                    yNR = Ring([T("yN%d" % i, [128, 256], BF16, stack=ph) for i in range(2)])
                    yNT = T("yNT", [128, 16, 512], BF16, stack=ph)
                    A0 = accR.tiles[0]
                    segR = Ring([bk3, bk7])
                    cbR = Ring([(0, bk4.dep)])
                    ydR = Ring([(0, bk5.dep)])
                    soR = Ring([(bk6, bk6.dep, bk6.dep), (accR.tiles[1], accR.tiles[1].dep, accR.tiles[1].dep)])
                    ptrA_d, ptrB_d = ptr.dep, [ptr.dep, ptr.dep]
                    state_done = {}

                    def stageA(g):
                        par = g % 2
                        xb, zs, xtk, xbd = xbP[par], zsP[par], xtkP[par], xb_d[par]
                        wb = pre_w.pop() if g == 0 else load_w(win_v, C_GRP + g * 768, 768)
                        vcopy(Upre.ap(0, [[515, 4], [1, 3]]), halo.ap(g * 12, [[3, 4], [1, 3]]), r=[halo_deps[g]], w=Upre_d)
                        for fi in range(4):
                            for kc in range(8):
                                mm(A0[:, :], wslice(wb, 768, kc, 256 + fi * 128, 256 + (fi + 1) * 128), hT.ap(kc * 512, [[1, 512]]),
                                   start=(kc == 0), stop=(kc == 7), r=[wb, hT], w=[A0])
                            acopy(Upre.ap(fi * 515 + 3, [[1, 512]]), A0[:, :], r=[A0], w=[Upre_d[fi]])
                            yield
                        vcopy(halo.ap(g * 12, [[3, 4], [1, 3]]), Upre.ap(512, [[515, 4], [1, 3]]), r=Upre_d, w=[halo_deps[g]])
                        for fi in range(4):
                            ct = g * 4 + fi
                            cvf = cv.ap(fi * 512, [[1, 512]])
                            act(cvf, Upre.ap(fi * 515, [[1, 512]]), AF.Identity, r=[Upre_d[fi], pc], w=[cv_d[fi]],
                                bias=pc[:, 128 + ct:129 + ct], scale=pc[:, ct * 4:ct * 4 + 1])
                            yield
                            for k in range(1, 4):
                                stt(cvf, Upre.ap(fi * 515 + k, [[1, 512]]), pc[:, ct * 4 + k:ct * 4 + k + 1], cvf, ALU.mult, ALU.add,
                                    r=[Upre_d[fi], pc, cv_d[fi]], w=[cv_d[fi]])
                                yield
                            th = thR.next()
                            act(th[:], cvf, AF.Tanh, r=[cv_d[fi]], w=[th])
                            stt(xb.ap(fi * 512, [[1, 512]]), th[:], 1.0, cvf, ALU.add, ALU.mult, r=[th, cv_d[fi]], w=[xbd[fi]])
                            yield
                        for c in range(4):
                            for kc in range(8):
                                mm(A0[:, 0:256], hT.ap(kc * 512 + c * 128, [[1, 128]]), wslice(wb, 768, kc, 0, 256),
                                   start=(kc == 0), stop=(kc == 7), r=[hT, wb], w=[A0])
                            th = thR.next()
                            act(th[:, 0:256], A0[:, 0:256], AF.Tanh, r=[A0], w=[th], scale=0.5)
                            stt(zs.ap(c * 256, [[1, 256]]), th[:, 0:256], 1.0, A0[:, 0:256], ALU.add, ALU.mult, r=[th, A0], w=[zs])
                            yield
                        for c in range(4):
                            for fi in range(3):
                                tr(ptr.ap(fi * 128, [[1, 128]]), xb.ap(fi * 512 + c * 128, [[1, 128]]), r=[xbd[fi]], w=[ptrA_d])
                            acopy(xtk.ap(c * 384, [[1, 384]]), ptr.ap(0, [[1, 384]]), r=[ptrA_d], w=[xtk])
                            yield

                    def chunkB(g, c):
                        par = g % 2
                        xb, zs, xtk, xbd = xbP[par], zsP[par], xtkP[par], xb_d[par]
                        hsl = c * 32 + g * 4
                        rhsA, CBm, Eseg, MT = rhsAR.next(), CBmR.next(), EsegR.next(), MTR.next()
                        xc, xcd, xsD, t1, yz = xcR.next(), xcdR.next(), xsDR.next(), t1R.next(), yzR.next()
                        smg = smgP[g % 2]
                        seg = segR.next()
                        cbo, cbd = cbR.next()
                        ydo, ydd = ydR.next()
                        sob, stsd, yofd = soR.next()
                        tt(rhsA.ap(0, [[128, 4], [1, 128]]), pk.ap(TRI, [[0, 4], [1, 128]]), a4.ap(hsl, [[1, 4], [0, 128]]),
                           ALU.mult, r=[pk, a4], w=[rhsA], eng="pool")
                        xs3 = xtk.ap(c * 384, [[64, 4], [1, 64]])
                        tt(xc.ap(0, [[64, 4], [1, 64]]), xs3, dt4.ap(hsl, [[1, 4], [0, 64]]), ALU.mult, r=[xtk, dt4], w=[xc], eng="pool")
                        tt(xcd.ap(0, [[64, 4], [1, 64]]), xs3, dtd4.ap(hsl, [[1, 4], [0, 64]]), ALU.mult, r=[xtk, dtd4], w=[xcd], eng="pool")
                        tt(xsD.ap(0, [[64, 4], [1, 64]]), xs3, pb.ap(DSK + g * 4, [[1, 4], [0, 64]]), ALU.mult, r=[xtk, pb], w=[xsD], eng="pool")
                        yield
                        mm(seg[:, :], pk[:, USTR:USTR + 128], rhsA[:, :], r=[pk, rhsA], w=[seg])
                        mm(bk4[:, cbo:cbo + 128], xb.ap(2 * 512 + c * 128, [[1, 128]]), xb.ap(3 * 512 + c * 128, [[1, 128]]),
                           r=[xbd[2], xbd[3]], w=[cbd])
                        tt(CBm[:], bk4[:, cbo:cbo + 128], pk[:, TRI:TRI + 128], ALU.mult, r=[cbd, pk], w=[CBm])
                        act(Eseg[:], seg[:, :], AF.Exp, r=[seg], w=[Eseg])
                        yield
                        tt(MT.ap(0, [[128, 4], [1, 128]]), Eseg.ap(0, [[128, 4], [1, 128]]), CBm.ap(0, [[0, 4], [1, 128]]),
                           ALU.mult, r=[Eseg, CBm], w=[MT])
                        yield
                        while c > 0 and not state_done.get((g, c - 1)):
                            yield
                        mm(sob[:, 256:512], xb.ap(3 * 512 + c * 128, [[1, 128]]), Sb.ap(g * 256, [[1, 256]]),
                           r=[xbd[3], Sb_deps[g]], w=[yofd])
                        for j in range(4):
                            mm(bk5[:, ydo + j * 64:ydo + (j + 1) * 64], MT[:, j * 128:(j + 1) * 128], xc[:, j * 64:(j + 1) * 64],
                               start=True, stop=True, r=[MT, xc], w=[ydd])
                        mm(sob[:, 0:256], xtk.ap(c * 384 + 256, [[1, 128]]), xcd[:], r=[xtk, xcd], w=[stsd])
                        tt(St.ap(g * 256, [[64, 4], [1, 64]]), St.ap(g * 256, [[64, 4], [1, 64]]),
                           cdb4.ap(hsl, [[1, 4], [0, 64]]), ALU.mult, r=[St_deps[g], cdb4], w=[St_deps[g]])
                        tt(St.ap(g * 256, [[1, 256]]), St.ap(g * 256, [[1, 256]]), sob[:, 0:256], ALU.add,
                           r=[St_deps[g], stsd], w=[St_deps[g]])
                        acopy(Sb.ap(g * 256, [[1, 256]]), St.ap(g * 256, [[1, 256]]), r=[St_deps[g]], w=[Sb_deps[g]])
                        state_done[(g, c)] = True
                        tt(t1.ap(0, [[64, 4], [1, 64]]), sob.ap(256, [[64, 4], [1, 64]]), eacs4.ap(hsl, [[1, 4], [0, 64]]),
                           ALU.mult, r=[yofd, eacs4], w=[t1])
                        tt(t1[:], bk5[:, ydo:ydo + 256], t1[:], ALU.add, r=[ydd, t1], w=[t1])
                        tt(t1[:], xsD[:], t1[:], ALU.add, r=[xsD, t1], w=[t1])
                        yield
                        tt(yz[:], t1[:], zs.ap(c * 256, [[1, 256]]), ALU.mult, r=[t1, zs], w=[yz])
                        act(yjk[:], yz[:], AF.Square, r=[yz], w=[yjk, smg], accum=smg[:, c:c + 1])
                        yzs[(g, c)] = yz
                        yield

                    def normB(g):
                        smg = smgP[g % 2]
                        act(smg[:, 4:8], smg[:, 0:4], AF.Sqrt, r=[smg, cc], w=[smg], bias=cc[:, 3:4], scale=1.0 / 256)
                        recip(smg[:, 8:12], smg[:, 4:8], r=[smg], w=[smg])
                        for c in range(4):
                            yz = yzs.pop((g, c))
                            yN = yNR.next()
                            stt(yN[:], yz[:], smg[:, 8 + c:9 + c], pb[:, SNW + g * 256:SNW + (g + 1) * 256], ALU.mult, ALU.mult,
                                r=[yz, smg, pb], w=[yN])
                            for i in range(2):
                                tr(ptr.ap(512 + (c % 2) * 256 + i * 128, [[1, 128]]), yN[:, i * 128:(i + 1) * 128], r=[yN], w=[ptrB_d[c % 2]])
                            acopy(yNT.ap((g * 2) * 512 + c * 128, [[512, 2], [1, 128]]), ptr.ap(512 + (c % 2) * 256, [[128, 2], [1, 128]]),
                                  r=[ptrB_d[c % 2]], w=[yNT])

                    def run_group(g):
                        pending = [chunkB(g, c) for c in range(4)]
                        active = [pending.pop(0), pending.pop(0)]
                        ag = stageA(g + 1) if g < 7 else None
                        while active or ag is not None:
                            for g_ in list(active):
                                try:
                                    next(g_)
                                except StopIteration:
                                    active.remove(g_)
                                    if pending:
                                        active.append(pending.pop(0))
                            for _rep in range(2):
                                if ag is not None:
                                    try:
                                        next(ag)
                                    except StopIteration:
                                        ag = None

                    for _ in stageA(0):
                        pass
                    for g in range(8):
                        run_group(g)
                        normB(g)
                    if seq == 0 and stl == 0:
                        dump("yNT", yNT, yNT[:], [128, 16, 512])
                    obanks = [accR.tiles[0], accR.tiles[1], bk3, bk7]
                    for half in range(2):
                        for kh in range(2):
                            wb = wring.next()
                            dma("pool", wb.ap(0, [[512, 8], [1, 512]]), wso_v[:, kh * 8:(kh + 1) * 8, half * 512:(half + 1) * 512], w=[wb])
                            for tt_ in range(4):
                                for kc in range(8):
                                    mm(obanks[tt_][:, :], yNT.ap((kh * 8 + kc) * 512 + tt_ * 128, [[1, 128]]), wslice(wb, 512, kc, 0, 512),
                                       start=(kh == 0 and kc == 0), stop=(kh == 1 and kc == 7), r=[yNT, wb], w=[obanks[tt_]])
                        for tt_ in range(4):
                            acopy(yssm.ap(tt_ * 1024 + half * 512, [[1, 512]]), obanks[tt_][:, :], r=[obanks[tt_]], w=[yssm])
                if seq == 0 and stl == 0:
                    dump("yssm", yssm, yssm[:], [128, 4, 1024])

                S.barrier()
                with ExitStack() as ph:
                    QA = T("QA", [128, 16, 512], BF16, stack=ph)
                    QI = T("QI", [128, 8, 512], BF16, stack=ph)
                    zA = T("zA", [128, 4, 1024], BF16, stack=ph)
                    isc = T("isc", [128, SEQ], F32, stack=ph)
                    rlR = Ring([T("rl%d" % i, [128, 512], F32, stack=ph) for i in range(2)])
                    maskb = T("maskb", [128, SEQ], BF16, stack=ph)
                    maskTP = [T("maskT%d" % i, [128, 16, 128], BF16, stack=ph) for i in range(2)]
                    ER = Ring([T("E%d" % i, [128, 512], BF16, stack=ph) for i in range(3)])
                    PR = Ring([T("P%d" % i, [128, 512], BF16, stack=ph) for i in range(3)])
                    og = T("og", [128, 1024], BF16, stack=ph)
                    Lm = T("Lm", [128, 512], F32, stack=ph)
                    OTs = T("OTs", [128, 512], F32, stack=ph)
                    thA = T("thA", [128, 512], F32, stack=ph)
                    oT = T("oT", [128, 8, 512], BF16, stack=ph)
                    bs = T("bs", [128, 4], F32, stack=ph)
                    rc = T("rc", [128, 4], F32, stack=ph)
                    bu = T("bu", [128, 2], U32, stack=ph)
                    LR = Ring([bk3, accR.tiles[1]])
                    A0 = accR.tiles[0]
                    A1 = accR.tiles[1]
                    Ob = [bk4, bk5, bk6, bk7]
                    Odeps = [Ob[j].dep for j in range(4)]
                    ptrA_d = ptrB_d = ptr.dep

                    dma("sp", QA.ap(0, [[512, 16], [1, 512]], p0=64, np_=5), qaug_d[:, :, t0:t0 + 512], w=[QA])
                    wb = load_w(win_v, C_QI, 512)
                    for h in range(8):
                        for kc in range(8):
                            mm(A0[0:64, :], wslice(wb, 512, kc, h * 64, (h + 1) * 64), hT.ap(kc * 512, [[1, 512]]),
                               start=(kc == 0), stop=(kc == 7), r=[wb, hT], w=[A0])
                        acopy(QI.ap(h * 512, [[1, 512]], np_=64), A0[0:64, :], r=[A0], w=[QI])

                    def stageI(tt_):
                        qb = stl * 4 + tt_
                        SL = (qb + 1) * 128
                        maskT = maskTP[tt_ % 2]
                        for c4 in range((SL + 511) // 512):
                            w_ = min(512, SL - c4 * 512)
                            for h in range(8):
                                mm(A0[:, 0:w_], QI.ap(h * 512 + tt_ * 128, [[1, 128]], np_=64), KIN.ap(c4 * 512, [[1, w_]], np_=64),
                                   r=[QI, KIN], w=[A0])
                                rl = rlR.next()
                                act(rl[:, 0:w_], A0[:, 0:w_], AF.Relu, r=[A0], w=[rl])
                                wcol = wis.ap(tt_ * 8 + h, [[1, 1]])
                                if h == 0:
                                    ts(isc[:, c4 * 512:c4 * 512 + w_], rl[:, 0:w_], wcol, ALU.mult, r=[rl, wis], w=[isc])
                                else:
                                    stt(isc[:, c4 * 512:c4 * 512 + w_], rl[:, 0:w_], wcol, isc[:, c4 * 512:c4 * 512 + w_],
                                        ALU.mult, ALU.add, r=[rl, wis, isc], w=[isc])
                                yield
                        if qb >= 2:
                            S.op("dve", lambda e, SL=SL, bs=bs, isc=isc: e.tensor_reduce(out=bs[:, 1:2], in_=isc[:, 0:SL], axis=AX.X, op=ALU.max), [isc], [bs])
                            S.op("dve", lambda e, SL=SL, bs=bs, isc=isc: e.tensor_reduce(out=bs[:, 0:1], in_=isc[:, 0:SL], axis=AX.X, op=ALU.min), [isc], [bs])
                            ts(bs[:, 1:2], bs[:, 1:2], 1.0, ALU.add, r=[bs], w=[bs])
                        tt(isc[:, SL - 128:SL], isc[:, SL - 128:SL], pk[:, NEGM:NEGM + 128], ALU.add, r=[isc, pk], w=[isc])
                        yield
                        if qb >= 2:
                            for it in range(NIT):
                                ts(bs[:, 2:3], bs[:, 0:1], bs[:, 1:2], ALU.add, r=[bs], w=[bs], s2=0.5, op1=ALU.mult)
                                ts(maskb[:, 0:SL], isc[:, 0:SL], bs[:, 2:3], ALU.is_ge, r=[isc, bs], w=[maskb, bs],
                                   s2=None, op1=ALU.add, accum=bs[:, 3:4])
                                yield
                                ts(bu[:, 0:1], bs[:, 3:4], TOPK - 0.5, ALU.is_ge, r=[bs], w=[bu])
                                ts(bu[:, 1:2], bs[:, 3:4], TOPK - 0.5, ALU.is_lt, r=[bs], w=[bu])
                                S.op("dve", lambda e, bs=bs, bu=bu: e.copy_predicated(out=bs[:, 0:1], mask=bu[:, 0:1], data=bs[:, 2:3]), [bs, bu], [bs])
                                S.op("dve", lambda e, bs=bs, bu=bu: e.copy_predicated(out=bs[:, 1:2], mask=bu[:, 1:2], data=bs[:, 2:3]), [bs, bu], [bs])
                                yield
                            thr = bs[:, 0:1]
                        else:
                            thr = cc[:, 2:3]
                        ts(maskb[:, 0:SL], isc[:, 0:SL], thr, ALU.is_ge, r=[isc, bs, cc], w=[maskb])
                        if seq == 0 and stl == 0 and tt_ == 3:
                            dump("isc", isc, isc[:, 0:512], [128, 512])
                            dump("bs", bs, bs[:], [128, 4])
                        for k0 in range(0, qb + 1, 4):
                            nk = min(4, qb + 1 - k0)
                            for kb in range(k0, k0 + nk):
                                tr(ptr.ap((kb - k0) * 128, [[1, 128]]), maskb[:, kb * 128:(kb + 1) * 128], r=[maskb], w=[ptrA_d])
                            acopy(maskT.ap(k0 * 128, [[1, nk * 128]]), ptr.ap(0, [[1, nk * 128]]), r=[ptrA_d], w=[maskT])
                            yield

                    def stageAT(tt_):
                        qb = stl * 4 + tt_
                        maskT = maskTP[tt_ % 2]
                        steps = [(hg, kb) for hg in range(4) for kb in range(qb + 1)]
                        Ps = {}

                        def front(i):
                            hg, kb = steps[i]
                            L = LR.next()
                            mm(L[:, :], KA.ap(kb * 128, [[1, 128]], np_=69),
                               QA.ap(hg * 4 * 512 + tt_ * 128, [[512, 4], [1, 128]], np_=69), r=[KA, QA], w=[L])
                            E = ER.next()
                            if kb == qb:
                                tt(Lm.ap(0, [[128, 4], [1, 128]]), L.ap(0, [[128, 4], [1, 128]]),
                                   pk.ap(NEGT, [[0, 4], [1, 128]]), ALU.add, r=[L, pk], w=[Lm])
                                act(E[:], Lm[:], AF.Exp, r=[Lm], w=[E])
                            else:
                                act(E[:], L[:, :], AF.Exp, r=[L], w=[E])
                            P = PR.next()
                            tt(P.ap(0, [[128, 4], [1, 128]]), E.ap(0, [[128, 4], [1, 128]]),
                               maskT.ap(kb * 128, [[0, 4], [1, 128]]), ALU.mult, r=[E, maskT], w=[P],
                               eng=("pool" if i % 2 == 0 else "dve"))
                            Ps[i] = P

                        def back(i):
                            hg, kb = steps[i]
                            P = Ps.pop(i)
                            mm(Ob[hg][0:66, :], VA.ap(kb * 66, [[1, 66]]), P[:, :], start=(kb == 0), stop=(kb == qb),
                               r=[VA, P], w=[Odeps[hg]])
                            if kb == qb:
                                acopy(OTs[0:66, :], Ob[hg][0:66, :], r=[Odeps[hg]], w=[OTs])
                                for j in range(4):
                                    S.op("pe", lambda e, j=j, hg=hg, OTs=OTs: e.transpose(out=Ob[hg][:, j * 66:(j + 1) * 66],
                                                                                         in_=OTs[0:66, j * 128:(j + 1) * 128],
                                                                                         identity=pk[0:66, IDENT:IDENT + 66]),
                                         [OTs, pk], [Odeps[hg]])
                                for j in range(4):
                                    h = hg * 4 + j
                                    recip(rc[:, j:j + 1], Ob[hg][:, j * 66 + 64:j * 66 + 65], r=[Odeps[hg]], w=[rc])
                                    stt(og[:, h * 64:(h + 1) * 64], Ob[hg][:, j * 66:j * 66 + 64], rc[:, j:j + 1],
                                        zA.ap(tt_ * 1024 + h * 64, [[1, 64]]), ALU.mult, ALU.mult, r=[Odeps[hg], rc, zA], w=[og])

                        front(0)
                        for i in range(len(steps)):
                            if i + 1 < len(steps):
                                front(i + 1)
                            back(i)
                            yield
                        for kc in range(8):
                            tr(ptr.ap(512 + (kc % 4) * 128, [[1, 128]]), og[:, kc * 128:(kc + 1) * 128], r=[og], w=[ptrB_d])
                            if kc % 4 == 3:
                                acopy(oT.ap((kc - 3) * 512 + tt_ * 128, [[512, 4], [1, 128]]), ptr.ap(512, [[128, 4], [1, 128]]),
                                      r=[ptrB_d], w=[oT])
                        yield

                    def stageProj():
                        for half in range(2):
                            wb = load_w(win_v, C_Q + half * 512, 512)
                            for hh in range(8):
                                h = half * 8 + hh
                                for kc in range(8):
                                    mm(A1[0:64, :], wslice(wb, 512, kc, hh * 64, (hh + 1) * 64), hT.ap(kc * 512, [[1, 512]]),
                                       start=(kc == 0), stop=(kc == 7), r=[wb, hT], w=[A1])
                                S.op("act", lambda e, h=h, QA=QA, A1=A1: e.mul(QA.ap(h * 512, [[1, 512]], np_=64), A1[0:64, :], 0.125), [A1], [QA])
                                yield
                        for half in range(2):
                            wb = load_w(win_v, C_AZ + half * 512, 512)
                            for tt_ in range(4):
                                for kc in range(8):
                                    mm(A1[:, :], hT.ap(kc * 512 + tt_ * 128, [[1, 128]]), wslice(wb, 512, kc, 0, 512),
                                       start=(kc == 0), stop=(kc == 7), r=[hT, wb], w=[A1])
                                act(thA[:], A1[:, :], AF.Tanh, r=[A1], w=[thA], scale=0.5)
                                stt(zA.ap(tt_ * 1024 + half * 512, [[1, 512]]), thA[:], 1.0, A1[:, :], ALU.add, ALU.mult,
                                    r=[thA, A1], w=[zA])
                                yield

                    interleave(stageI(0), stageProj())
                    if seq == 0 and stl == 0:
                        dump("QA", QA, QA[:], [128, 16, 512])
                        dump("QI", QI, QI[:], [128, 8, 512])
                    for tt_ in range(4):
                        interleave(stageAT(tt_), stageI(tt_ + 1) if tt_ < 3 else iter(()))
                    if seq == 0 and stl == 0:
                        dump("oT", oT, oT[:], [128, 8, 512])
                    for half in range(2):
                        wb = load_w(wao_v, half * 512, 512)
                        for tt_ in range(4):
                            acc = accR.next()
                            for kc in range(8):
                                mm(acc[:, :], oT.ap(kc * 512 + tt_ * 128, [[1, 128]]), wslice(wb, 512, kc, 0, 512),
                                   start=(kc == 0), stop=(kc == 7), r=[oT, wb], w=[acc])
                            acopy(yattn.ap(tt_ * 1024 + half * 512, [[1, 512]]), acc[:, :], r=[acc], w=[yattn])
                if seq == 0 and stl == 0:
                    dump("yattn", yattn, yattn[:], [128, 4, 1024])

                S.barrier()
                with ExitStack() as ph:
                    gtR = Ring([T("gt%d" % i, [128, 512], F32, stack=ph) for i in range(2)])
                    gsR = Ring([T("gs%d" % i, [128, 512], F32, stack=ph) for i in range(2)])
                    mg = T("mg", [128, 4, 1024], F32, stack=ph)
                    mb = T("mb", [128, 1024], BF16, stack=ph)
                    mT = T("mT", [128, 8, 512], BF16, stack=ph)
                    r4 = T("r4", [128, 4, 1024], F32, stack=ph)
                    roR = Ring([T("ro%d" % i, [128, 1024], F32, stack=ph) for i in range(2)])
                    p5 = T("p5", [128, NPB - NRES], F32, stack=ph)
                    dma("sp", p5[:], pb_d[:, NRES:NPB], w=[p5])
                    for u in range(4):
                        wb = load_w(win_v, C_GATE + u * 512, 512)
                        for tt_ in range(4):
                            acc = accR.next()
                            for kc in range(8):
                                mm(acc[:, :], hT.ap(kc * 512 + tt_ * 128, [[1, 128]]), wslice(wb, 512, kc, 0, 512),
                                   start=(kc == 0), stop=(kc == 7), r=[hT, wb], w=[acc])
                            gtt = gtR.next()
                            tt(gtt[:], acc[:, :], p5[:, u * 512:(u + 1) * 512], ALU.add, r=[acc, p5], w=[gtt])
                            gs = gsR.next()
                            act(gs[:], gtt[:], AF.Tanh, r=[gtt], w=[gs], scale=0.5)
                            if u < 2:
                                stt(mg.ap(tt_ * 1024 + u * 512, [[1, 512]]), gs[:], 1.0, yssm.ap(tt_ * 1024 + u * 512, [[1, 512]]),
                                    ALU.add, ALU.mult, r=[gs, yssm], w=[mg])
                            else:
                                stt(gs[:], gs[:], 1.0, yattn.ap(tt_ * 1024 + (u - 2) * 512, [[1, 512]]), ALU.add, ALU.mult,
                                    r=[gs, yattn], w=[gs])
                                tt(mg.ap(tt_ * 1024 + (u - 2) * 512, [[1, 512]]), mg.ap(tt_ * 1024 + (u - 2) * 512, [[1, 512]]),
                                   gs[:], ALU.add, r=[mg, gs], w=[mg])
                    for tt_ in range(4):
                        S.op("act", lambda e, tt_=tt_, mb=mb, mg=mg: e.mul(mb[:], mg.ap(tt_ * 1024, [[1, 1024]]), 0.5), [mg], [mb])
                        for kc in range(8):
                            tr(ptr.ap(kc * 128, [[1, 128]]), mb[:, kc * 128:(kc + 1) * 128], r=[mb], w=[ptr])
                        acopy(mT.ap(tt_ * 128, [[512, 8], [1, 128]]), ptr.ap(0, [[128, 8], [1, 128]]), r=[ptr], w=[mT])
                    for half in range(2):
                        wb = load_w(wout_v, half * 512, 512)
                        for tt_ in range(4):
                            acc = accR.next()
                            for kc in range(8):
                                mm(acc[:, :], mT.ap(kc * 512 + tt_ * 128, [[1, 128]]), wslice(wb, 512, kc, 0, 512),
                                   start=(kc == 0), stop=(kc == 7), r=[mT, wb], w=[acc])
                            acopy(r4.ap(tt_ * 1024 + half * 512, [[1, 512]]), acc[:, :], r=[acc], w=[r4])
                    for tt_ in range(4):
                        xt = xring.next()
                        dma("sp", xt[:], x_d[seq, t0 + tt_ * 128:t0 + (tt_ + 1) * 128, :], w=[xt])
                        r4s = r4.ap(tt_ * 1024, [[1, 1024]])
                        tt(r4s, r4s, xt[:], ALU.add, r=[r4, xt], w=[r4])
                        act(mb[:], r4s, AF.Square, r=[r4], w=[mb, sm], accum=sm[:, 0:1])
                        rstd_col(sm[:, 2:3], sm[:, 0:1], 1.0 / D_MODEL, cc[:, 0:1], sm)
                        ro = roR.next()
                        stt(ro[:], r4s, sm[:, 2:3], p5[:, FNW - NRES:FNW - NRES + 1024], ALU.mult, ALU.mult, r=[r4, sm, p5], w=[ro])
                        dma("sp", out_d[seq, t0 + tt_ * 128:t0 + (tt_ + 1) * 128, :], ro[:], r=[ro], w=[ro])
                S.barrier()
        S.final_wait("sp")
        S.emit()
    return nc, dbg_outs


def host_prep(inputs):
    f32 = np.float32
    w_in = np.asarray(inputs["w_in"], f32)[0]
    O_Z, O_XBC, O_DT, O_Q, O_K, O_V, O_AZ, O_QI, O_KI, O_WI, O_G = 0, 2048, 6144, 6176, 7200, 7264, 7328, 8352, 8864, 8928, 8936
    cols = []
    cols += list(range(O_DT, O_DT + 32)) + list(range(O_K, O_K + 64)) + list(range(O_V, O_V + 64))
    cols += list(range(O_KI, O_KI + 64)) + list(range(O_WI, O_WI + 8))
    for g in range(8):
        cols += list(range(O_Z + g * 256, O_Z + (g + 1) * 256))
        cols += list(range(O_XBC + g * 256, O_XBC + (g + 1) * 256))
        cols += list(range(O_XBC + 2048 + g * 128, O_XBC + 2048 + (g + 1) * 128))
        cols += list(range(O_XBC + 3072 + g * 128, O_XBC + 3072 + (g + 1) * 128))
    cols += list(range(O_Q, O_Q + 1024)) + list(range(O_QI, O_QI + 512)) + list(range(O_AZ, O_AZ + 1024))
    cols += list(range(O_G, O_G + 2048))
    cols = np.asarray(cols)
    assert cols.shape[0] == IN_TOTAL and np.unique(cols).shape[0] == IN_TOTAL
    win = np.ascontiguousarray(w_in[:, cols])

    pb = np.zeros((128, NPB), f32)

    def put(off, v):
        v = np.asarray(v, f32).reshape(-1)
        pb[:, off:off + v.shape[0]] = v[None, :]
    put(NW, inputs["norm_w"][0])
    put(FNW, inputs["final_norm_w"])
    put(GB, inputs["gate_b"][0])
    put(SNW, inputs["ssm_norm_w"][0])
    put(DTB, inputs["dt_bias"][0])
    put(ALOG, inputs["a_log"][0])
    put(DSK, inputs["d_skip"][0])
    put(KIW, inputs["idx_k_norm_w"][0])
    put(KIB, inputs["idx_k_norm_b"][0])

    conv_w = np.asarray(inputs["conv_w"], f32)[0]
    conv_b = np.asarray(inputs["conv_b"], f32)[0]
    pc = np.zeros((128, 160), f32)
    for g in range(8):
        for fi in range(4):
            ct = g * 4 + fi
            if fi < 2:
                ch0 = g * 256 + fi * 128
            elif fi == 2:
                ch0 = 2048 + g * 128
            else:
                ch0 = 3072 + g * 128
            pc[:, ct * 4:(ct + 1) * 4] = conv_w[:, ch0:ch0 + 128].T
            pc[:, 128 + ct] = conv_b[ch0:ch0 + 128]

    pk = np.zeros((128, NPK), f32)
    i = np.arange(128)
    pk[:, IDENT:IDENT + 128] = np.eye(128, dtype=f32)
    pk[:, TRI:TRI + 128] = (i[:, None] <= i[None, :]).astype(f32)
    pk[:, USTR:USTR + 128] = (i[:, None] > i[None, :]).astype(f32)
    pk[:, NEGM:NEGM + 128] = np.where(i[None, :] > i[:, None], f32(-1e30), f32(0))
    pk[:, ONES:ONES + 128] = 1.0
    pk[:, NEGT:NEGT + 128] = np.where(i[:, None] > i[None, :], f32(-1e30), f32(0))

    bf = ml_dtypes.bfloat16
    slopes = np.exp2(-8.0 * np.arange(1, 17, dtype=np.float64) / 16).astype(f32)
    s_hi = slopes.astype(bf)
    s_lo = (slopes - s_hi.astype(f32)).astype(bf)
    spos = np.arange(SEQ)
    kaug = np.zeros((5, SEQ), f32)
    kaug[0] = 1.0
    kaug[1] = spos % 128
    kaug[2] = (spos // 128) * 128
    kaug[3] = spos % 128
    kaug[4] = (spos // 128) * 128
    kaug = kaug.astype(bf)
    qaug = np.zeros((5, 16, SEQ), f32)
    qaug[0] = -(slopes[:, None].astype(np.float64) * spos[None, :]).astype(f32)
    qaug[1] = s_hi.astype(f32)[:, None]
    qaug[2] = s_hi.astype(f32)[:, None]
    qaug[3] = s_lo.astype(f32)[:, None]
    qaug[4] = s_lo.astype(f32)[:, None]
    qaug = qaug.astype(bf)
    shared = {
        "win": win,
        "wso": np.ascontiguousarray(np.asarray(inputs["w_ssm_out"], f32)[0]),
        "wao": np.ascontiguousarray(np.asarray(inputs["w_attn_out"], f32)[0]),
        "wout": np.ascontiguousarray(np.asarray(inputs["w_out"], f32)[0]),
        "pb": pb, "pc": pc, "pk": pk, "kaug": kaug, "qaug": qaug,
    }
    return shared


_CACHE = {}


def kernel(**inputs):
    x = np.asarray(inputs["x"], np.float32)
    shared = host_prep(inputs)
    if "nc" not in _CACHE:
        _CACHE["nc"] = build_program(2)[0]
    nc = _CACHE["nc"]
    in_maps = []
    for c in range(8):
        m = dict(shared)
        m["x"] = np.ascontiguousarray(x[2 * c:2 * c + 2])
        in_maps.append(m)
    res = run_bass_kernel_spmd(nc, in_maps, core_ids=list(range(8)))
    out = np.concatenate([np.asarray(r["out"], np.float32) for r in res.results], axis=0)
    return out
```

```python
import numpy as np
import ml_dtypes
from contextlib import ExitStack
import concourse.bass as bass
import concourse.mybir as mybir
from concourse.bass_utils import run_bass_kernel_spmd

F32 = mybir.dt.float32
BF16 = mybir.dt.bfloat16
U32 = mybir.dt.uint32
AF = mybir.ActivationFunctionType
ALU = mybir.AluOpType
AX = mybir.AxisListType

D_MODEL = 1024
SEQ = 2048
IN_TOTAL = 10984
NIT = 16
TOPK = 256
EPS = 1e-6
IDX_SCALE = (8 ** -0.5) * (64 ** -0.5)

NW, SNW, DTB, ALOG, DSK, KIW, KIB, NRES, GB, FNW, NPB = 0, 1024, 3072, 3104, 3136, 3168, 3232, 3296, 3296, 5344, 6368
IDENT, TRI, USTR, NEGM, ONES, NEGT, NPK = 0, 128, 256, 384, 512, 640, 768
C_SMALL, C_GRP, C_Q, C_QI, C_AZ, C_GATE = 0, 232, 6376, 7400, 7912, 8936


class Dep:
    __slots__ = ("name", "w", "r")

    def __init__(self, name=""):
        self.name = name
        self.w = None
        self.r = {}


class Sched:
    ENGS = ("pe", "act", "dve", "pool", "sp")

    def __init__(self, nc, stack, n_dma_sems=24):
        self.nc = nc
        self.lists = {e: [] for e in self.ENGS}
        self.sems = {}
        self.cnt = {}
        for e in ("pe", "act", "dve", "pool"):
            self.sems[e] = stack.enter_context(nc.semaphore("s_" + e))
            self.cnt[e] = 0
        self.dma_pool = {}
        for q, n in (("sp", n_dma_sems), ("pool", 8)):
            keys = []
            for i in range(n):
                k = "d_%s_%d" % (q, i)
                self.sems[k] = stack.enter_context(nc.semaphore(k))
                self.cnt[k] = 0
                keys.append(k)
            self.dma_pool[q] = [keys, 0]
        self.seen = {e: {} for e in self.ENGS}
        self.n_ops = 0

    def _needs(self, eng, reads, writes):
        needs = {}

        def add(k, v):
            if v > needs.get(k, 0):
                needs[k] = v
        for d in reads:
            if d.w is not None and not (eng == "pe" and d.w[0] == "pe"):
                add(*d.w)
        for d in writes:
            if d.w is not None and not (eng == "pe" and d.w[0] == "pe"):
                add(*d.w)
            for k, v in d.r.items():
                if not (eng == "pe" and k == "pe"):
                    add(k, v)
        out = []
        seen = self.seen[eng]
        for k, v in needs.items():
            if seen.get(k, 0) >= v:
                continue
            seen[k] = v
            out.append((k, v))
        return out

    def op(self, eng, fn, reads=(), writes=()):
        reads = [getattr(d, "dep", d) for d in reads]
        writes = [getattr(d, "dep", d) for d in writes]
        waits = self._needs(eng, reads, writes)
        self.cnt[eng] += 1
        v = self.cnt[eng]
        self.lists[eng].append((waits, fn, eng, 1))
        for d in reads:
            d.r[eng] = v
        for d in writes:
            d.w = (eng, v)
            d.r = {}
        self.n_ops += 1

    def dma(self, q, fn, reads=(), writes=()):
        reads = [getattr(d, "dep", d) for d in reads]
        writes = [getattr(d, "dep", d) for d in writes]
        keys, idx = self.dma_pool[q]
        k = keys[idx % len(keys)]
        self.dma_pool[q][1] = idx + 1
        waits = self._needs(q, reads, writes)
        prev = self.cnt[k]
        if prev > 0 and self.seen[q].get(k, 0) < prev:
            self.seen[q][k] = prev
            waits.append((k, prev))
        self.cnt[k] += 16
        v = self.cnt[k]
        self.lists[q].append((waits, fn, k, 16))
        for d in reads:
            d.r[k] = v
        for d in writes:
            d.w = (k, v)
            d.r = {}
        self.n_ops += 1

    def barrier(self, engs=("pe", "act", "dve", "sp")):
        for e in engs:
            waits = []
            for k, v in self.cnt.items():
                if k == e or k.startswith("d_pool") or v == 0:
                    continue
                if self.seen[e].get(k, 0) < v:
                    self.seen[e][k] = v
                    waits.append((k, v))
            if waits:
                self.lists[e].append((waits, None, None, 0))

    def final_wait(self, eng):
        waits = []
        for k, v in self.cnt.items():
            if k == eng or v == 0:
                continue
            if self.seen[eng].get(k, 0) < v:
                self.seen[eng][k] = v
                waits.append((k, v))
        self.lists[eng].append((waits, None, None, 0))

    def emit(self):
        nc = self.nc
        sems = self.sems
        lists = self.lists

        def replay(e, lst):
            for waits, fn, k, inc in lst:
                for (wk, wv) in waits:
                    e.wait_ge(sems[wk], wv)
                if fn is not None:
                    fn(e).then_inc(sems[k], inc)

        with nc.Block() as block:
            @block.tensor
            def _(e):
                replay(e, lists["pe"])

            @block.scalar
            def _(e):
                replay(e, lists["act"])

            @block.vector
            def _(e):
                replay(e, lists["dve"])

            @block.gpsimd
            def _(e):
                replay(e, lists["pool"])

            @block.sync
            def _(e):
                replay(e, lists["sp"])


def build_program(n_seq=2, dbg=None):
    nc = bass.Bass("TRN2", target_bir_lowering=False)

    def dram(name, shape, dt=F32, kind="ExternalInput"):
        return nc.dram_tensor(name, shape, dt, kind=kind).ap()

    x_d = dram("x", [n_seq, SEQ, D_MODEL])
    win_d = dram("win", [D_MODEL, IN_TOTAL])
    wso_d = dram("wso", [2048, 1024])
    wao_d = dram("wao", [1024, 1024])
    wout_d = dram("wout", [1024, 1024])
    pb_d = dram("pb", [128, NPB])
    pc_d = dram("pc", [128, 160])
    pk_d = dram("pk", [128, NPK])
    kaug_d = dram("kaug", [5, SEQ], BF16)
    qaug_d = dram("qaug", [5, 16, SEQ], BF16)
    out_d = dram("out", [n_seq, SEQ, D_MODEL], kind="ExternalOutput")
    dbg_outs = {}

    win_v = win_d.rearrange("(kc p) n -> p kc n", p=128)
    wso_v = wso_d.rearrange("(kc p) n -> p kc n", p=128)
    wao_v = wao_d.rearrange("(kc p) n -> p kc n", p=128)
    wout_v = wout_d.rearrange("(kc p) n -> p kc n", p=128)

    with ExitStack() as st0:
        S = Sched(nc, st0)
        uid = [0]

        class T:
            def __init__(self, name, shape, dt, psum=False, stack=st0):
                uid[0] += 1
                nm = "%s_%d" % (name, uid[0])
                alloc = nc.psum_tensor if psum else nc.sbuf_tensor
                self.t = stack.enter_context(alloc(nm, list(shape), dt))
                self.dep = Dep(nm)
                self.row = int(np.prod(shape[1:]))

            def __getitem__(self, k):
                return self.t[k]

            def ap(self, col0, dims, p0=0, np_=128):
                return bass.AP(self.t, p0 * self.row + col0, [[self.row, np_]] + [list(d) for d in dims])

        class Ring:
            def __init__(self, tiles):
                self.tiles = tiles
                self.i = 0

            def next(self):
                t = self.tiles[self.i % len(self.tiles)]
                self.i += 1
                return t

        def mm(out, lhsT, rhs, start=True, stop=True, r=(), w=()):
            S.op("pe", lambda e: e.matmul(out, lhsT=lhsT, rhs=rhs, start=start, stop=stop), r, w)

        def tr(out, in_, r=(), w=()):
            S.op("pe", lambda e: e.transpose(out=out, in_=in_, identity=identb[:]), list(r) + [identb], w)

        def act(out, in_, func, r=(), w=(), bias=None, scale=None, accum=None):
            kw = {}
            if bias is not None:
                kw["bias"] = bias
            if scale is not None:
                kw["scale"] = scale
            if accum is not None:
                kw["accum_out"] = accum
            S.op("act", lambda e: e.activation(out=out, in_=in_, func=func, **kw), r, w)

        def acopy(out, in_, r=(), w=()):
            S.op("act", lambda e: e.copy(out=out, in_=in_), r, w)

        def tt(out, a, b, op, r=(), w=(), eng="dve"):
            S.op(eng, lambda e: e.tensor_tensor(out=out, in0=a, in1=b, op=op), r, w)

        def ts(out, a, s1, op0, r=(), w=(), s2=None, op1=None, accum=None):
            kw = {}
            if op1 is not None:
                kw["op1"] = op1
            if accum is not None:
                kw["accum_out"] = accum
            S.op("dve", lambda e: e.tensor_scalar(out=out, in0=a, scalar1=s1, scalar2=s2, op0=op0, **kw), r, w)

        def stt(out, a, s, b, op0, op1, r=(), w=()):
            S.op("dve", lambda e: e.scalar_tensor_tensor(out=out, in0=a, scalar=s, in1=b, op0=op0, op1=op1), r, w)

        def vcopy(out, in_, r=(), w=()):
            S.op("dve", lambda e: e.tensor_copy(out=out, in_=in_), r, w)

        def memset(ap, val, w=()):
            S.op("dve", lambda e: e.memset(ap, val), (), w)

        def recip(out, in_, r=(), w=()):
            S.op("dve", lambda e: e.reciprocal(out=out, in_=in_), r, w)

        def dma(q, out, in_, r=(), w=()):
            S.dma(q, lambda e: e.dma_start(out=out, in_=in_), r, w)

        def rstd_col(out_col, ssq_col, scale, eps_col, tile):
            act(ssq_col, ssq_col, AF.Sqrt, r=[tile, cc], w=[tile], bias=eps_col, scale=scale)
            recip(out_col, ssq_col, r=[tile], w=[tile])

        def interleave(*gens):
            alive = list(gens)
            while alive:
                for g_ in list(alive):
                    try:
                        next(g_)
                    except StopIteration:
                        alive.remove(g_)

        def dump(name, tile, ap, shape):
            if dbg is None or name not in dbg:
                return
            d = nc.dram_tensor("dbg_" + name, list(shape), tile.t.dtype, kind="ExternalOutput").ap()
            dbg_outs[name] = "dbg_" + name
            dma("sp", d, ap, r=[tile], w=[])

        pb = T("pb", [128, NRES], F32)
        pc = T("pc", [128, 160], F32)
        pk = T("pk", [128, NPK], F32)
        identb = T("identb", [128, 128], BF16)
        cc = T("cc", [128, 8], F32)
        Abc = T("Abc", [128, 32], F32)
        Wsm = T("Wsm", [128, 8, 232], BF16)
        KA = T("KA", [128, SEQ], BF16)
        KIN = T("KIN", [128, SEQ], BF16)
        VA = T("VA", [128, 16, 66], BF16)
        St = T("St", [128, 8, 256], F32)
        Sb = T("Sb", [128, 8, 256], BF16)
        St_deps = [Dep("St%d" % g) for g in range(8)]
        Sb_deps = [Dep("Sb%d" % g) for g in range(8)]
        halo = T("halo", [128, 32, 3], F32)
        halo_deps = [Dep("halo%d" % g) for g in range(8)]
        xring = Ring([T("xt%d" % i, [128, 1024], F32) for i in range(2)])
        hb = T("hb", [128, 1024], BF16)
        hT = T("hT", [128, 8, 512], BF16)
        sm = T("sm", [128, 16], F32)
        dt4 = T("dt4", [128, 4, 32], F32)
        a4 = T("a4", [128, 4, 32], F32)
        eacs4 = T("eacs4", [128, 4, 32], F32)
        cdb4 = T("cdb4", [128, 4, 32], F32)
        dtd4 = T("dtd4", [128, 4, 32], F32)
        wis = T("wis", [128, 4, 8], F32)
        s32 = [T("s32_%d" % i, [128, 32], F32) for i in range(4)]
        kvb = T("kvb", [128, 128], BF16)
        kif = T("kif", [128, 64], F32)
        wring = Ring([T("wb%d" % i, [128, 6144], BF16) for i in range(3)])
        yssm = T("yssm", [128, 4, 1024], BF16)
        yattn = T("yattn", [128, 4, 1024], BF16)

        accR = Ring([T("acc%d" % i, [128, 512], F32, psum=True) for i in range(2)])
        ptr = T("ptr", [128, 1024], BF16, psum=True)
        bk3 = T("bk3", [128, 512], F32, psum=True)
        bk4 = T("bk4", [128, 512], F32, psum=True)
        bk5 = T("bk5", [128, 512], F32, psum=True)
        bk6 = T("bk6", [128, 512], F32, psum=True)
        bk7 = T("bk7", [128, 512], F32, psum=True)
        cb_dep = acs_dep = bk4.dep
        sts_dep = yoff_dep = bk6.dep

        dma("sp", pb[:], pb_d[:, 0:NRES], w=[pb])
        dma("sp", pc[:], pc_d, w=[pc])
        dma("sp", pk[:], pk_d, w=[pk])
        vcopy(identb[:], pk[:, IDENT:IDENT + 128], r=[pk], w=[identb])
        memset(cc[:, 0:1], EPS, w=[cc])
        memset(cc[:, 1:2], 1.0, w=[cc])
        memset(cc[:, 2:3], -1e29, w=[cc])
        memset(cc[:, 3:4], 4.0 * EPS, w=[cc])
        ts(pc[:], pc[:], 0.5, ALU.mult, r=[pc], w=[pc])
        act(Abc[:], pb[:, ALOG:ALOG + 32], AF.Exp, r=[pb], w=[Abc])
        ts(Abc[:], Abc[:], -1.0, ALU.mult, r=[Abc], w=[Abc])
        memset(KA[:], 0.0, w=[KA])
        dma("sp", KA.ap(0, [[1, SEQ]], p0=64, np_=5), kaug_d, w=[KA])
        memset(VA[:], 2.0, w=[VA])
        dma("pool", Wsm[:], win_v[:, :, C_SMALL:C_SMALL + 232], w=[Wsm])

        def load_w(src_v, c0, n, nkc=8):
            wb = wring.next()
            dma("pool", wb.ap(0, [[n, nkc], [1, n]]), src_v[:, :, c0:c0 + n], w=[wb])
            return wb

        def wslice(wb, n, kc, a, b):
            return wb.ap(kc * n + a, [[1, b - a]])

        for seq in range(n_seq):
            memset(St[:], 0.0, w=St_deps)
            memset(Sb[:], 0.0, w=Sb_deps)
            memset(halo[:], 0.0, w=halo_deps)
            for stl in range(4):
                t0 = stl * 512
                pre_w = [load_w(win_v, C_GRP, 768)]
                for tt_ in range(4):
                    xt = xring.next()
                    dma("sp", xt[:], x_d[seq, t0 + tt_ * 128:t0 + (tt_ + 1) * 128, :], w=[xt])
                    act(hb[:], xt[:], AF.Square, r=[xt], w=[hb, sm], accum=sm[:, 0:1])
                    rstd_col(sm[:, 2:3], sm[:, 0:1], 1.0 / D_MODEL, cc[:, 0:1], sm)
                    stt(hb[:], xt[:], sm[:, 2:3], pb[:, NW:NW + 1024], ALU.mult, ALU.mult, r=[xt, sm, pb], w=[hb])
                    for kc in range(8):
                        tr(ptr.ap(kc * 128, [[1, 128]]), hb[:, kc * 128:(kc + 1) * 128], r=[hb], w=[ptr])
                    acopy(hT.ap(tt_ * 128, [[512, 8], [1, 128]]), ptr.ap(0, [[128, 8], [1, 128]]), r=[ptr], w=[hT])
                if seq == 0 and stl == 0:
                    dump("hT", hT, hT[:], [128, 8, 512])

                for tt_ in range(4):
                    gt = stl * 4 + tt_
                    acc = accR.next()
                    for kc in range(8):
                        mm(acc[:, 0:232], hT.ap(kc * 512 + tt_ * 128, [[1, 128]]), Wsm.ap(kc * 232, [[1, 232]]),
                           start=(kc == 0), stop=(kc == 7), r=[hT, Wsm], w=[acc])
                    x32, e32, acs_sb, dd = s32
                    tt(x32[:], acc[:, 0:32], pb[:, DTB:DTB + 32], ALU.add, r=[acc, pb], w=[x32])
                    act(e32[:], x32[:], AF.Exp, r=[x32], w=[e32])
                    act(dt4.ap(tt_ * 32, [[1, 32]]), e32[:], AF.Ln, r=[e32, cc], w=[dt4], bias=cc[:, 1:2], scale=1.0)
                    tt(a4.ap(tt_ * 32, [[1, 32]]), dt4.ap(tt_ * 32, [[1, 32]]), Abc[:], ALU.mult, r=[dt4, Abc], w=[a4])
                    acopy(kvb[:, 0:64], acc[:, 32:96], r=[acc], w=[kvb])
                    acopy(VA.ap(gt * 66, [[1, 64]]), acc[:, 96:160], r=[acc], w=[VA])
                    S.op("dve", lambda e, acc=acc: e.bn_stats(out=sm[:, 4:10], in_=acc[:, 160:224]), [acc], [sm])
                    S.op("dve", lambda e: e.bn_aggr(out=sm[:, 10:12], in_=sm[:, 4:10]), [sm], [sm])
                    rstd_col(sm[:, 13:14], sm[:, 11:12], 1.0, cc[:, 0:1], sm)
                    ts(kif[:], acc[:, 160:224], sm[:, 10:11], ALU.subtract, r=[acc, sm], w=[kif], s2=sm[:, 13:14], op1=ALU.mult)
                    tt(kif[:], kif[:], pb[:, KIW:KIW + 64], ALU.mult, r=[kif, pb], w=[kif])
                    tt(kvb[:, 64:128], kif[:], pb[:, KIB:KIB + 64], ALU.add, r=[kif, pb], w=[kvb])
                    ts(wis.ap(tt_ * 8, [[1, 8]]), acc[:, 224:232], IDX_SCALE, ALU.mult, r=[acc], w=[wis])
                    tr(ptr.ap(0, [[1, 128]], np_=64), kvb[:, 0:64], r=[kvb], w=[ptr])
                    tr(ptr.ap(128, [[1, 128]], np_=64), kvb[:, 64:128], r=[kvb], w=[ptr])
                    acopy(KA.ap(gt * 128, [[1, 128]], np_=64), ptr.ap(0, [[1, 128]], np_=64), r=[ptr], w=[KA])
                    acopy(KIN.ap(gt * 128, [[1, 128]], np_=64), ptr.ap(128, [[1, 128]], np_=64), r=[ptr], w=[KIN])
                    mm(bk4[:, 128:160], pk[:, TRI:TRI + 128], a4.ap(tt_ * 32, [[1, 32]]), r=[pk, a4], w=[acs_dep])
                    mm(bk4[:, 160:192], pk[:, ONES:ONES + 128], a4.ap(tt_ * 32, [[1, 32]]), r=[pk, a4], w=[acs_dep])
                    acopy(acs_sb[:], bk4[:, 128:160], r=[acs_dep], w=[acs_sb])
                    act(eacs4.ap(tt_ * 32, [[1, 32]]), bk4[:, 128:160], AF.Exp, r=[acs_dep], w=[eacs4])
                    act(cdb4.ap(tt_ * 32, [[1, 32]]), bk4[:, 160:192], AF.Exp, r=[acs_dep], w=[cdb4])
                    tt(dd[:], bk4[:, 160:192], acs_sb[:], ALU.subtract, r=[acs_dep, acs_sb], w=[dd])
                    act(dd[:], dd[:], AF.Exp, r=[dd], w=[dd])
                    tt(dtd4.ap(tt_ * 32, [[1, 32]]), dt4.ap(tt_ * 32, [[1, 32]]), dd[:], ALU.mult, r=[dt4, dd], w=[dtd4])
                if seq == 0 and stl == 0:
                    dump("dt4", dt4, dt4[:], [128, 4, 32])
                    dump("KA", KA, KA[:], [128, SEQ])
                    dump("KIN", KIN, KIN[:], [128, SEQ])
                    dump("VA", VA, VA[:], [128, 16, 66])
                    dump("wis", wis, wis[:], [128, 4, 8])
                    dump("eacs4", eacs4, eacs4[:], [128, 4, 32])
                    dump("dtd4", dtd4, dtd4[:], [128, 4, 32])

                S.barrier(("pe", "act", "dve", "sp", "pool"))
                with ExitStack() as ph:
                    Upre = T("Upre", [128, 4, 515], F32, stack=ph)
                    Upre_d = [Dep("Upre%d" % i) for i in range(4)]
                    cv = T("cv", [128, 4, 512], F32, stack=ph)
                    cv_d = [Dep("cv%d" % i) for i in range(4)]
                    thR = Ring([T("th%d" % i, [128, 512], F32, stack=ph) for i in range(2)])
                    xbP = [T("xb%d" % i, [128, 4, 512], BF16, stack=ph) for i in range(2)]
                    xb_d = [[Dep("xb%d_%d" % (i, f)) for f in range(4)] for i in range(2)]
                    zsP = [T("zs%d" % i, [128, 4, 256], F32, stack=ph) for i in range(2)]
                    xtkP = [T("xtk%d" % i, [128, 4, 384], BF16, stack=ph) for i in range(2)]
                    rhsAR = Ring([T("rhsA%d" % i, [128, 512], F32, stack=ph) for i in range(2)])
                    CBmR = Ring([T("CBm%d" % i, [128, 128], F32, stack=ph) for i in range(2)])
                    EsegR = Ring([T("Eseg%d" % i, [128, 512], BF16, stack=ph) for i in range(2)])
                    MTR = Ring([T("MT%d" % i, [128, 512], BF16, stack=ph) for i in range(2)])
                    xcR = Ring([T("xc%d" % i, [128, 256], BF16, stack=ph) for i in range(2)])
                    xcdR = Ring([T("xcd%d" % i, [128, 256], BF16, stack=ph) for i in range(2)])
                    xsDR = Ring([T("xsD%d" % i, [128, 256], F32, stack=ph) for i in range(2)])
                    t1R = Ring([T("t1%d" % i, [128, 256], F32, stack=ph) for i in range(2)])
                    yzR = Ring([T("yz%d" % i, [128, 256], F32, stack=ph) for i in range(4)])
                    smgP = [T("smg%d" % i, [128, 12], F32, stack=ph) for i in range(2)]
                    yzs = {}
                    yjk = T("yjk", [128, 256], BF16, stack=ph)
                    yNR = Ring([T("yN%d" % i, [128, 256], BF16, stack=ph) for i in range(2)])
                    yNT = T("yNT", [128, 16, 512], BF16, stack=ph)
                    A0 = accR.tiles[0]
                    segR = Ring([bk3, bk7])
                    cbR = Ring([(0, bk4.dep)])
                    ydR = Ring([(0, bk5.dep)])
                    soR = Ring([(bk6, bk6.dep, bk6.dep), (accR.tiles[1], accR.tiles[1].dep, accR.tiles[1].dep)])
                    ptrA_d, ptrB_d = ptr.dep, [ptr.dep, ptr.dep]
                    state_done = {}

                    def stageA(g):
                        par = g % 2
                        xb, zs, xtk, xbd = xbP[par], zsP[par], xtkP[par], xb_d[par]
                        wb = wq.pop(g)
                        vcopy(Upre.ap(0, [[515, 4], [1, 3]]), halo.ap(g * 12, [[3, 4], [1, 3]]), r=[halo_deps[g]], w=Upre_d)
                        for fi in range(4):
                            for kc in range(8):
                                mm(A0[:, :], wslice(wb, 768, kc, 256 + fi * 128, 256 + (fi + 1) * 128), hT.ap(kc * 512, [[1, 512]]),
                                   start=(kc == 0), stop=(kc == 7), r=[wb, hT], w=[A0])
                            acopy(Upre.ap(fi * 515 + 3, [[1, 512]]), A0[:, :], r=[A0], w=[Upre_d[fi]])
                            yield
                        vcopy(halo.ap(g * 12, [[3, 4], [1, 3]]), Upre.ap(512, [[515, 4], [1, 3]]), r=Upre_d, w=[halo_deps[g]])
                        for fi in range(4):
                            ct = g * 4 + fi
                            cvf = cv.ap(fi * 512, [[1, 512]])
                            act(cvf, Upre.ap(fi * 515, [[1, 512]]), AF.Identity, r=[Upre_d[fi], pc], w=[cv_d[fi]],
                                bias=pc[:, 128 + ct:129 + ct], scale=pc[:, ct * 4:ct * 4 + 1])
                            yield
                            for k in range(1, 4):
                                stt(cvf, Upre.ap(fi * 515 + k, [[1, 512]]), pc[:, ct * 4 + k:ct * 4 + k + 1], cvf, ALU.mult, ALU.add,
                                    r=[Upre_d[fi], pc, cv_d[fi]], w=[cv_d[fi]])
                                yield
                            th = thR.next()
                            act(th[:], cvf, AF.Tanh, r=[cv_d[fi]], w=[th])
                            stt(xb.ap(fi * 512, [[1, 512]]), th[:], 1.0, cvf, ALU.add, ALU.mult, r=[th, cv_d[fi]], w=[xbd[fi]])
                            yield
                        for c in range(4):
                            for kc in range(8):
                                mm(A0[:, 0:256], hT.ap(kc * 512 + c * 128, [[1, 128]]), wslice(wb, 768, kc, 0, 256),
                                   start=(kc == 0), stop=(kc == 7), r=[hT, wb], w=[A0])
                            th = thR.next()
                            act(th[:, 0:256], A0[:, 0:256], AF.Tanh, r=[A0], w=[th], scale=0.5)
                            stt(zs.ap(c * 256, [[1, 256]]), th[:, 0:256], 1.0, A0[:, 0:256], ALU.add, ALU.mult, r=[th, A0], w=[zs])
                            yield
                        for c in range(4):
                            for fi in range(3):
                                tr(ptr.ap(fi * 128, [[1, 128]]), xb.ap(fi * 512 + c * 128, [[1, 128]]), r=[xbd[fi]], w=[ptrA_d])
                            acopy(xtk.ap(c * 384, [[1, 384]]), ptr.ap(0, [[1, 384]]), r=[ptrA_d], w=[xtk])
                            yield

                    def chunkB(g, c):
                        par = g % 2
                        xb, zs, xtk, xbd = xbP[par], zsP[par], xtkP[par], xb_d[par]
                        hsl = c * 32 + g * 4
                        rhsA, CBm, Eseg, MT = rhsAR.next(), CBmR.next(), EsegR.next(), MTR.next()
                        xc, xcd, xsD, t1, yz = xcR.next(), xcdR.next(), xsDR.next(), t1R.next(), yzR.next()
                        smg = smgP[g % 2]
                        seg = segR.next()
                        cbo, cbd = cbR.next()
                        ydo, ydd = ydR.next()
                        sob, stsd, yofd = soR.next()
                        tt(rhsA.ap(0, [[128, 4], [1, 128]]), pk.ap(TRI, [[0, 4], [1, 128]]), a4.ap(hsl, [[1, 4], [0, 128]]),
                           ALU.mult, r=[pk, a4], w=[rhsA], eng="pool")
                        xs3 = xtk.ap(c * 384, [[64, 4], [1, 64]])
                        tt(xc.ap(0, [[64, 4], [1, 64]]), xs3, dt4.ap(hsl, [[1, 4], [0, 64]]), ALU.mult, r=[xtk, dt4], w=[xc], eng="pool")
                        tt(xcd.ap(0, [[64, 4], [1, 64]]), xs3, dtd4.ap(hsl, [[1, 4], [0, 64]]), ALU.mult, r=[xtk, dtd4], w=[xcd], eng="pool")
                        tt(xsD.ap(0, [[64, 4], [1, 64]]), xs3, pb.ap(DSK + g * 4, [[1, 4], [0, 64]]), ALU.mult, r=[xtk, pb], w=[xsD], eng="pool")
                        yield
                        mm(seg[:, :], pk[:, USTR:USTR + 128], rhsA[:, :], r=[pk, rhsA], w=[seg])
                        mm(bk4[:, cbo:cbo + 128], xb.ap(2 * 512 + c * 128, [[1, 128]]), xb.ap(3 * 512 + c * 128, [[1, 128]]),
                           r=[xbd[2], xbd[3]], w=[cbd])
                        tt(CBm[:], bk4[:, cbo:cbo + 128], pk[:, TRI:TRI + 128], ALU.mult, r=[cbd, pk], w=[CBm])
                        act(Eseg[:], seg[:, :], AF.Exp, r=[seg], w=[Eseg])
                        yield
                        tt(MT.ap(0, [[128, 4], [1, 128]]), Eseg.ap(0, [[128, 4], [1, 128]]), CBm.ap(0, [[0, 4], [1, 128]]),
                           ALU.mult, r=[Eseg, CBm], w=[MT])
                        yield
                        while c > 0 and not state_done.get((g, c - 1)):
                            yield
                        mm(sob[:, 256:512], xb.ap(3 * 512 + c * 128, [[1, 128]]), Sb.ap(g * 256, [[1, 256]]),
                           r=[xbd[3], Sb_deps[g]], w=[yofd])
                        for j in range(4):
                            mm(bk5[:, ydo + j * 64:ydo + (j + 1) * 64], MT[:, j * 128:(j + 1) * 128], xc[:, j * 64:(j + 1) * 64],
                               start=True, stop=True, r=[MT, xc], w=[ydd])
                        mm(sob[:, 0:256], xtk.ap(c * 384 + 256, [[1, 128]]), xcd[:], r=[xtk, xcd], w=[stsd])
                        tt(St.ap(g * 256, [[64, 4], [1, 64]]), St.ap(g * 256, [[64, 4], [1, 64]]),
                           cdb4.ap(hsl, [[1, 4], [0, 64]]), ALU.mult, r=[St_deps[g], cdb4], w=[St_deps[g]])
                        tt(St.ap(g * 256, [[1, 256]]), St.ap(g * 256, [[1, 256]]), sob[:, 0:256], ALU.add,
                           r=[St_deps[g], stsd], w=[St_deps[g]])
                        acopy(Sb.ap(g * 256, [[1, 256]]), St.ap(g * 256, [[1, 256]]), r=[St_deps[g]], w=[Sb_deps[g]])
                        state_done[(g, c)] = True
                        tt(t1.ap(0, [[64, 4], [1, 64]]), sob.ap(256, [[64, 4], [1, 64]]), eacs4.ap(hsl, [[1, 4], [0, 64]]),
                           ALU.mult, r=[yofd, eacs4], w=[t1])
                        tt(t1[:], bk5[:, ydo:ydo + 256], t1[:], ALU.add, r=[ydd, t1], w=[t1])
                        tt(t1[:], xsD[:], t1[:], ALU.add, r=[xsD, t1], w=[t1])
                        yield
                        tt(yz[:], t1[:], zs.ap(c * 256, [[1, 256]]), ALU.mult, r=[t1, zs], w=[yz])
                        act(yjk[:], yz[:], AF.Square, r=[yz], w=[yjk, smg], accum=smg[:, c:c + 1])
                        yzs[(g, c)] = yz
                        yield

                    def normB(g):
                        smg = smgP[g % 2]
                        act(smg[:, 4:8], smg[:, 0:4], AF.Sqrt, r=[smg, cc], w=[smg], bias=cc[:, 3:4], scale=1.0 / 256)
                        recip(smg[:, 8:12], smg[:, 4:8], r=[smg], w=[smg])
                        for c in range(4):
                            yz = yzs.pop((g, c))
                            yN = yNR.next()
                            stt(yN[:], yz[:], smg[:, 8 + c:9 + c], pb[:, SNW + g * 256:SNW + (g + 1) * 256], ALU.mult, ALU.mult,
                                r=[yz, smg, pb], w=[yN])
                            for i in range(2):
                                tr(ptr.ap(512 + (c % 2) * 256 + i * 128, [[1, 128]]), yN[:, i * 128:(i + 1) * 128], r=[yN], w=[ptrB_d[c % 2]])
                            acopy(yNT.ap((g * 2) * 512 + c * 128, [[512, 2], [1, 128]]), ptr.ap(512 + (c % 2) * 256, [[128, 2], [1, 128]]),
                                  r=[ptrB_d[c % 2]], w=[yNT])

                    def run_group(g):
                        if g + 2 < 8:
                            wq[g + 2] = load_w(win_v, C_GRP + (g + 2) * 768, 768)
                        pending = [chunkB(g, c) for c in range(4)]
                        active = [pending.pop(0), pending.pop(0)]
                        ag = stageA(g + 1) if g < 7 else None
                        while active or ag is not None:
                            for g_ in list(active):
                                try:
                                    next(g_)
                                except StopIteration:
                                    active.remove(g_)
                                    if pending:
                                        active.append(pending.pop(0))
                            for _rep in range(2):
                                if ag is not None:
                                    try:
                                        next(ag)
                                    except StopIteration:
                                        ag = None

                    wq = {0: pre_w.pop(), 1: load_w(win_v, C_GRP + 768, 768)}
                    for _ in stageA(0):
                        pass
                    for g in range(8):
                        run_group(g)
                        normB(g)
                    if seq == 0 and stl == 0:
                        dump("yNT", yNT, yNT[:], [128, 16, 512])
                    obanks = [accR.tiles[0], accR.tiles[1], bk3, bk7]
                    for half in range(2):
                        for kh in range(2):
                            wb = wring.next()
                            dma("pool", wb.ap(0, [[512, 8], [1, 512]]), wso_v[:, kh * 8:(kh + 1) * 8, half * 512:(half + 1) * 512], w=[wb])
                            for tt_ in range(4):
                                for kc in range(8):
                                    mm(obanks[tt_][:, :], yNT.ap((kh * 8 + kc) * 512 + tt_ * 128, [[1, 128]]), wslice(wb, 512, kc, 0, 512),
                                       start=(kh == 0 and kc == 0), stop=(kh == 1 and kc == 7), r=[yNT, wb], w=[obanks[tt_]])
                        for tt_ in range(4):
                            acopy(yssm.ap(tt_ * 1024 + half * 512, [[1, 512]]), obanks[tt_][:, :], r=[obanks[tt_]], w=[yssm])
                if seq == 0 and stl == 0:
                    dump("yssm", yssm, yssm[:], [128, 4, 1024])

                S.barrier()
                with ExitStack() as ph:
                    QA = T("QA", [128, 16, 512], BF16, stack=ph)
                    QI = T("QI", [128, 8, 512], BF16, stack=ph)
                    zA = T("zA", [128, 4, 1024], BF16, stack=ph)
                    isc = T("isc", [128, SEQ], F32, stack=ph)
                    rlR = Ring([T("rl%d" % i, [128, 512], F32, stack=ph) for i in range(2)])
                    maskb = T("maskb", [128, SEQ], BF16, stack=ph)
                    maskTP = [T("maskT%d" % i, [128, 16, 128], BF16, stack=ph) for i in range(2)]
                    ER = Ring([T("E%d" % i, [128, 512], BF16, stack=ph) for i in range(3)])
                    PR = Ring([T("P%d" % i, [128, 512], BF16, stack=ph) for i in range(3)])
                    og = T("og", [128, 1024], BF16, stack=ph)
                    Lm = T("Lm", [128, 512], F32, stack=ph)
                    OTs = T("OTs", [128, 512], F32, stack=ph)
                    thA = Lm
                    oT = T("oT", [128, 8, 512], BF16, stack=ph)
                    bs = T("bs", [128, 4], F32, stack=ph)
                    rc = T("rc", [128, 4], F32, stack=ph)
                    bu = T("bu", [128, 2], U32, stack=ph)
                    LR = Ring([bk3, accR.tiles[1]])
                    A0 = accR.tiles[0]
                    A1 = accR.tiles[1]
                    Ob = [bk4, bk5, bk6, bk7]
                    Odeps = [Ob[j].dep for j in range(4)]
                    ptrA_d = ptrB_d = ptr.dep

                    dma("sp", QA.ap(0, [[512, 16], [1, 512]], p0=64, np_=5), qaug_d[:, :, t0:t0 + 512], w=[QA])
                    wb = load_w(win_v, C_QI, 512)
                    for h in range(8):
                        for kc in range(8):
                            mm(A0[0:64, :], wslice(wb, 512, kc, h * 64, (h + 1) * 64), hT.ap(kc * 512, [[1, 512]]),
                               start=(kc == 0), stop=(kc == 7), r=[wb, hT], w=[A0])
                        acopy(QI.ap(h * 512, [[1, 512]], np_=64), A0[0:64, :], r=[A0], w=[QI])

                    def stageI(tt_):
                        qb = stl * 4 + tt_
                        SL = (qb + 1) * 128
                        maskT = maskTP[tt_ % 2]
                        for c4 in range((SL + 511) // 512):
                            w_ = min(512, SL - c4 * 512)
                            for h in range(8):
                                mm(A0[:, 0:w_], QI.ap(h * 512 + tt_ * 128, [[1, 128]], np_=64), KIN.ap(c4 * 512, [[1, w_]], np_=64),
                                   r=[QI, KIN], w=[A0])
                                rl = rlR.next()
                                act(rl[:, 0:w_], A0[:, 0:w_], AF.Relu, r=[A0], w=[rl])
                                wcol = wis.ap(tt_ * 8 + h, [[1, 1]])
                                if h == 0:
                                    ts(isc[:, c4 * 512:c4 * 512 + w_], rl[:, 0:w_], wcol, ALU.mult, r=[rl, wis], w=[isc])
                                else:
                                    stt(isc[:, c4 * 512:c4 * 512 + w_], rl[:, 0:w_], wcol, isc[:, c4 * 512:c4 * 512 + w_],
                                        ALU.mult, ALU.add, r=[rl, wis, isc], w=[isc])
                                yield
                        if qb >= 2:
                            S.op("dve", lambda e, SL=SL, bs=bs, isc=isc: e.tensor_reduce(out=bs[:, 1:2], in_=isc[:, 0:SL], axis=AX.X, op=ALU.max), [isc], [bs])
                            S.op("dve", lambda e, SL=SL, bs=bs, isc=isc: e.tensor_reduce(out=bs[:, 0:1], in_=isc[:, 0:SL], axis=AX.X, op=ALU.min), [isc], [bs])
                            ts(bs[:, 1:2], bs[:, 1:2], 1.0, ALU.add, r=[bs], w=[bs])
                        tt(isc[:, SL - 128:SL], isc[:, SL - 128:SL], pk[:, NEGM:NEGM + 128], ALU.add, r=[isc, pk], w=[isc])
                        yield
                        if qb >= 2:
                            for it in range(NIT):
                                ts(bs[:, 2:3], bs[:, 0:1], bs[:, 1:2], ALU.add, r=[bs], w=[bs], s2=0.5, op1=ALU.mult)
                                ts(maskb[:, 0:SL], isc[:, 0:SL], bs[:, 2:3], ALU.is_ge, r=[isc, bs], w=[maskb, bs],
                                   s2=None, op1=ALU.add, accum=bs[:, 3:4])
                                yield
                                ts(bu[:, 0:1], bs[:, 3:4], TOPK - 0.5, ALU.is_ge, r=[bs], w=[bu])
                                ts(bu[:, 1:2], bs[:, 3:4], TOPK - 0.5, ALU.is_lt, r=[bs], w=[bu])
                                S.op("dve", lambda e, bs=bs, bu=bu: e.copy_predicated(out=bs[:, 0:1], mask=bu[:, 0:1], data=bs[:, 2:3]), [bs, bu], [bs])
                                S.op("dve", lambda e, bs=bs, bu=bu: e.copy_predicated(out=bs[:, 1:2], mask=bu[:, 1:2], data=bs[:, 2:3]), [bs, bu], [bs])
                                yield
                            thr = bs[:, 0:1]
                        else:
                            thr = cc[:, 2:3]
                        ts(maskb[:, 0:SL], isc[:, 0:SL], thr, ALU.is_ge, r=[isc, bs, cc], w=[maskb])
                        if seq == 0 and stl == 0 and tt_ == 3:
                            dump("isc", isc, isc[:, 0:512], [128, 512])
                            dump("bs", bs, bs[:], [128, 4])
                        for k0 in range(0, qb + 1, 4):
                            nk = min(4, qb + 1 - k0)
                            for kb in range(k0, k0 + nk):
                                tr(ptr.ap((kb - k0) * 128, [[1, 128]]), maskb[:, kb * 128:(kb + 1) * 128], r=[maskb], w=[ptrA_d])
                            acopy(maskT.ap(k0 * 128, [[1, nk * 128]]), ptr.ap(0, [[1, nk * 128]]), r=[ptrA_d], w=[maskT])
                            yield

                    def stageAT(tt_):
                        qb = stl * 4 + tt_
                        maskT = maskTP[tt_ % 2]
                        steps = [(hg, kb) for hg in range(4) for kb in range(qb + 1)]
                        Ps = {}

                        def front(i):
                            hg, kb = steps[i]
                            L = LR.next()
                            mm(L[:, :], KA.ap(kb * 128, [[1, 128]], np_=69),
                               QA.ap(hg * 4 * 512 + tt_ * 128, [[512, 4], [1, 128]], np_=69), r=[KA, QA], w=[L])
                            E = ER.next()
                            if kb == qb:
                                tt(Lm.ap(0, [[128, 4], [1, 128]]), L.ap(0, [[128, 4], [1, 128]]),
                                   pk.ap(NEGT, [[0, 4], [1, 128]]), ALU.add, r=[L, pk], w=[Lm])
                                act(E[:], Lm[:], AF.Exp, r=[Lm], w=[E])
                            else:
                                act(E[:], L[:, :], AF.Exp, r=[L], w=[E])
                            P = PR.next()
                            tt(P.ap(0, [[128, 4], [1, 128]]), E.ap(0, [[128, 4], [1, 128]]),
                               maskT.ap(kb * 128, [[0, 4], [1, 128]]), ALU.mult, r=[E, maskT], w=[P],
                               eng=("pool" if i % 2 == 0 else "dve"))
                            Ps[i] = P

                        def back(i):
                            hg, kb = steps[i]
                            P = Ps.pop(i)
                            mm(Ob[hg][0:66, :], VA.ap(kb * 66, [[1, 66]]), P[:, :], start=(kb == 0), stop=(kb == qb),
                               r=[VA, P], w=[Odeps[hg]])
                            if kb == qb:
                                acopy(OTs[0:66, :], Ob[hg][0:66, :], r=[Odeps[hg]], w=[OTs])
                                for j in range(4):
                                    S.op("pe", lambda e, j=j, hg=hg, OTs=OTs: e.transpose(out=Ob[hg][:, j * 66:(j + 1) * 66],
                                                                                         in_=OTs[0:66, j * 128:(j + 1) * 128],
                                                                                         identity=pk[0:66, IDENT:IDENT + 66]),
                                         [OTs, pk], [Odeps[hg]])
                                for j in range(4):
                                    h = hg * 4 + j
                                    recip(rc[:, j:j + 1], Ob[hg][:, j * 66 + 64:j * 66 + 65], r=[Odeps[hg]], w=[rc])
                                    stt(og[:, h * 64:(h + 1) * 64], Ob[hg][:, j * 66:j * 66 + 64], rc[:, j:j + 1],
                                        zA.ap(tt_ * 1024 + h * 64, [[1, 64]]), ALU.mult, ALU.mult, r=[Odeps[hg], rc, zA], w=[og])

                        front(0)
                        for i in range(len(steps)):
                            if i + 1 < len(steps):
                                front(i + 1)
                            back(i)
                            yield
                        for kc in range(8):
                            tr(ptr.ap(512 + (kc % 4) * 128, [[1, 128]]), og[:, kc * 128:(kc + 1) * 128], r=[og], w=[ptrB_d])
                            if kc % 4 == 3:
                                acopy(oT.ap((kc - 3) * 512 + tt_ * 128, [[512, 4], [1, 128]]), ptr.ap(512, [[128, 4], [1, 128]]),
                                      r=[ptrB_d], w=[oT])
                        yield

                    def stageProj():
                        for half in range(2):
                            wb = load_w(win_v, C_Q + half * 512, 512)
                            for hh in range(8):
                                h = half * 8 + hh
                                for kc in range(8):
                                    mm(A1[0:64, :], wslice(wb, 512, kc, hh * 64, (hh + 1) * 64), hT.ap(kc * 512, [[1, 512]]),
                                       start=(kc == 0), stop=(kc == 7), r=[wb, hT], w=[A1])
                                S.op("act", lambda e, h=h, QA=QA, A1=A1: e.mul(QA.ap(h * 512, [[1, 512]], np_=64), A1[0:64, :], 0.125), [A1], [QA])
                                yield
                        for half in range(2):
                            wb = load_w(win_v, C_AZ + half * 512, 512)
                            for tt_ in range(4):
                                for kc in range(8):
                                    mm(A1[:, :], hT.ap(kc * 512 + tt_ * 128, [[1, 128]]), wslice(wb, 512, kc, 0, 512),
                                       start=(kc == 0), stop=(kc == 7), r=[hT, wb], w=[A1])
                                act(thA[:], A1[:, :], AF.Tanh, r=[A1], w=[thA], scale=0.5)
                                stt(zA.ap(tt_ * 1024 + half * 512, [[1, 512]]), thA[:], 1.0, A1[:, :], ALU.add, ALU.mult,
                                    r=[thA, A1], w=[zA])
                                yield

                    interleave(stageI(0), stageProj())
                    if seq == 0 and stl == 0:
                        dump("QA", QA, QA[:], [128, 16, 512])
                        dump("QI", QI, QI[:], [128, 8, 512])
                    for tt_ in range(4):
                        interleave(stageAT(tt_), stageI(tt_ + 1) if tt_ < 3 else iter(()))
                    if seq == 0 and stl == 0:
                        dump("oT", oT, oT[:], [128, 8, 512])
                    for half in range(2):
                        wb = load_w(wao_v, half * 512, 512)
                        for tt_ in range(4):
                            acc = accR.next()
                            for kc in range(8):
                                mm(acc[:, :], oT.ap(kc * 512 + tt_ * 128, [[1, 128]]), wslice(wb, 512, kc, 0, 512),
                                   start=(kc == 0), stop=(kc == 7), r=[oT, wb], w=[acc])
                            acopy(yattn.ap(tt_ * 1024 + half * 512, [[1, 512]]), acc[:, :], r=[acc], w=[yattn])
                if seq == 0 and stl == 0:
                    dump("yattn", yattn, yattn[:], [128, 4, 1024])

                S.barrier()
                with ExitStack() as ph:
                    gtR = Ring([T("gt%d" % i, [128, 512], F32, stack=ph) for i in range(2)])
                    gsR = Ring([T("gs%d" % i, [128, 512], F32, stack=ph) for i in range(2)])
                    mg = T("mg", [128, 4, 1024], F32, stack=ph)
                    mb = T("mb", [128, 1024], BF16, stack=ph)
                    mT = T("mT", [128, 8, 512], BF16, stack=ph)
                    r4 = T("r4", [128, 4, 1024], F32, stack=ph)
                    roR = Ring([T("ro%d" % i, [128, 1024], F32, stack=ph) for i in range(2)])
                    p5 = T("p5", [128, NPB - NRES], F32, stack=ph)
                    dma("sp", p5[:], pb_d[:, NRES:NPB], w=[p5])
                    for u in range(4):
                        wb = load_w(win_v, C_GATE + u * 512, 512)
                        for tt_ in range(4):
                            acc = accR.next()
                            for kc in range(8):
                                mm(acc[:, :], hT.ap(kc * 512 + tt_ * 128, [[1, 128]]), wslice(wb, 512, kc, 0, 512),
                                   start=(kc == 0), stop=(kc == 7), r=[hT, wb], w=[acc])
                            gtt = gtR.next()
                            tt(gtt[:], acc[:, :], p5[:, u * 512:(u + 1) * 512], ALU.add, r=[acc, p5], w=[gtt])
                            gs = gsR.next()
                            act(gs[:], gtt[:], AF.Tanh, r=[gtt], w=[gs], scale=0.5)
                            if u < 2:
                                stt(mg.ap(tt_ * 1024 + u * 512, [[1, 512]]), gs[:], 1.0, yssm.ap(tt_ * 1024 + u * 512, [[1, 512]]),
                                    ALU.add, ALU.mult, r=[gs, yssm], w=[mg])
                            else:
                                stt(gs[:], gs[:], 1.0, yattn.ap(tt_ * 1024 + (u - 2) * 512, [[1, 512]]), ALU.add, ALU.mult,
                                    r=[gs, yattn], w=[gs])
                                tt(mg.ap(tt_ * 1024 + (u - 2) * 512, [[1, 512]]), mg.ap(tt_ * 1024 + (u - 2) * 512, [[1, 512]]),
                                   gs[:], ALU.add, r=[mg, gs], w=[mg])
                    for tt_ in range(4):
                        S.op("act", lambda e, tt_=tt_, mb=mb, mg=mg: e.mul(mb[:], mg.ap(tt_ * 1024, [[1, 1024]]), 0.5), [mg], [mb])
                        for kc in range(8):
                            tr(ptr.ap(kc * 128, [[1, 128]]), mb[:, kc * 128:(kc + 1) * 128], r=[mb], w=[ptr])
                        acopy(mT.ap(tt_ * 128, [[512, 8], [1, 128]]), ptr.ap(0, [[128, 8], [1, 128]]), r=[ptr], w=[mT])
                    for half in range(2):
                        wb = load_w(wout_v, half * 512, 512)
                        for tt_ in range(4):
                            acc = accR.next()
                            for kc in range(8):
                                mm(acc[:, :], mT.ap(kc * 512 + tt_ * 128, [[1, 128]]), wslice(wb, 512, kc, 0, 512),
                                   start=(kc == 0), stop=(kc == 7), r=[mT, wb], w=[acc])
                            acopy(r4.ap(tt_ * 1024 + half * 512, [[1, 512]]), acc[:, :], r=[acc], w=[r4])
                    for tt_ in range(4):
                        xt = xring.next()
                        dma("sp", xt[:], x_d[seq, t0 + tt_ * 128:t0 + (tt_ + 1) * 128, :], w=[xt])
                        r4s = r4.ap(tt_ * 1024, [[1, 1024]])
                        tt(r4s, r4s, xt[:], ALU.add, r=[r4, xt], w=[r4])
                        act(mb[:], r4s, AF.Square, r=[r4], w=[mb, sm], accum=sm[:, 0:1])
                        rstd_col(sm[:, 2:3], sm[:, 0:1], 1.0 / D_MODEL, cc[:, 0:1], sm)
                        ro = roR.next()
                        stt(ro[:], r4s, sm[:, 2:3], p5[:, FNW - NRES:FNW - NRES + 1024], ALU.mult, ALU.mult, r=[r4, sm, p5], w=[ro])
                        dma("sp", out_d[seq, t0 + tt_ * 128:t0 + (tt_ + 1) * 128, :], ro[:], r=[ro], w=[ro])
                S.barrier()
        S.final_wait("sp")
        S.emit()
    return nc, dbg_outs


def host_prep(inputs):
    f32 = np.float32
    w_in = np.asarray(inputs["w_in"], f32)[0]
    O_Z, O_XBC, O_DT, O_Q, O_K, O_V, O_AZ, O_QI, O_KI, O_WI, O_G = 0, 2048, 6144, 6176, 7200, 7264, 7328, 8352, 8864, 8928, 8936
    cols = []
    cols += list(range(O_DT, O_DT + 32)) + list(range(O_K, O_K + 64)) + list(range(O_V, O_V + 64))
    cols += list(range(O_KI, O_KI + 64)) + list(range(O_WI, O_WI + 8))
    for g in range(8):
        cols += list(range(O_Z + g * 256, O_Z + (g + 1) * 256))
        cols += list(range(O_XBC + g * 256, O_XBC + (g + 1) * 256))
        cols += list(range(O_XBC + 2048 + g * 128, O_XBC + 2048 + (g + 1) * 128))
        cols += list(range(O_XBC + 3072 + g * 128, O_XBC + 3072 + (g + 1) * 128))
    cols += list(range(O_Q, O_Q + 1024)) + list(range(O_QI, O_QI + 512)) + list(range(O_AZ, O_AZ + 1024))
    cols += list(range(O_G, O_G + 2048))
    cols = np.asarray(cols)
    assert cols.shape[0] == IN_TOTAL and np.unique(cols).shape[0] == IN_TOTAL
    win = np.ascontiguousarray(w_in[:, cols])

    pb = np.zeros((128, NPB), f32)

    def put(off, v):
        v = np.asarray(v, f32).reshape(-1)
        pb[:, off:off + v.shape[0]] = v[None, :]
    put(NW, inputs["norm_w"][0])
    put(FNW, inputs["final_norm_w"])
    put(GB, inputs["gate_b"][0])
    put(SNW, inputs["ssm_norm_w"][0])
    put(DTB, inputs["dt_bias"][0])
    put(ALOG, inputs["a_log"][0])
    put(DSK, inputs["d_skip"][0])
    put(KIW, inputs["idx_k_norm_w"][0])
    put(KIB, inputs["idx_k_norm_b"][0])

    conv_w = np.asarray(inputs["conv_w"], f32)[0]
    conv_b = np.asarray(inputs["conv_b"], f32)[0]
    pc = np.zeros((128, 160), f32)
    for g in range(8):
        for fi in range(4):
            ct = g * 4 + fi
            if fi < 2:
                ch0 = g * 256 + fi * 128
            elif fi == 2:
                ch0 = 2048 + g * 128
            else:
                ch0 = 3072 + g * 128
            pc[:, ct * 4:(ct + 1) * 4] = conv_w[:, ch0:ch0 + 128].T
            pc[:, 128 + ct] = conv_b[ch0:ch0 + 128]

    pk = np.zeros((128, NPK), f32)
    i = np.arange(128)
    pk[:, IDENT:IDENT + 128] = np.eye(128, dtype=f32)
    pk[:, TRI:TRI + 128] = (i[:, None] <= i[None, :]).astype(f32)
    pk[:, USTR:USTR + 128] = (i[:, None] > i[None, :]).astype(f32)
    pk[:, NEGM:NEGM + 128] = np.where(i[None, :] > i[:, None], f32(-1e30), f32(0))
    pk[:, ONES:ONES + 128] = 1.0
    pk[:, NEGT:NEGT + 128] = np.where(i[:, None] > i[None, :], f32(-1e30), f32(0))

    bf = ml_dtypes.bfloat16
    slopes = np.exp2(-8.0 * np.arange(1, 17, dtype=np.float64) / 16).astype(f32)
    s_hi = slopes.astype(bf)
    s_lo = (slopes - s_hi.astype(f32)).astype(bf)
    spos = np.arange(SEQ)
    kaug = np.zeros((5, SEQ), f32)
    kaug[0] = 1.0
    kaug[1] = spos % 128
    kaug[2] = (spos // 128) * 128
    kaug[3] = spos % 128
    kaug[4] = (spos // 128) * 128
    kaug = kaug.astype(bf)
    qaug = np.zeros((5, 16, SEQ), f32)
    qaug[0] = -(slopes[:, None].astype(np.float64) * spos[None, :]).astype(f32)
    qaug[1] = s_hi.astype(f32)[:, None]
    qaug[2] = s_hi.astype(f32)[:, None]
    qaug[3] = s_lo.astype(f32)[:, None]
    qaug[4] = s_lo.astype(f32)[:, None]
    qaug = qaug.astype(bf)
    shared = {
        "win": win,
        "wso": np.ascontiguousarray(np.asarray(inputs["w_ssm_out"], f32)[0]),
        "wao": np.ascontiguousarray(np.asarray(inputs["w_attn_out"], f32)[0]),
        "wout": np.ascontiguousarray(np.asarray(inputs["w_out"], f32)[0]),
        "pb": pb, "pc": pc, "pk": pk, "kaug": kaug, "qaug": qaug,
    }
    return shared


_CACHE = {}


def kernel(**inputs):
    x = np.asarray(inputs["x"], np.float32)
    shared = host_prep(inputs)
    if "nc" not in _CACHE:
        _CACHE["nc"] = build_program(2)[0]
    nc = _CACHE["nc"]
    in_maps = []
    for c in range(8):
        m = dict(shared)
        m["x"] = np.ascontiguousarray(x[2 * c:2 * c + 2])
        in_maps.append(m)
    res = run_bass_kernel_spmd(nc, in_maps, core_ids=list(range(8)))
    out = np.concatenate([np.asarray(r["out"], np.float32) for r in res.results], axis=0)
    return out
```

```python
import numpy as np
import ml_dtypes
from contextlib import ExitStack
import concourse.bass as bass
import concourse.mybir as mybir
from concourse.bass_utils import run_bass_kernel_spmd

F32 = mybir.dt.float32
BF16 = mybir.dt.bfloat16
U32 = mybir.dt.uint32
AF = mybir.ActivationFunctionType
ALU = mybir.AluOpType
AX = mybir.AxisListType

D_MODEL = 1024
SEQ = 2048
IN_TOTAL = 10984
NIT = 13
TOPK = 256
EPS = 1e-6
IDX_SCALE = (8 ** -0.5) * (64 ** -0.5)

NW, SNW, DTB, ALOG, DSK, KIW, KIB, NRES, GB, FNW, NPB = 0, 1024, 3072, 3104, 3136, 3168, 3232, 3296, 3296, 5344, 6368
IDENT, TRI, USTR, NEGM, ONES, NEGT, NPK = 0, 128, 256, 384, 512, 640, 768
C_SMALL, C_GRP, C_Q, C_QI, C_AZ, C_GATE = 0, 232, 6376, 7400, 7912, 8936


class Dep:
    __slots__ = ("name", "w", "r")

    def __init__(self, name=""):
        self.name = name
        self.w = None
        self.r = {}


class Sched:
    ENGS = ("pe", "act", "dve", "pool", "sp")

    def __init__(self, nc, stack, n_dma_sems=24):
        self.nc = nc
        self.lists = {e: [] for e in self.ENGS}
        self.sems = {}
        self.cnt = {}
        for e in ("pe", "act", "dve", "pool"):
            self.sems[e] = stack.enter_context(nc.semaphore("s_" + e))
            self.cnt[e] = 0
        self.dma_pool = {}
        for q, n in (("sp", n_dma_sems), ("pool", 8)):
            keys = []
            for i in range(n):
                k = "d_%s_%d" % (q, i)
                self.sems[k] = stack.enter_context(nc.semaphore(k))
                self.cnt[k] = 0
                keys.append(k)
            self.dma_pool[q] = [keys, 0]
        self.seen = {e: {} for e in self.ENGS}
        self.n_ops = 0

    def _needs(self, eng, reads, writes):
        needs = {}

        def add(k, v):
            if v > needs.get(k, 0):
                needs[k] = v
        for d in reads:
            if d.w is not None and not (eng == "pe" and d.w[0] == "pe"):
                add(*d.w)
        for d in writes:
            if d.w is not None and not (eng == "pe" and d.w[0] == "pe"):
                add(*d.w)
            for k, v in d.r.items():
                if not (eng == "pe" and k == "pe"):
                    add(k, v)
        out = []
        seen = self.seen[eng]
        for k, v in needs.items():
            if seen.get(k, 0) >= v:
                continue
            seen[k] = v
            out.append((k, v))
        return out

    def op(self, eng, fn, reads=(), writes=()):
        reads = [getattr(d, "dep", d) for d in reads]
        writes = [getattr(d, "dep", d) for d in writes]
        waits = self._needs(eng, reads, writes)
        self.cnt[eng] += 1
        v = self.cnt[eng]
        self.lists[eng].append((waits, fn, eng, 1))
        for d in reads:
            d.r[eng] = v
        for d in writes:
            d.w = (eng, v)
            d.r = {}
        self.n_ops += 1

    def dma(self, q, fn, reads=(), writes=()):
        reads = [getattr(d, "dep", d) for d in reads]
        writes = [getattr(d, "dep", d) for d in writes]
        keys, idx = self.dma_pool[q]
        k = keys[idx % len(keys)]
        self.dma_pool[q][1] = idx + 1
        waits = self._needs(q, reads, writes)
        prev = self.cnt[k]
        if prev > 0 and self.seen[q].get(k, 0) < prev:
            self.seen[q][k] = prev
            waits.append((k, prev))
        self.cnt[k] += 16
        v = self.cnt[k]
        self.lists[q].append((waits, fn, k, 16))
        for d in reads:
            d.r[k] = v
        for d in writes:
            d.w = (k, v)
            d.r = {}
        self.n_ops += 1

    def barrier(self, engs=("pe", "act", "dve", "sp")):
        for e in engs:
            waits = []
            for k, v in self.cnt.items():
                if k == e or k.startswith("d_pool") or v == 0:
                    continue
                if self.seen[e].get(k, 0) < v:
                    self.seen[e][k] = v
                    waits.append((k, v))
            if waits:
                self.lists[e].append((waits, None, None, 0))

    def final_wait(self, eng):
        waits = []
        for k, v in self.cnt.items():
            if k == eng or v == 0:
                continue
            if self.seen[eng].get(k, 0) < v:
                self.seen[eng][k] = v
                waits.append((k, v))
        self.lists[eng].append((waits, None, None, 0))

    def emit(self):
        nc = self.nc
        sems = self.sems
        lists = self.lists

        def replay(e, lst):
            for waits, fn, k, inc in lst:
                for (wk, wv) in waits:
                    e.wait_ge(sems[wk], wv)
                if fn is not None:
                    fn(e).then_inc(sems[k], inc)

        with nc.Block() as block:
            @block.tensor
            def _(e):
                replay(e, lists["pe"])

            @block.scalar
            def _(e):
                replay(e, lists["act"])

            @block.vector
            def _(e):
                replay(e, lists["dve"])

            @block.gpsimd
            def _(e):
                replay(e, lists["pool"])

            @block.sync
            def _(e):
                replay(e, lists["sp"])


def build_program(n_seq=2, dbg=None):
    nc = bass.Bass("TRN2", target_bir_lowering=False)

    def dram(name, shape, dt=F32, kind="ExternalInput"):
        return nc.dram_tensor(name, shape, dt, kind=kind).ap()

    x_d = dram("x", [n_seq, SEQ, D_MODEL])
    win_d = dram("win", [D_MODEL, IN_TOTAL])
    wso_d = dram("wso", [2048, 1024])
    wao_d = dram("wao", [1024, 1024])
    wout_d = dram("wout", [1024, 1024])
    pb_d = dram("pb", [128, NPB])
    pc_d = dram("pc", [128, 160])
    pk_d = dram("pk", [128, NPK])
    kaug_d = dram("kaug", [5, SEQ], BF16)
    qaug_d = dram("qaug", [5, 16, SEQ], BF16)
    out_d = dram("out", [n_seq, SEQ, D_MODEL], kind="ExternalOutput")
    dbg_outs = {}

    win_v = win_d.rearrange("(kc p) n -> p kc n", p=128)
    wso_v = wso_d.rearrange("(kc p) n -> p kc n", p=128)
    wao_v = wao_d.rearrange("(kc p) n -> p kc n", p=128)
    wout_v = wout_d.rearrange("(kc p) n -> p kc n", p=128)

    with ExitStack() as st0:
        S = Sched(nc, st0)
        uid = [0]

        class T:
            def __init__(self, name, shape, dt, psum=False, stack=st0):
                uid[0] += 1
                nm = "%s_%d" % (name, uid[0])
                alloc = nc.psum_tensor if psum else nc.sbuf_tensor
                self.t = stack.enter_context(alloc(nm, list(shape), dt))
                self.dep = Dep(nm)
                self.row = int(np.prod(shape[1:]))

            def __getitem__(self, k):
                return self.t[k]

            def ap(self, col0, dims, p0=0, np_=128):
                return bass.AP(self.t, p0 * self.row + col0, [[self.row, np_]] + [list(d) for d in dims])

        class Ring:
            def __init__(self, tiles):
                self.tiles = tiles
                self.i = 0

            def next(self):
                t = self.tiles[self.i % len(self.tiles)]
                self.i += 1
                return t

        def mm(out, lhsT, rhs, start=True, stop=True, r=(), w=()):
            S.op("pe", lambda e: e.matmul(out, lhsT=lhsT, rhs=rhs, start=start, stop=stop), r, w)

        def tr(out, in_, r=(), w=()):
            S.op("pe", lambda e: e.transpose(out=out, in_=in_, identity=identb[:]), list(r) + [identb], w)

        def act(out, in_, func, r=(), w=(), bias=None, scale=None, accum=None):
            kw = {}
            if bias is not None:
                kw["bias"] = bias
            if scale is not None:
                kw["scale"] = scale
            if accum is not None:
                kw["accum_out"] = accum
            S.op("act", lambda e: e.activation(out=out, in_=in_, func=func, **kw), r, w)

        def acopy(out, in_, r=(), w=()):
            S.op("act", lambda e: e.copy(out=out, in_=in_), r, w)

        def tt(out, a, b, op, r=(), w=(), eng="dve"):
            S.op(eng, lambda e: e.tensor_tensor(out=out, in0=a, in1=b, op=op), r, w)

        def ts(out, a, s1, op0, r=(), w=(), s2=None, op1=None, accum=None):
            kw = {}
            if op1 is not None:
                kw["op1"] = op1
            if accum is not None:
                kw["accum_out"] = accum
            S.op("dve", lambda e: e.tensor_scalar(out=out, in0=a, scalar1=s1, scalar2=s2, op0=op0, **kw), r, w)

        def stt(out, a, s, b, op0, op1, r=(), w=()):
            S.op("dve", lambda e: e.scalar_tensor_tensor(out=out, in0=a, scalar=s, in1=b, op0=op0, op1=op1), r, w)

        def vcopy(out, in_, r=(), w=()):
            S.op("dve", lambda e: e.tensor_copy(out=out, in_=in_), r, w)

        def memset(ap, val, w=()):
            S.op("dve", lambda e: e.memset(ap, val), (), w)

        def recip(out, in_, r=(), w=()):
            S.op("dve", lambda e: e.reciprocal(out=out, in_=in_), r, w)

        def dma(q, out, in_, r=(), w=()):
            S.dma(q, lambda e: e.dma_start(out=out, in_=in_), r, w)

        def rstd_col(out_col, ssq_col, scale, eps_col, tile):
            act(ssq_col, ssq_col, AF.Sqrt, r=[tile, cc], w=[tile], bias=eps_col, scale=scale)
            recip(out_col, ssq_col, r=[tile], w=[tile])

        def interleave(*gens):
            alive = list(gens)
            while alive:
                for g_ in list(alive):
                    try:
                        next(g_)
                    except StopIteration:
                        alive.remove(g_)

        def dump(name, tile, ap, shape):
            if dbg is None or name not in dbg:
                return
            d = nc.dram_tensor("dbg_" + name, list(shape), tile.t.dtype, kind="ExternalOutput").ap()
            dbg_outs[name] = "dbg_" + name
            dma("sp", d, ap, r=[tile], w=[])

        pb = T("pb", [128, NRES], F32)
        pc = T("pc", [128, 160], F32)
        pk = T("pk", [128, NPK], F32)
        identb = T("identb", [128, 128], BF16)
        cc = T("cc", [128, 8], F32)
        Abc = T("Abc", [128, 32], F32)
        Wsm = T("Wsm", [128, 8, 232], BF16)
        KA = T("KA", [128, SEQ], BF16)
        KIN = T("KIN", [128, SEQ], BF16)
        VA = T("VA", [128, 16, 66], BF16)
        St = T("St", [128, 8, 256], F32)
        Sb = T("Sb", [128, 8, 256], BF16)
        St_deps = [Dep("St%d" % g) for g in range(8)]
        Sb_deps = [Dep("Sb%d" % g) for g in range(8)]
        halo = T("halo", [128, 32, 3], F32)
        halo_deps = [Dep("halo%d" % g) for g in range(8)]
        xring = Ring([T("xt%d" % i, [128, 1024], F32) for i in range(2)])
        hb = T("hb", [128, 1024], BF16)
        hT = T("hT", [128, 8, 512], BF16)
        sm = T("sm", [128, 16], F32)
        dt4 = T("dt4", [128, 4, 32], F32)
        a4 = T("a4", [128, 4, 32], F32)
        eacs4 = T("eacs4", [128, 4, 32], F32)
        cdb4 = T("cdb4", [128, 4, 32], F32)
        dtd4 = T("dtd4", [128, 4, 32], F32)
        wis = T("wis", [128, 4, 8], F32)
        s32 = [T("s32_%d" % i, [128, 32], F32) for i in range(4)]
        kvb = T("kvb", [128, 128], BF16)
        kif = T("kif", [128, 64], F32)
        wring = Ring([T("wb%d" % i, [128, 6144], BF16) for i in range(3)])
        yssm = T("yssm", [128, 4, 1024], BF16)
        yattn = T("yattn", [128, 4, 1024], BF16)

        accR = Ring([T("acc%d" % i, [128, 512], F32, psum=True) for i in range(2)])
        ptr = T("ptr", [128, 1024], BF16, psum=True)
        bk3 = T("bk3", [128, 512], F32, psum=True)
        bk4 = T("bk4", [128, 512], F32, psum=True)
        bk5 = T("bk5", [128, 512], F32, psum=True)
        bk6 = T("bk6", [128, 512], F32, psum=True)
        bk7 = T("bk7", [128, 512], F32, psum=True)
        cb_dep = acs_dep = bk4.dep
        sts_dep = yoff_dep = bk6.dep

        dma("sp", pb[:], pb_d[:, 0:NRES], w=[pb])
        dma("sp", pc[:], pc_d, w=[pc])
        dma("sp", pk[:], pk_d, w=[pk])
        vcopy(identb[:], pk[:, IDENT:IDENT + 128], r=[pk], w=[identb])
        memset(cc[:, 0:1], EPS, w=[cc])
        memset(cc[:, 1:2], 1.0, w=[cc])
        memset(cc[:, 2:3], -1e29, w=[cc])
        memset(cc[:, 3:4], 4.0 * EPS, w=[cc])
        ts(pc[:], pc[:], 0.5, ALU.mult, r=[pc], w=[pc])
        act(Abc[:], pb[:, ALOG:ALOG + 32], AF.Exp, r=[pb], w=[Abc])
        ts(Abc[:], Abc[:], -1.0, ALU.mult, r=[Abc], w=[Abc])
        memset(KA[:], 0.0, w=[KA])
        dma("sp", KA.ap(0, [[1, SEQ]], p0=64, np_=5), kaug_d, w=[KA])
        memset(VA[:], 2.0, w=[VA])
        dma("pool", Wsm[:], win_v[:, :, C_SMALL:C_SMALL + 232], w=[Wsm])

        def load_w(src_v, c0, n, nkc=8):
            wb = wring.next()
            dma("pool", wb.ap(0, [[n, nkc], [1, n]]), src_v[:, :, c0:c0 + n], w=[wb])
            return wb

        def wslice(wb, n, kc, a, b):
            return wb.ap(kc * n + a, [[1, b - a]])

        for seq in range(n_seq):
            memset(St[:], 0.0, w=St_deps)
            memset(Sb[:], 0.0, w=Sb_deps)
            memset(halo[:], 0.0, w=halo_deps)
            for stl in range(4):
                t0 = stl * 512
                pre_w = [load_w(win_v, C_GRP, 768)]
                for tt_ in range(4):
                    xt = xring.next()
                    dma("sp", xt[:], x_d[seq, t0 + tt_ * 128:t0 + (tt_ + 1) * 128, :], w=[xt])
                    act(hb[:], xt[:], AF.Square, r=[xt], w=[hb, sm], accum=sm[:, 0:1])
                    rstd_col(sm[:, 2:3], sm[:, 0:1], 1.0 / D_MODEL, cc[:, 0:1], sm)
                    stt(hb[:], xt[:], sm[:, 2:3], pb[:, NW:NW + 1024], ALU.mult, ALU.mult, r=[xt, sm, pb], w=[hb])
                    for kc in range(8):
                        tr(ptr.ap(kc * 128, [[1, 128]]), hb[:, kc * 128:(kc + 1) * 128], r=[hb], w=[ptr])
                    acopy(hT.ap(tt_ * 128, [[512, 8], [1, 128]]), ptr.ap(0, [[128, 8], [1, 128]]), r=[ptr], w=[hT])
                if seq == 0 and stl == 0:
                    dump("hT", hT, hT[:], [128, 8, 512])

                for tt_ in range(4):
                    gt = stl * 4 + tt_
                    acc = accR.next()
                    for kc in range(8):
                        mm(acc[:, 0:232], hT.ap(kc * 512 + tt_ * 128, [[1, 128]]), Wsm.ap(kc * 232, [[1, 232]]),
                           start=(kc == 0), stop=(kc == 7), r=[hT, Wsm], w=[acc])
                    x32, e32, acs_sb, dd = s32
                    tt(x32[:], acc[:, 0:32], pb[:, DTB:DTB + 32], ALU.add, r=[acc, pb], w=[x32])
                    act(e32[:], x32[:], AF.Exp, r=[x32], w=[e32])
                    act(dt4.ap(tt_ * 32, [[1, 32]]), e32[:], AF.Ln, r=[e32, cc], w=[dt4], bias=cc[:, 1:2], scale=1.0)
                    tt(a4.ap(tt_ * 32, [[1, 32]]), dt4.ap(tt_ * 32, [[1, 32]]), Abc[:], ALU.mult, r=[dt4, Abc], w=[a4])
                    acopy(kvb[:, 0:64], acc[:, 32:96], r=[acc], w=[kvb])
                    acopy(VA.ap(gt * 66, [[1, 64]]), acc[:, 96:160], r=[acc], w=[VA])
                    S.op("dve", lambda e, acc=acc: e.bn_stats(out=sm[:, 4:10], in_=acc[:, 160:224]), [acc], [sm])
                    S.op("dve", lambda e: e.bn_aggr(out=sm[:, 10:12], in_=sm[:, 4:10]), [sm], [sm])
                    rstd_col(sm[:, 13:14], sm[:, 11:12], 1.0, cc[:, 0:1], sm)
                    ts(kif[:], acc[:, 160:224], sm[:, 10:11], ALU.subtract, r=[acc, sm], w=[kif], s2=sm[:, 13:14], op1=ALU.mult)
                    tt(kif[:], kif[:], pb[:, KIW:KIW + 64], ALU.mult, r=[kif, pb], w=[kif])
                    tt(kvb[:, 64:128], kif[:], pb[:, KIB:KIB + 64], ALU.add, r=[kif, pb], w=[kvb])
                    ts(wis.ap(tt_ * 8, [[1, 8]]), acc[:, 224:232], IDX_SCALE, ALU.mult, r=[acc], w=[wis])
                    tr(ptr.ap(0, [[1, 128]], np_=64), kvb[:, 0:64], r=[kvb], w=[ptr])
                    tr(ptr.ap(128, [[1, 128]], np_=64), kvb[:, 64:128], r=[kvb], w=[ptr])
                    acopy(KA.ap(gt * 128, [[1, 128]], np_=64), ptr.ap(0, [[1, 128]], np_=64), r=[ptr], w=[KA])
                    acopy(KIN.ap(gt * 128, [[1, 128]], np_=64), ptr.ap(128, [[1, 128]], np_=64), r=[ptr], w=[KIN])
                    mm(bk4[:, 128:160], pk[:, TRI:TRI + 128], a4.ap(tt_ * 32, [[1, 32]]), r=[pk, a4], w=[acs_dep])
                    mm(bk4[:, 160:192], pk[:, ONES:ONES + 128], a4.ap(tt_ * 32, [[1, 32]]), r=[pk, a4], w=[acs_dep])
                    acopy(acs_sb[:], bk4[:, 128:160], r=[acs_dep], w=[acs_sb])
                    act(eacs4.ap(tt_ * 32, [[1, 32]]), bk4[:, 128:160], AF.Exp, r=[acs_dep], w=[eacs4])
                    act(cdb4.ap(tt_ * 32, [[1, 32]]), bk4[:, 160:192], AF.Exp, r=[acs_dep], w=[cdb4])
                    tt(dd[:], bk4[:, 160:192], acs_sb[:], ALU.subtract, r=[acs_dep, acs_sb], w=[dd])
                    act(dd[:], dd[:], AF.Exp, r=[dd], w=[dd])
                    tt(dtd4.ap(tt_ * 32, [[1, 32]]), dt4.ap(tt_ * 32, [[1, 32]]), dd[:], ALU.mult, r=[dt4, dd], w=[dtd4])
                if seq == 0 and stl == 0:
                    dump("dt4", dt4, dt4[:], [128, 4, 32])
                    dump("KA", KA, KA[:], [128, SEQ])
                    dump("KIN", KIN, KIN[:], [128, SEQ])
                    dump("VA", VA, VA[:], [128, 16, 66])
                    dump("wis", wis, wis[:], [128, 4, 8])
                    dump("eacs4", eacs4, eacs4[:], [128, 4, 32])
                    dump("dtd4", dtd4, dtd4[:], [128, 4, 32])

                S.barrier(("pe", "act", "dve", "sp", "pool"))
                with ExitStack() as ph:
                    Upre = T("Upre", [128, 4, 515], F32, stack=ph)
                    Upre_d = [Dep("Upre%d" % i) for i in range(4)]
                    cv = T("cv", [128, 4, 512], F32, stack=ph)
                    cv_d = [Dep("cv%d" % i) for i in range(4)]
                    thR = Ring([T("th%d" % i, [128, 512], F32, stack=ph) for i in range(2)])
                    xbP = [T("xb%d" % i, [128, 4, 512], BF16, stack=ph) for i in range(2)]
                    xb_d = [[Dep("xb%d_%d" % (i, f)) for f in range(4)] for i in range(2)]
                    zsP = [T("zs%d" % i, [128, 4, 256], F32, stack=ph) for i in range(2)]
                    xtkP = [T("xtk%d" % i, [128, 4, 384], BF16, stack=ph) for i in range(2)]
                    rhsAR = Ring([T("rhsA%d" % i, [128, 512], F32, stack=ph) for i in range(2)])
                    CBmR = Ring([T("CBm%d" % i, [128, 128], F32, stack=ph) for i in range(2)])
                    EsegR = Ring([T("Eseg%d" % i, [128, 512], BF16, stack=ph) for i in range(2)])
                    MTR = Ring([T("MT%d" % i, [128, 512], BF16, stack=ph) for i in range(2)])
                    xcR = Ring([T("xc%d" % i, [128, 256], BF16, stack=ph) for i in range(2)])
                    xcdR = Ring([T("xcd%d" % i, [128, 256], BF16, stack=ph) for i in range(2)])
                    xsDR = Ring([T("xsD%d" % i, [128, 256], F32, stack=ph) for i in range(2)])
                    t1R = Ring([T("t1%d" % i, [128, 256], F32, stack=ph) for i in range(2)])
                    yzR = Ring([T("yz%d" % i, [128, 256], F32, stack=ph) for i in range(4)])
                    smgP = [T("smg%d" % i, [128, 12], F32, stack=ph) for i in range(2)]
                    yzs = {}
                    yjk = T("yjk", [128, 256], BF16, stack=ph)
                    yNR = Ring([T("yN%d" % i, [128, 256], BF16, stack=ph) for i in range(2)])
                    yNT = T("yNT", [128, 16, 512], BF16, stack=ph)
                    A0 = accR.tiles[0]
                    segR = Ring([bk3, bk7])
                    cbR = Ring([(0, bk4.dep)])
                    ydR = Ring([(0, bk5.dep)])
                    soR = Ring([(bk6, bk6.dep, bk6.dep), (accR.tiles[1], accR.tiles[1].dep, accR.tiles[1].dep)])
                    ptrA_d, ptrB_d = ptr.dep, [ptr.dep, ptr.dep]
                    state_done = {}

                    def stageA(g):
                        par = g % 2
                        xb, zs, xtk, xbd = xbP[par], zsP[par], xtkP[par], xb_d[par]
                        wb = wq.pop(g)
                        vcopy(Upre.ap(0, [[515, 4], [1, 3]]), halo.ap(g * 12, [[3, 4], [1, 3]]), r=[halo_deps[g]], w=Upre_d)
                        for fi in range(4):
                            for kc in range(8):
                                mm(A0[:, :], wslice(wb, 768, kc, 256 + fi * 128, 256 + (fi + 1) * 128), hT.ap(kc * 512, [[1, 512]]),
                                   start=(kc == 0), stop=(kc == 7), r=[wb, hT], w=[A0])
                            acopy(Upre.ap(fi * 515 + 3, [[1, 512]]), A0[:, :], r=[A0], w=[Upre_d[fi]])
                            yield
                        vcopy(halo.ap(g * 12, [[3, 4], [1, 3]]), Upre.ap(512, [[515, 4], [1, 3]]), r=Upre_d, w=[halo_deps[g]])
                        for fi in range(4):
                            ct = g * 4 + fi
                            cvf = cv.ap(fi * 512, [[1, 512]])
                            act(cvf, Upre.ap(fi * 515, [[1, 512]]), AF.Identity, r=[Upre_d[fi], pc], w=[cv_d[fi]],
                                bias=pc[:, 128 + ct:129 + ct], scale=pc[:, ct * 4:ct * 4 + 1])
                            yield
                            for k in range(1, 4):
                                stt(cvf, Upre.ap(fi * 515 + k, [[1, 512]]), pc[:, ct * 4 + k:ct * 4 + k + 1], cvf, ALU.mult, ALU.add,
                                    r=[Upre_d[fi], pc, cv_d[fi]], w=[cv_d[fi]])
                                yield
                            th = thR.next()
                            act(th[:], cvf, AF.Tanh, r=[cv_d[fi]], w=[th])
                            stt(xb.ap(fi * 512, [[1, 512]]), th[:], 1.0, cvf, ALU.add, ALU.mult, r=[th, cv_d[fi]], w=[xbd[fi]])
                            yield
                        for c in range(4):
                            for kc in range(8):
                                mm(A0[:, 0:256], hT.ap(kc * 512 + c * 128, [[1, 128]]), wslice(wb, 768, kc, 0, 256),
                                   start=(kc == 0), stop=(kc == 7), r=[hT, wb], w=[A0])
                            th = thR.next()
                            act(th[:, 0:256], A0[:, 0:256], AF.Tanh, r=[A0], w=[th], scale=0.5)
                            stt(zs.ap(c * 256, [[1, 256]]), th[:, 0:256], 1.0, A0[:, 0:256], ALU.add, ALU.mult, r=[th, A0], w=[zs])
                            yield
                        for c in range(4):
                            for fi in range(3):
                                tr(ptr.ap(fi * 128, [[1, 128]]), xb.ap(fi * 512 + c * 128, [[1, 128]]), r=[xbd[fi]], w=[ptrA_d])
                            acopy(xtk.ap(c * 384, [[1, 384]]), ptr.ap(0, [[1, 384]]), r=[ptrA_d], w=[xtk])
                            yield

                    def chunkB(g, c):
                        par = g % 2
                        xb, zs, xtk, xbd = xbP[par], zsP[par], xtkP[par], xb_d[par]
                        hsl = c * 32 + g * 4
                        rhsA, CBm, Eseg, MT = rhsAR.next(), CBmR.next(), EsegR.next(), MTR.next()
                        xc, xcd, xsD, t1, yz = xcR.next(), xcdR.next(), xsDR.next(), t1R.next(), yzR.next()
                        smg = smgP[g % 2]
                        seg = segR.next()
                        cbo, cbd = cbR.next()
                        ydo, ydd = ydR.next()
                        sob, stsd, yofd = soR.next()
                        tt(rhsA.ap(0, [[128, 4], [1, 128]]), pk.ap(TRI, [[0, 4], [1, 128]]), a4.ap(hsl, [[1, 4], [0, 128]]),
                           ALU.mult, r=[pk, a4], w=[rhsA], eng="pool")
                        xs3 = xtk.ap(c * 384, [[64, 4], [1, 64]])
                        tt(xc.ap(0, [[64, 4], [1, 64]]), xs3, dt4.ap(hsl, [[1, 4], [0, 64]]), ALU.mult, r=[xtk, dt4], w=[xc], eng="pool")
                        tt(xcd.ap(0, [[64, 4], [1, 64]]), xs3, dtd4.ap(hsl, [[1, 4], [0, 64]]), ALU.mult, r=[xtk, dtd4], w=[xcd], eng="pool")
                        tt(xsD.ap(0, [[64, 4], [1, 64]]), xs3, pb.ap(DSK + g * 4, [[1, 4], [0, 64]]), ALU.mult, r=[xtk, pb], w=[xsD], eng="pool")
                        yield
                        mm(seg[:, :], pk[:, USTR:USTR + 128], rhsA[:, :], r=[pk, rhsA], w=[seg])
                        mm(bk4[:, cbo:cbo + 128], xb.ap(2 * 512 + c * 128, [[1, 128]]), xb.ap(3 * 512 + c * 128, [[1, 128]]),
                           r=[xbd[2], xbd[3]], w=[cbd])
                        tt(CBm[:], bk4[:, cbo:cbo + 128], pk[:, TRI:TRI + 128], ALU.mult, r=[cbd, pk], w=[CBm])
                        act(Eseg[:], seg[:, :], AF.Exp, r=[seg], w=[Eseg])
                        yield
                        tt(MT.ap(0, [[128, 4], [1, 128]]), Eseg.ap(0, [[128, 4], [1, 128]]), CBm.ap(0, [[0, 4], [1, 128]]),
                           ALU.mult, r=[Eseg, CBm], w=[MT])
                        yield
                        while c > 0 and not state_done.get((g, c - 1)):
                            yield
                        mm(sob[:, 256:512], xb.ap(3 * 512 + c * 128, [[1, 128]]), Sb.ap(g * 256, [[1, 256]]),
                           r=[xbd[3], Sb_deps[g]], w=[yofd])
                        for j in range(4):
                            mm(bk5[:, ydo + j * 64:ydo + (j + 1) * 64], MT[:, j * 128:(j + 1) * 128], xc[:, j * 64:(j + 1) * 64],
                               start=True, stop=True, r=[MT, xc], w=[ydd])
                        mm(sob[:, 0:256], xtk.ap(c * 384 + 256, [[1, 128]]), xcd[:], r=[xtk, xcd], w=[stsd])
                        tt(St.ap(g * 256, [[64, 4], [1, 64]]), St.ap(g * 256, [[64, 4], [1, 64]]),
                           cdb4.ap(hsl, [[1, 4], [0, 64]]), ALU.mult, r=[St_deps[g], cdb4], w=[St_deps[g]])
                        tt(St.ap(g * 256, [[1, 256]]), St.ap(g * 256, [[1, 256]]), sob[:, 0:256], ALU.add,
                           r=[St_deps[g], stsd], w=[St_deps[g]])
                        acopy(Sb.ap(g * 256, [[1, 256]]), St.ap(g * 256, [[1, 256]]), r=[St_deps[g]], w=[Sb_deps[g]])
                        state_done[(g, c)] = True
                        tt(t1.ap(0, [[64, 4], [1, 64]]), sob.ap(256, [[64, 4], [1, 64]]), eacs4.ap(hsl, [[1, 4], [0, 64]]),
                           ALU.mult, r=[yofd, eacs4], w=[t1])
                        tt(t1[:], bk5[:, ydo:ydo + 256], t1[:], ALU.add, r=[ydd, t1], w=[t1])
                        tt(t1[:], xsD[:], t1[:], ALU.add, r=[xsD, t1], w=[t1])
                        yield
                        tt(yz[:], t1[:], zs.ap(c * 256, [[1, 256]]), ALU.mult, r=[t1, zs], w=[yz])
                        act(yjk[:], yz[:], AF.Square, r=[yz], w=[yjk, smg], accum=smg[:, c:c + 1])
                        yzs[(g, c)] = yz
                        yield

                    def normB(g):
                        smg = smgP[g % 2]
                        act(smg[:, 4:8], smg[:, 0:4], AF.Sqrt, r=[smg, cc], w=[smg], bias=cc[:, 3:4], scale=1.0 / 256)
                        recip(smg[:, 8:12], smg[:, 4:8], r=[smg], w=[smg])
                        for c in range(4):
                            yz = yzs.pop((g, c))
                            yN = yNR.next()
                            stt(yN[:], yz[:], smg[:, 8 + c:9 + c], pb[:, SNW + g * 256:SNW + (g + 1) * 256], ALU.mult, ALU.mult,
                                r=[yz, smg, pb], w=[yN])
                            for i in range(2):
                                tr(ptr.ap(512 + (c % 2) * 256 + i * 128, [[1, 128]]), yN[:, i * 128:(i + 1) * 128], r=[yN], w=[ptrB_d[c % 2]])
                            acopy(yNT.ap((g * 2) * 512 + c * 128, [[512, 2], [1, 128]]), ptr.ap(512 + (c % 2) * 256, [[128, 2], [1, 128]]),
                                  r=[ptrB_d[c % 2]], w=[yNT])

                    def run_group(g):
                        if g + 2 < 8:
                            wq[g + 2] = load_w(win_v, C_GRP + (g + 2) * 768, 768)
                        pending = [chunkB(g, c) for c in range(4)]
                        active = [pending.pop(0), pending.pop(0)]
                        ag = stageA(g + 1) if g < 7 else None
                        while active or ag is not None:
                            for g_ in list(active):
                                try:
                                    next(g_)
                                except StopIteration:
                                    active.remove(g_)
                                    if pending:
                                        active.append(pending.pop(0))
                            for _rep in range(2):
                                if ag is not None:
                                    try:
                                        next(ag)
                                    except StopIteration:
                                        ag = None

                    wq = {0: pre_w.pop(), 1: load_w(win_v, C_GRP + 768, 768)}
                    for _ in stageA(0):
                        pass
                    for g in range(8):
                        run_group(g)
                        normB(g)
                    if seq == 0 and stl == 0:
                        dump("yNT", yNT, yNT[:], [128, 16, 512])
                    obanks = [accR.tiles[0], accR.tiles[1], bk3, bk7]
                    for half in range(2):
                        for kh in range(2):
                            wb = wring.next()
                            dma("pool", wb.ap(0, [[512, 8], [1, 512]]), wso_v[:, kh * 8:(kh + 1) * 8, half * 512:(half + 1) * 512], w=[wb])
                            for tt_ in range(4):
                                for kc in range(8):
                                    mm(obanks[tt_][:, :], yNT.ap((kh * 8 + kc) * 512 + tt_ * 128, [[1, 128]]), wslice(wb, 512, kc, 0, 512),
                                       start=(kh == 0 and kc == 0), stop=(kh == 1 and kc == 7), r=[yNT, wb], w=[obanks[tt_]])
                        for tt_ in range(4):
                            acopy(yssm.ap(tt_ * 1024 + half * 512, [[1, 512]]), obanks[tt_][:, :], r=[obanks[tt_]], w=[yssm])
                if seq == 0 and stl == 0:
                    dump("yssm", yssm, yssm[:], [128, 4, 1024])

                S.barrier()
                with ExitStack() as ph:
                    QA = T("QA", [128, 16, 512], BF16, stack=ph)
                    QI = T("QI", [128, 8, 512], BF16, stack=ph)
                    zA = T("zA", [128, 4, 1024], BF16, stack=ph)
                    isc = T("isc", [128, SEQ], F32, stack=ph)
                    rlR = Ring([T("rl%d" % i, [128, 512], F32, stack=ph) for i in range(2)])
                    maskb = T("maskb", [128, SEQ], BF16, stack=ph)
                    maskTP = [T("maskT%d" % i, [128, 16, 128], BF16, stack=ph) for i in range(2)]
                    ER = Ring([T("E%d" % i, [128, 512], BF16, stack=ph) for i in range(3)])
                    PR = Ring([T("P%d" % i, [128, 512], BF16, stack=ph) for i in range(3)])
                    og = T("og", [128, 1024], BF16, stack=ph)
                    Lm = T("Lm", [128, 512], F32, stack=ph)
                    OTs = T("OTs", [128, 512], F32, stack=ph)
                    thA = Lm
                    oT = T("oT", [128, 8, 512], BF16, stack=ph)
                    bs = T("bs", [128, 4], F32, stack=ph)
                    rc = T("rc", [128, 4], F32, stack=ph)
                    bu = T("bu", [128, 2], U32, stack=ph)
                    LR = Ring([bk3, accR.tiles[1]])
                    A0 = accR.tiles[0]
                    A1 = accR.tiles[1]
                    Ob = [bk4, bk5, bk6, bk7]
                    Odeps = [Ob[j].dep for j in range(4)]
                    ptrA_d = ptrB_d = ptr.dep

                    dma("sp", QA.ap(0, [[512, 16], [1, 512]], p0=64, np_=5), qaug_d[:, :, t0:t0 + 512], w=[QA])
                    wb = load_w(win_v, C_QI, 512)
                    for h in range(8):
                        for kc in range(8):
                            mm(A0[0:64, :], wslice(wb, 512, kc, h * 64, (h + 1) * 64), hT.ap(kc * 512, [[1, 512]]),
                               start=(kc == 0), stop=(kc == 7), r=[wb, hT], w=[A0])
                        acopy(QI.ap(h * 512, [[1, 512]], np_=64), A0[0:64, :], r=[A0], w=[QI])

                    def stageI(tt_):
                        qb = stl * 4 + tt_
                        SL = (qb + 1) * 128
                        maskT = maskTP[tt_ % 2]
                        for c4 in range((SL + 511) // 512):
                            w_ = min(512, SL - c4 * 512)
                            for h in range(8):
                                mm(A0[:, 0:w_], QI.ap(h * 512 + tt_ * 128, [[1, 128]], np_=64), KIN.ap(c4 * 512, [[1, w_]], np_=64),
                                   r=[QI, KIN], w=[A0])
                                rl = rlR.next()
                                act(rl[:, 0:w_], A0[:, 0:w_], AF.Relu, r=[A0], w=[rl])
                                wcol = wis.ap(tt_ * 8 + h, [[1, 1]])
                                if h == 0:
                                    ts(isc[:, c4 * 512:c4 * 512 + w_], rl[:, 0:w_], wcol, ALU.mult, r=[rl, wis], w=[isc])
                                else:
                                    stt(isc[:, c4 * 512:c4 * 512 + w_], rl[:, 0:w_], wcol, isc[:, c4 * 512:c4 * 512 + w_],
                                        ALU.mult, ALU.add, r=[rl, wis, isc], w=[isc])
                                yield
                        if qb >= 2:
                            S.op("dve", lambda e, SL=SL, bs=bs, isc=isc: e.tensor_reduce(out=bs[:, 1:2], in_=isc[:, 0:SL], axis=AX.X, op=ALU.max), [isc], [bs])
                            S.op("dve", lambda e, SL=SL, bs=bs, isc=isc: e.tensor_reduce(out=bs[:, 0:1], in_=isc[:, 0:SL], axis=AX.X, op=ALU.min), [isc], [bs])
                            ts(bs[:, 1:2], bs[:, 1:2], 1.0, ALU.add, r=[bs], w=[bs])
                        tt(isc[:, SL - 128:SL], isc[:, SL - 128:SL], pk[:, NEGM:NEGM + 128], ALU.add, r=[isc, pk], w=[isc])
                        yield
                        if qb >= 2:
                            for it in range(NIT):
                                ts(bs[:, 2:3], bs[:, 0:1], bs[:, 1:2], ALU.add, r=[bs], w=[bs], s2=0.5, op1=ALU.mult)
                                ts(maskb[:, 0:SL], isc[:, 0:SL], bs[:, 2:3], ALU.is_ge, r=[isc, bs], w=[maskb, bs],
                                   s2=None, op1=ALU.add, accum=bs[:, 3:4])
                                yield
                                ts(bu[:, 0:1], bs[:, 3:4], TOPK - 0.5, ALU.is_ge, r=[bs], w=[bu])
                                ts(bu[:, 1:2], bs[:, 3:4], TOPK - 0.5, ALU.is_lt, r=[bs], w=[bu])
                                S.op("dve", lambda e, bs=bs, bu=bu: e.copy_predicated(out=bs[:, 0:1], mask=bu[:, 0:1], data=bs[:, 2:3]), [bs, bu], [bs])
                                S.op("dve", lambda e, bs=bs, bu=bu: e.copy_predicated(out=bs[:, 1:2], mask=bu[:, 1:2], data=bs[:, 2:3]), [bs, bu], [bs])
                                yield
                            thr = bs[:, 0:1]
                        else:
                            thr = cc[:, 2:3]
                        ts(maskb[:, 0:SL], isc[:, 0:SL], thr, ALU.is_ge, r=[isc, bs, cc], w=[maskb])
                        if seq == 0 and stl == 0 and tt_ == 3:
                            dump("isc", isc, isc[:, 0:512], [128, 512])
                            dump("bs", bs, bs[:], [128, 4])
                        for k0 in range(0, qb + 1, 4):
                            nk = min(4, qb + 1 - k0)
                            for kb in range(k0, k0 + nk):
                                tr(ptr.ap((kb - k0) * 128, [[1, 128]]), maskb[:, kb * 128:(kb + 1) * 128], r=[maskb], w=[ptrA_d])
                            acopy(maskT.ap(k0 * 128, [[1, nk * 128]]), ptr.ap(0, [[1, nk * 128]]), r=[ptrA_d], w=[maskT])
                            yield

                    def stageAT(tt_):
                        qb = stl * 4 + tt_
                        maskT = maskTP[tt_ % 2]
                        steps = [(hg, kb) for hg in range(4) for kb in range(qb + 1)]
                        Ps = {}

                        def front(i):
                            hg, kb = steps[i]
                            L = LR.next()
                            mm(L[:, :], KA.ap(kb * 128, [[1, 128]], np_=69),
                               QA.ap(hg * 4 * 512 + tt_ * 128, [[512, 4], [1, 128]], np_=69), r=[KA, QA], w=[L])
                            E = ER.next()
                            if kb == qb:
                                tt(Lm.ap(0, [[128, 4], [1, 128]]), L.ap(0, [[128, 4], [1, 128]]),
                                   pk.ap(NEGT, [[0, 4], [1, 128]]), ALU.add, r=[L, pk], w=[Lm])
                                act(E[:], Lm[:], AF.Exp, r=[Lm], w=[E])
                            else:
                                act(E[:], L[:, :], AF.Exp, r=[L], w=[E])
                            P = PR.next()
                            tt(P.ap(0, [[128, 4], [1, 128]]), E.ap(0, [[128, 4], [1, 128]]),
                               maskT.ap(kb * 128, [[0, 4], [1, 128]]), ALU.mult, r=[E, maskT], w=[P],
                               eng=("pool" if i % 2 == 0 else "dve"))
                            Ps[i] = P

                        def back(i):
                            hg, kb = steps[i]
                            P = Ps.pop(i)
                            mm(Ob[hg][0:66, :], VA.ap(kb * 66, [[1, 66]]), P[:, :], start=(kb == 0), stop=(kb == qb),
                               r=[VA, P], w=[Odeps[hg]])
                            if kb == qb:
                                acopy(OTs[0:66, :], Ob[hg][0:66, :], r=[Odeps[hg]], w=[OTs])
                                for j in range(4):
                                    S.op("pe", lambda e, j=j, hg=hg, OTs=OTs: e.transpose(out=Ob[hg][:, j * 66:(j + 1) * 66],
                                                                                         in_=OTs[0:66, j * 128:(j + 1) * 128],
                                                                                         identity=pk[0:66, IDENT:IDENT + 66]),
                                         [OTs, pk], [Odeps[hg]])
                                for j in range(4):
                                    h = hg * 4 + j
                                    recip(rc[:, j:j + 1], Ob[hg][:, j * 66 + 64:j * 66 + 65], r=[Odeps[hg]], w=[rc])
                                    stt(og[:, h * 64:(h + 1) * 64], Ob[hg][:, j * 66:j * 66 + 64], rc[:, j:j + 1],
                                        zA.ap(tt_ * 1024 + h * 64, [[1, 64]]), ALU.mult, ALU.mult, r=[Odeps[hg], rc, zA], w=[og])

                        front(0)
                        for i in range(len(steps)):
                            if i + 1 < len(steps):
                                front(i + 1)
                            back(i)
                            yield
                        for kc in range(8):
                            tr(ptr.ap(512 + (kc % 4) * 128, [[1, 128]]), og[:, kc * 128:(kc + 1) * 128], r=[og], w=[ptrB_d])
                            if kc % 4 == 3:
                                acopy(oT.ap((kc - 3) * 512 + tt_ * 128, [[512, 4], [1, 128]]), ptr.ap(512, [[128, 4], [1, 128]]),
                                      r=[ptrB_d], w=[oT])
                        yield

                    def stageProj():
                        for half in range(2):
                            wb = load_w(win_v, C_Q + half * 512, 512)
                            for hh in range(8):
                                h = half * 8 + hh
                                for kc in range(8):
                                    mm(A1[0:64, :], wslice(wb, 512, kc, hh * 64, (hh + 1) * 64), hT.ap(kc * 512, [[1, 512]]),
                                       start=(kc == 0), stop=(kc == 7), r=[wb, hT], w=[A1])
                                S.op("act", lambda e, h=h, QA=QA, A1=A1: e.mul(QA.ap(h * 512, [[1, 512]], np_=64), A1[0:64, :], 0.125), [A1], [QA])
                                yield
                        for half in range(2):
                            wb = load_w(win_v, C_AZ + half * 512, 512)
                            for tt_ in range(4):
                                for kc in range(8):
                                    mm(A1[:, :], hT.ap(kc * 512 + tt_ * 128, [[1, 128]]), wslice(wb, 512, kc, 0, 512),
                                       start=(kc == 0), stop=(kc == 7), r=[hT, wb], w=[A1])
                                act(thA[:], A1[:, :], AF.Tanh, r=[A1], w=[thA], scale=0.5)
                                stt(zA.ap(tt_ * 1024 + half * 512, [[1, 512]]), thA[:], 1.0, A1[:, :], ALU.add, ALU.mult,
                                    r=[thA, A1], w=[zA])
                                yield

                    interleave(stageI(0), stageProj())
                    if seq == 0 and stl == 0:
                        dump("QA", QA, QA[:], [128, 16, 512])
                        dump("QI", QI, QI[:], [128, 8, 512])
                    for tt_ in range(4):
                        interleave(stageAT(tt_), stageI(tt_ + 1) if tt_ < 3 else iter(()))
                    if seq == 0 and stl == 0:
                        dump("oT", oT, oT[:], [128, 8, 512])
                    for half in range(2):
                        wb = load_w(wao_v, half * 512, 512)
                        for tt_ in range(4):
                            acc = accR.next()
                            for kc in range(8):
                                mm(acc[:, :], oT.ap(kc * 512 + tt_ * 128, [[1, 128]]), wslice(wb, 512, kc, 0, 512),
                                   start=(kc == 0), stop=(kc == 7), r=[oT, wb], w=[acc])
                            acopy(yattn.ap(tt_ * 1024 + half * 512, [[1, 512]]), acc[:, :], r=[acc], w=[yattn])
                if seq == 0 and stl == 0:
                    dump("yattn", yattn, yattn[:], [128, 4, 1024])

                S.barrier()
                with ExitStack() as ph:
                    gtR = Ring([T("gt%d" % i, [128, 512], F32, stack=ph) for i in range(2)])
                    gsR = Ring([T("gs%d" % i, [128, 512], F32, stack=ph) for i in range(2)])
                    mg = T("mg", [128, 4, 1024], F32, stack=ph)
                    mb = T("mb", [128, 1024], BF16, stack=ph)
                    mT = T("mT", [128, 8, 512], BF16, stack=ph)
                    r4 = T("r4", [128, 4, 1024], F32, stack=ph)
                    roR = Ring([T("ro%d" % i, [128, 1024], F32, stack=ph) for i in range(2)])
                    p5 = T("p5", [128, NPB - NRES], F32, stack=ph)
                    dma("sp", p5[:], pb_d[:, NRES:NPB], w=[p5])
                    for u in range(4):
                        wb = load_w(win_v, C_GATE + u * 512, 512)
                        for tt_ in range(4):
                            acc = accR.next()
                            for kc in range(8):
                                mm(acc[:, :], hT.ap(kc * 512 + tt_ * 128, [[1, 128]]), wslice(wb, 512, kc, 0, 512),
                                   start=(kc == 0), stop=(kc == 7), r=[hT, wb], w=[acc])
                            gtt = gtR.next()
                            tt(gtt[:], acc[:, :], p5[:, u * 512:(u + 1) * 512], ALU.add, r=[acc, p5], w=[gtt])
                            gs = gsR.next()
                            act(gs[:], gtt[:], AF.Tanh, r=[gtt], w=[gs], scale=0.5)
                            if u < 2:
                                stt(mg.ap(tt_ * 1024 + u * 512, [[1, 512]]), gs[:], 1.0, yssm.ap(tt_ * 1024 + u * 512, [[1, 512]]),
                                    ALU.add, ALU.mult, r=[gs, yssm], w=[mg])
                            else:
                                stt(gs[:], gs[:], 1.0, yattn.ap(tt_ * 1024 + (u - 2) * 512, [[1, 512]]), ALU.add, ALU.mult,
                                    r=[gs, yattn], w=[gs])
                                tt(mg.ap(tt_ * 1024 + (u - 2) * 512, [[1, 512]]), mg.ap(tt_ * 1024 + (u - 2) * 512, [[1, 512]]),
                                   gs[:], ALU.add, r=[mg, gs], w=[mg])
                    for tt_ in range(4):
                        S.op("act", lambda e, tt_=tt_, mb=mb, mg=mg: e.mul(mb[:], mg.ap(tt_ * 1024, [[1, 1024]]), 0.5), [mg], [mb])
                        for kc in range(8):
                            tr(ptr.ap(kc * 128, [[1, 128]]), mb[:, kc * 128:(kc + 1) * 128], r=[mb], w=[ptr])
                        acopy(mT.ap(tt_ * 128, [[512, 8], [1, 128]]), ptr.ap(0, [[128, 8], [1, 128]]), r=[ptr], w=[mT])
                    for half in range(2):
                        wb = load_w(wout_v, half * 512, 512)
                        for tt_ in range(4):
                            acc = accR.next()
                            for kc in range(8):
                                mm(acc[:, :], mT.ap(kc * 512 + tt_ * 128, [[1, 128]]), wslice(wb, 512, kc, 0, 512),
                                   start=(kc == 0), stop=(kc == 7), r=[mT, wb], w=[acc])
                            acopy(r4.ap(tt_ * 1024 + half * 512, [[1, 512]]), acc[:, :], r=[acc], w=[r4])
                    for tt_ in range(4):
                        xt = xring.next()
                        dma("sp", xt[:], x_d[seq, t0 + tt_ * 128:t0 + (tt_ + 1) * 128, :], w=[xt])
                        r4s = r4.ap(tt_ * 1024, [[1, 1024]])
                        tt(r4s, r4s, xt[:], ALU.add, r=[r4, xt], w=[r4])
                        act(mb[:], r4s, AF.Square, r=[r4], w=[mb, sm], accum=sm[:, 0:1])
                        rstd_col(sm[:, 2:3], sm[:, 0:1], 1.0 / D_MODEL, cc[:, 0:1], sm)
                        ro = roR.next()
                        stt(ro[:], r4s, sm[:, 2:3], p5[:, FNW - NRES:FNW - NRES + 1024], ALU.mult, ALU.mult, r=[r4, sm, p5], w=[ro])
                        dma("sp", out_d[seq, t0 + tt_ * 128:t0 + (tt_ + 1) * 128, :], ro[:], r=[ro], w=[ro])
        S.final_wait("sp")
        S.emit()
    return nc, dbg_outs


def host_prep(inputs):
    f32 = np.float32
    w_in = np.asarray(inputs["w_in"], f32)[0]
    O_Z, O_XBC, O_DT, O_Q, O_K, O_V, O_AZ, O_QI, O_KI, O_WI, O_G = 0, 2048, 6144, 6176, 7200, 7264, 7328, 8352, 8864, 8928, 8936
    cols = []
    cols += list(range(O_DT, O_DT + 32)) + list(range(O_K, O_K + 64)) + list(range(O_V, O_V + 64))
    cols += list(range(O_KI, O_KI + 64)) + list(range(O_WI, O_WI + 8))
    for g in range(8):
        cols += list(range(O_Z + g * 256, O_Z + (g + 1) * 256))
        cols += list(range(O_XBC + g * 256, O_XBC + (g + 1) * 256))
        cols += list(range(O_XBC + 2048 + g * 128, O_XBC + 2048 + (g + 1) * 128))
        cols += list(range(O_XBC + 3072 + g * 128, O_XBC + 3072 + (g + 1) * 128))
    cols += list(range(O_Q, O_Q + 1024)) + list(range(O_QI, O_QI + 512)) + list(range(O_AZ, O_AZ + 1024))
    cols += list(range(O_G, O_G + 2048))
    cols = np.asarray(cols)
    assert cols.shape[0] == IN_TOTAL and np.unique(cols).shape[0] == IN_TOTAL
    win = np.ascontiguousarray(w_in[:, cols])

    pb = np.zeros((128, NPB), f32)

    def put(off, v):
        v = np.asarray(v, f32).reshape(-1)
        pb[:, off:off + v.shape[0]] = v[None, :]
    put(NW, inputs["norm_w"][0])
    put(FNW, inputs["final_norm_w"])
    put(GB, inputs["gate_b"][0])
    put(SNW, inputs["ssm_norm_w"][0])
    put(DTB, inputs["dt_bias"][0])
    put(ALOG, inputs["a_log"][0])
    put(DSK, inputs["d_skip"][0])
    put(KIW, inputs["idx_k_norm_w"][0])
    put(KIB, inputs["idx_k_norm_b"][0])

    conv_w = np.asarray(inputs["conv_w"], f32)[0]
    conv_b = np.asarray(inputs["conv_b"], f32)[0]
    pc = np.zeros((128, 160), f32)
    for g in range(8):
        for fi in range(4):
            ct = g * 4 + fi
            if fi < 2:
                ch0 = g * 256 + fi * 128
            elif fi == 2:
                ch0 = 2048 + g * 128
            else:
                ch0 = 3072 + g * 128
            pc[:, ct * 4:(ct + 1) * 4] = conv_w[:, ch0:ch0 + 128].T
            pc[:, 128 + ct] = conv_b[ch0:ch0 + 128]

    pk = np.zeros((128, NPK), f32)
    i = np.arange(128)
    pk[:, IDENT:IDENT + 128] = np.eye(128, dtype=f32)
    pk[:, TRI:TRI + 128] = (i[:, None] <= i[None, :]).astype(f32)
    pk[:, USTR:USTR + 128] = (i[:, None] > i[None, :]).astype(f32)
    pk[:, NEGM:NEGM + 128] = np.where(i[None, :] > i[:, None], f32(-1e30), f32(0))
    pk[:, ONES:ONES + 128] = 1.0
    pk[:, NEGT:NEGT + 128] = np.where(i[:, None] > i[None, :], f32(-1e30), f32(0))

    bf = ml_dtypes.bfloat16
    slopes = np.exp2(-8.0 * np.arange(1, 17, dtype=np.float64) / 16).astype(f32)
    s_hi = slopes.astype(bf)
    s_lo = (slopes - s_hi.astype(f32)).astype(bf)
    spos = np.arange(SEQ)
    kaug = np.zeros((5, SEQ), f32)
    kaug[0] = 1.0
    kaug[1] = spos % 128
    kaug[2] = (spos // 128) * 128
    kaug[3] = spos % 128
    kaug[4] = (spos // 128) * 128
    kaug = kaug.astype(bf)
    qaug = np.zeros((5, 16, SEQ), f32)
    qaug[0] = -(slopes[:, None].astype(np.float64) * spos[None, :]).astype(f32)
    qaug[1] = s_hi.astype(f32)[:, None]
    qaug[2] = s_hi.astype(f32)[:, None]
    qaug[3] = s_lo.astype(f32)[:, None]
    qaug[4] = s_lo.astype(f32)[:, None]
    qaug = qaug.astype(bf)
    shared = {
        "win": win,
        "wso": np.ascontiguousarray(np.asarray(inputs["w_ssm_out"], f32)[0]),
        "wao": np.ascontiguousarray(np.asarray(inputs["w_attn_out"], f32)[0]),
        "wout": np.ascontiguousarray(np.asarray(inputs["w_out"], f32)[0]),
        "pb": pb, "pc": pc, "pk": pk, "kaug": kaug, "qaug": qaug,
    }
    return shared


_CACHE = {}


def kernel(**inputs):
    x = np.asarray(inputs["x"], np.float32)
    shared = host_prep(inputs)
    if "nc" not in _CACHE:
        _CACHE["nc"] = build_program(2)[0]
    nc = _CACHE["nc"]
    in_maps = []
    for c in range(8):
        m = dict(shared)
        m["x"] = np.ascontiguousarray(x[2 * c:2 * c + 2])
        in_maps.append(m)
    res = run_bass_kernel_spmd(nc, in_maps, core_ids=list(range(8)))
    out = np.concatenate([np.asarray(r["out"], np.float32) for r in res.results], axis=0)
    return out
```

```python
import numpy as np
import ml_dtypes
from contextlib import ExitStack
import concourse.bass as bass
import concourse.mybir as mybir
from concourse.bass_utils import run_bass_kernel_spmd

F32 = mybir.dt.float32
BF16 = mybir.dt.bfloat16
U32 = mybir.dt.uint32
AF = mybir.ActivationFunctionType
ALU = mybir.AluOpType
AX = mybir.AxisListType

D_MODEL = 1024
SEQ = 2048
IN_TOTAL = 10984
NIT = 13
TOPK = 256
EPS = 1e-6
IDX_SCALE = (8 ** -0.5) * (64 ** -0.5)

NW, SNW, DTB, ALOG, DSK, KIW, KIB, NRES, GB, FNW, NPB = 0, 1024, 3072, 3104, 3136, 3168, 3232, 3296, 3296, 5344, 6368
IDENT, TRI, USTR, NEGM, ONES, NEGT, NPK = 0, 128, 256, 384, 512, 640, 768
C_SMALL, C_GRP, C_Q, C_QI, C_AZ, C_GATE = 0, 232, 6376, 7400, 7912, 8936


class Dep:
    __slots__ = ("name", "w", "r")

    def __init__(self, name=""):
        self.name = name
        self.w = None
        self.r = {}


class Sched:
    ENGS = ("pe", "act", "dve", "pool", "sp")

    def __init__(self, nc, stack, n_dma_sems=24):
        self.nc = nc
        self.lists = {e: [] for e in self.ENGS}
        self.sems = {}
        self.cnt = {}
        for e in ("pe", "act", "dve", "pool"):
            self.sems[e] = stack.enter_context(nc.semaphore("s_" + e))
            self.cnt[e] = 0
        self.dma_pool = {}
        for q, n in (("sp", n_dma_sems), ("pool", 8)):
            keys = []
            for i in range(n):
                k = "d_%s_%d" % (q, i)
                self.sems[k] = stack.enter_context(nc.semaphore(k))
                self.cnt[k] = 0
                keys.append(k)
            self.dma_pool[q] = [keys, 0]
        self.seen = {e: {} for e in self.ENGS}
        self.n_ops = 0

    def _needs(self, eng, reads, writes):
        needs = {}

        def add(k, v):
            if v > needs.get(k, 0):
                needs[k] = v
        for d in reads:
            if d.w is not None and not (eng == "pe" and d.w[0] == "pe"):
                add(*d.w)
        for d in writes:
            if d.w is not None and not (eng == "pe" and d.w[0] == "pe"):
                add(*d.w)
            for k, v in d.r.items():
                if not (eng == "pe" and k == "pe"):
                    add(k, v)
        out = []
        seen = self.seen[eng]
        for k, v in needs.items():
            if seen.get(k, 0) >= v:
                continue
            seen[k] = v
            out.append((k, v))
        return out

    def op(self, eng, fn, reads=(), writes=()):
        reads = [getattr(d, "dep", d) for d in reads]
        writes = [getattr(d, "dep", d) for d in writes]
        waits = self._needs(eng, reads, writes)
        self.cnt[eng] += 1
        v = self.cnt[eng]
        self.lists[eng].append((waits, fn, eng, 1))
        for d in reads:
            d.r[eng] = v
        for d in writes:
            d.w = (eng, v)
            d.r = {}
        self.n_ops += 1

    def dma(self, q, fn, reads=(), writes=()):
        reads = [getattr(d, "dep", d) for d in reads]
        writes = [getattr(d, "dep", d) for d in writes]
        keys, idx = self.dma_pool[q]
        k = keys[idx % len(keys)]
        self.dma_pool[q][1] = idx + 1
        waits = self._needs(q, reads, writes)
        prev = self.cnt[k]
        if prev > 0 and self.seen[q].get(k, 0) < prev:
            self.seen[q][k] = prev
            waits.append((k, prev))
        self.cnt[k] += 16
        v = self.cnt[k]
        self.lists[q].append((waits, fn, k, 16))
        for d in reads:
            d.r[k] = v
        for d in writes:
            d.w = (k, v)
            d.r = {}
        self.n_ops += 1

    def barrier(self, engs=("pe", "act", "dve", "sp")):
        for e in engs:
            waits = []
            for k, v in self.cnt.items():
                if k == e or k.startswith("d_pool") or v == 0:
                    continue
                if self.seen[e].get(k, 0) < v:
                    self.seen[e][k] = v
                    waits.append((k, v))
            if waits:
                self.lists[e].append((waits, None, None, 0))

    def final_wait(self, eng):
        waits = []
        for k, v in self.cnt.items():
            if k == eng or v == 0:
                continue
            if self.seen[eng].get(k, 0) < v:
                self.seen[eng][k] = v
                waits.append((k, v))
        self.lists[eng].append((waits, None, None, 0))

    def emit(self):
        nc = self.nc
        sems = self.sems
        lists = self.lists

        def replay(e, lst):
            for waits, fn, k, inc in lst:
                for (wk, wv) in waits:
                    e.wait_ge(sems[wk], wv)
                if fn is not None:
                    fn(e).then_inc(sems[k], inc)

        with nc.Block() as block:
            @block.tensor
            def _(e):
                replay(e, lists["pe"])

            @block.scalar
            def _(e):
                replay(e, lists["act"])

            @block.vector
            def _(e):
                replay(e, lists["dve"])

            @block.gpsimd
            def _(e):
                replay(e, lists["pool"])

            @block.sync
            def _(e):
                replay(e, lists["sp"])


def build_program(n_seq=2, dbg=None):
    nc = bass.Bass("TRN2", target_bir_lowering=False)

    def dram(name, shape, dt=F32, kind="ExternalInput"):
        return nc.dram_tensor(name, shape, dt, kind=kind).ap()

    x_d = dram("x", [n_seq, SEQ, D_MODEL])
    win_d = dram("win", [D_MODEL, IN_TOTAL])
    wso_d = dram("wso", [2048, 1024])
    wao_d = dram("wao", [1024, 1024])
    wout_d = dram("wout", [1024, 1024])
    pb_d = dram("pb", [128, NPB])
    pc_d = dram("pc", [128, 160])
    pk_d = dram("pk", [128, NPK])
    kaug_d = dram("kaug", [5, SEQ], BF16)
    qaug_d = dram("qaug", [5, 16, SEQ], BF16)
    out_d = dram("out", [n_seq, SEQ, D_MODEL], kind="ExternalOutput")
    dbg_outs = {}

    win_v = win_d.rearrange("(kc p) n -> p kc n", p=128)
    wso_v = wso_d.rearrange("(kc p) n -> p kc n", p=128)
    wao_v = wao_d.rearrange("(kc p) n -> p kc n", p=128)
    wout_v = wout_d.rearrange("(kc p) n -> p kc n", p=128)

    with ExitStack() as st0:
        S = Sched(nc, st0)
        uid = [0]

        class T:
            def __init__(self, name, shape, dt, psum=False, stack=st0):
                uid[0] += 1
                nm = "%s_%d" % (name, uid[0])
                alloc = nc.psum_tensor if psum else nc.sbuf_tensor
                self.t = stack.enter_context(alloc(nm, list(shape), dt))
                self.dep = Dep(nm)
                self.row = int(np.prod(shape[1:]))

            def __getitem__(self, k):
                return self.t[k]

            def ap(self, col0, dims, p0=0, np_=128):
                return bass.AP(self.t, p0 * self.row + col0, [[self.row, np_]] + [list(d) for d in dims])

        class Ring:
            def __init__(self, tiles):
                self.tiles = tiles
                self.i = 0

            def next(self):
                t = self.tiles[self.i % len(self.tiles)]
                self.i += 1
                return t

        def mm(out, lhsT, rhs, start=True, stop=True, r=(), w=()):
            S.op("pe", lambda e: e.matmul(out, lhsT=lhsT, rhs=rhs, start=start, stop=stop), r, w)

        def tr(out, in_, r=(), w=()):
            S.op("pe", lambda e: e.transpose(out=out, in_=in_, identity=identb[:]), list(r) + [identb], w)

        def act(out, in_, func, r=(), w=(), bias=None, scale=None, accum=None):
            kw = {}
            if bias is not None:
                kw["bias"] = bias
            if scale is not None:
                kw["scale"] = scale
            if accum is not None:
                kw["accum_out"] = accum
            S.op("act", lambda e: e.activation(out=out, in_=in_, func=func, **kw), r, w)

        def acopy(out, in_, r=(), w=()):
            S.op("act", lambda e: e.copy(out=out, in_=in_), r, w)

        def tt(out, a, b, op, r=(), w=(), eng="dve"):
            S.op(eng, lambda e: e.tensor_tensor(out=out, in0=a, in1=b, op=op), r, w)

        def ts(out, a, s1, op0, r=(), w=(), s2=None, op1=None, accum=None):
            kw = {}
            if op1 is not None:
                kw["op1"] = op1
            if accum is not None:
                kw["accum_out"] = accum
            S.op("dve", lambda e: e.tensor_scalar(out=out, in0=a, scalar1=s1, scalar2=s2, op0=op0, **kw), r, w)

        def stt(out, a, s, b, op0, op1, r=(), w=()):
            S.op("dve", lambda e: e.scalar_tensor_tensor(out=out, in0=a, scalar=s, in1=b, op0=op0, op1=op1), r, w)

        def vcopy(out, in_, r=(), w=()):
            S.op("dve", lambda e: e.tensor_copy(out=out, in_=in_), r, w)

        def memset(ap, val, w=()):
            S.op("dve", lambda e: e.memset(ap, val), (), w)

        def recip(out, in_, r=(), w=()):
            S.op("dve", lambda e: e.reciprocal(out=out, in_=in_), r, w)

        def dma(q, out, in_, r=(), w=()):
            S.dma(q, lambda e: e.dma_start(out=out, in_=in_), r, w)

        def rstd_col(out_col, ssq_col, scale, eps_col, tile):
            act(ssq_col, ssq_col, AF.Sqrt, r=[tile, cc], w=[tile], bias=eps_col, scale=scale)
            recip(out_col, ssq_col, r=[tile], w=[tile])

        def interleave(*gens):
            alive = list(gens)
            while alive:
                for g_ in list(alive):
                    try:
                        next(g_)
                    except StopIteration:
                        alive.remove(g_)

        def dump(name, tile, ap, shape):
            if dbg is None or name not in dbg:
                return
            d = nc.dram_tensor("dbg_" + name, list(shape), tile.t.dtype, kind="ExternalOutput").ap()
            dbg_outs[name] = "dbg_" + name
            dma("sp", d, ap, r=[tile], w=[])

        pb = T("pb", [128, NRES], F32)
        pc = T("pc", [128, 160], F32)
        pk = T("pk", [128, NPK], F32)
        identb = T("identb", [128, 128], BF16)
        cc = T("cc", [128, 8], F32)
        Abc = T("Abc", [128, 32], F32)
        Wsm = T("Wsm", [128, 8, 232], BF16)
        KA = T("KA", [128, SEQ], BF16)
        KIN = T("KIN", [128, SEQ], BF16)
        VA = T("VA", [128, 16, 66], BF16)
        St = T("St", [128, 8, 256], F32)
        Sb = T("Sb", [128, 8, 256], BF16)
        St_deps = [Dep("St%d" % g) for g in range(8)]
        Sb_deps = [Dep("Sb%d" % g) for g in range(8)]
        halo = T("halo", [128, 32, 3], F32)
        halo_deps = [Dep("halo%d" % g) for g in range(8)]
        xring = Ring([T("xt%d" % i, [128, 1024], F32) for i in range(2)])
        hb = T("hb", [128, 1024], BF16)
        hT = T("hT", [128, 8, 512], BF16)
        sm = T("sm", [128, 16], F32)
        dt4 = T("dt4", [128, 4, 32], F32)
        a4 = T("a4", [128, 4, 32], F32)
        eacs4 = T("eacs4", [128, 4, 32], F32)
        cdb4 = T("cdb4", [128, 4, 32], F32)
        dtd4 = T("dtd4", [128, 4, 32], F32)
        wis = T("wis", [128, 4, 8], F32)
        s32 = [T("s32_%d" % i, [128, 32], F32) for i in range(4)]
        kvb = T("kvb", [128, 128], BF16)
        kif = T("kif", [128, 64], F32)
        wring = Ring([T("wb%d" % i, [128, 6144], BF16) for i in range(3)])
        yssm = T("yssm", [128, 4, 1024], BF16)
        yattn = T("yattn", [128, 4, 1024], BF16)

        accR = Ring([T("acc%d" % i, [128, 512], F32, psum=True) for i in range(2)])
        ptr = T("ptr", [128, 1024], BF16, psum=True)
        bk3 = T("bk3", [128, 512], F32, psum=True)
        bk4 = T("bk4", [128, 512], F32, psum=True)
        bk5 = T("bk5", [128, 512], F32, psum=True)
        bk6 = T("bk6", [128, 512], F32, psum=True)
        bk7 = T("bk7", [128, 512], F32, psum=True)
        cb_dep = acs_dep = bk4.dep
        sts_dep = yoff_dep = bk6.dep

        dma("sp", pb[:], pb_d[:, 0:NRES], w=[pb])
        dma("sp", pc[:], pc_d, w=[pc])
        dma("sp", pk[:], pk_d, w=[pk])
        vcopy(identb[:], pk[:, IDENT:IDENT + 128], r=[pk], w=[identb])
        memset(cc[:, 0:1], EPS, w=[cc])
        memset(cc[:, 1:2], 1.0, w=[cc])
        memset(cc[:, 2:3], -1e29, w=[cc])
        memset(cc[:, 3:4], 4.0 * EPS, w=[cc])
        ts(pc[:], pc[:], 0.5, ALU.mult, r=[pc], w=[pc])
        act(Abc[:], pb[:, ALOG:ALOG + 32], AF.Exp, r=[pb], w=[Abc])
        ts(Abc[:], Abc[:], -1.0, ALU.mult, r=[Abc], w=[Abc])
        memset(KA[:], 0.0, w=[KA])
        dma("sp", KA.ap(0, [[1, SEQ]], p0=64, np_=5), kaug_d, w=[KA])
        memset(VA[:], 2.0, w=[VA])
        dma("pool", Wsm[:], win_v[:, :, C_SMALL:C_SMALL + 232], w=[Wsm])

        def load_w(src_v, c0, n, nkc=8):
            wb = wring.next()
            dma("pool", wb.ap(0, [[n, nkc], [1, n]]), src_v[:, :, c0:c0 + n], w=[wb])
            return wb

        def wslice(wb, n, kc, a, b):
            return wb.ap(kc * n + a, [[1, b - a]])

        for seq in range(n_seq):
            memset(St[:], 0.0, w=St_deps)
            memset(Sb[:], 0.0, w=Sb_deps)
            memset(halo[:], 0.0, w=halo_deps)
            for stl in range(4):
                t0 = stl * 512
                pre_w = [load_w(win_v, C_GRP, 768)]
                for tt_ in range(4):
                    xt = xring.next()
                    dma("sp", xt[:], x_d[seq, t0 + tt_ * 128:t0 + (tt_ + 1) * 128, :], w=[xt])
                    act(hb[:], xt[:], AF.Square, r=[xt], w=[hb, sm], accum=sm[:, 0:1])
                    rstd_col(sm[:, 2:3], sm[:, 0:1], 1.0 / D_MODEL, cc[:, 0:1], sm)
                    stt(hb[:], xt[:], sm[:, 2:3], pb[:, NW:NW + 1024], ALU.mult, ALU.mult, r=[xt, sm, pb], w=[hb])
                    for kc in range(8):
                        tr(ptr.ap(kc * 128, [[1, 128]]), hb[:, kc * 128:(kc + 1) * 128], r=[hb], w=[ptr])
                    acopy(hT.ap(tt_ * 128, [[512, 8], [1, 128]]), ptr.ap(0, [[128, 8], [1, 128]]), r=[ptr], w=[hT])
                if seq == 0 and stl == 0:
                    dump("hT", hT, hT[:], [128, 8, 512])

                for tt_ in range(4):
                    gt = stl * 4 + tt_
                    acc = accR.next()
                    for kc in range(8):
                        mm(acc[:, 0:232], hT.ap(kc * 512 + tt_ * 128, [[1, 128]]), Wsm.ap(kc * 232, [[1, 232]]),
                           start=(kc == 0), stop=(kc == 7), r=[hT, Wsm], w=[acc])
                    x32, e32, acs_sb, dd = s32
                    tt(x32[:], acc[:, 0:32], pb[:, DTB:DTB + 32], ALU.add, r=[acc, pb], w=[x32])
                    act(e32[:], x32[:], AF.Exp, r=[x32], w=[e32])
                    act(dt4.ap(tt_ * 32, [[1, 32]]), e32[:], AF.Ln, r=[e32, cc], w=[dt4], bias=cc[:, 1:2], scale=1.0)
                    tt(a4.ap(tt_ * 32, [[1, 32]]), dt4.ap(tt_ * 32, [[1, 32]]), Abc[:], ALU.mult, r=[dt4, Abc], w=[a4])
                    acopy(kvb[:, 0:64], acc[:, 32:96], r=[acc], w=[kvb])
                    acopy(VA.ap(gt * 66, [[1, 64]]), acc[:, 96:160], r=[acc], w=[VA])
                    S.op("dve", lambda e, acc=acc: e.bn_stats(out=sm[:, 4:10], in_=acc[:, 160:224]), [acc], [sm])
                    S.op("dve", lambda e: e.bn_aggr(out=sm[:, 10:12], in_=sm[:, 4:10]), [sm], [sm])
                    rstd_col(sm[:, 13:14], sm[:, 11:12], 1.0, cc[:, 0:1], sm)
                    ts(kif[:], acc[:, 160:224], sm[:, 10:11], ALU.subtract, r=[acc, sm], w=[kif], s2=sm[:, 13:14], op1=ALU.mult)
                    tt(kif[:], kif[:], pb[:, KIW:KIW + 64], ALU.mult, r=[kif, pb], w=[kif])
                    tt(kvb[:, 64:128], kif[:], pb[:, KIB:KIB + 64], ALU.add, r=[kif, pb], w=[kvb])
                    ts(wis.ap(tt_ * 8, [[1, 8]]), acc[:, 224:232], IDX_SCALE, ALU.mult, r=[acc], w=[wis])
                    tr(ptr.ap(0, [[1, 128]], np_=64), kvb[:, 0:64], r=[kvb], w=[ptr])
                    tr(ptr.ap(128, [[1, 128]], np_=64), kvb[:, 64:128], r=[kvb], w=[ptr])
                    acopy(KA.ap(gt * 128, [[1, 128]], np_=64), ptr.ap(0, [[1, 128]], np_=64), r=[ptr], w=[KA])
                    acopy(KIN.ap(gt * 128, [[1, 128]], np_=64), ptr.ap(128, [[1, 128]], np_=64), r=[ptr], w=[KIN])
                    mm(bk4[:, 128:160], pk[:, TRI:TRI + 128], a4.ap(tt_ * 32, [[1, 32]]), r=[pk, a4], w=[acs_dep])
                    mm(bk4[:, 160:192], pk[:, ONES:ONES + 128], a4.ap(tt_ * 32, [[1, 32]]), r=[pk, a4], w=[acs_dep])
                    acopy(acs_sb[:], bk4[:, 128:160], r=[acs_dep], w=[acs_sb])
                    act(eacs4.ap(tt_ * 32, [[1, 32]]), bk4[:, 128:160], AF.Exp, r=[acs_dep], w=[eacs4])
                    act(cdb4.ap(tt_ * 32, [[1, 32]]), bk4[:, 160:192], AF.Exp, r=[acs_dep], w=[cdb4])
                    tt(dd[:], bk4[:, 160:192], acs_sb[:], ALU.subtract, r=[acs_dep, acs_sb], w=[dd])
                    act(dd[:], dd[:], AF.Exp, r=[dd], w=[dd])
                    tt(dtd4.ap(tt_ * 32, [[1, 32]]), dt4.ap(tt_ * 32, [[1, 32]]), dd[:], ALU.mult, r=[dt4, dd], w=[dtd4])
                if seq == 0 and stl == 0:
                    dump("dt4", dt4, dt4[:], [128, 4, 32])
                    dump("KA", KA, KA[:], [128, SEQ])
                    dump("KIN", KIN, KIN[:], [128, SEQ])
                    dump("VA", VA, VA[:], [128, 16, 66])
                    dump("wis", wis, wis[:], [128, 4, 8])
                    dump("eacs4", eacs4, eacs4[:], [128, 4, 32])
                    dump("dtd4", dtd4, dtd4[:], [128, 4, 32])

                S.barrier(("pe", "act", "dve", "sp", "pool"))
                with ExitStack() as ph:
                    Upre = T("Upre", [128, 4, 515], F32, stack=ph)
                    Upre_d = [Dep("Upre%d" % i) for i in range(4)]
                    cv = T("cv", [128, 4, 512], F32, stack=ph)
                    cv_d = [Dep("cv%d" % i) for i in range(4)]
                    thR = Ring([T("th%d" % i, [128, 512], F32, stack=ph) for i in range(2)])
                    xbP = [T("xb%d" % i, [128, 4, 512], BF16, stack=ph) for i in range(2)]
                    xb_d = [[Dep("xb%d_%d" % (i, f)) for f in range(4)] for i in range(2)]
                    zsP = [T("zs%d" % i, [128, 4, 256], F32, stack=ph) for i in range(2)]
                    xtkP = [T("xtk%d" % i, [128, 4, 384], BF16, stack=ph) for i in range(2)]
                    rhsAR = Ring([T("rhsA%d" % i, [128, 512], F32, stack=ph) for i in range(2)])
                    CBmR = Ring([T("CBm%d" % i, [128, 128], F32, stack=ph) for i in range(2)])
                    EsegR = Ring([T("Eseg%d" % i, [128, 512], BF16, stack=ph) for i in range(2)])
                    MTR = Ring([T("MT%d" % i, [128, 512], BF16, stack=ph) for i in range(2)])
                    xcR = Ring([T("xc%d" % i, [128, 256], BF16, stack=ph) for i in range(2)])
                    xcdR = Ring([T("xcd%d" % i, [128, 256], BF16, stack=ph) for i in range(2)])
                    xsDR = Ring([T("xsD%d" % i, [128, 256], F32, stack=ph) for i in range(2)])
                    t1R = Ring([T("t1%d" % i, [128, 256], F32, stack=ph) for i in range(2)])
                    yzR = Ring([T("yz%d" % i, [128, 256], F32, stack=ph) for i in range(4)])
                    smgP = [T("smg%d" % i, [128, 12], F32, stack=ph) for i in range(2)]
                    yzs = {}
                    yjk = T("yjk", [128, 256], BF16, stack=ph)
                    yNR = Ring([T("yN%d" % i, [128, 256], BF16, stack=ph) for i in range(2)])
                    yNT = T("yNT", [128, 16, 512], BF16, stack=ph)
                    A0 = accR.tiles[0]
                    segR = Ring([bk3, bk7])
                    cbR = Ring([(0, bk4.dep)])
                    ydR = Ring([(0, bk5.dep)])
                    soR = Ring([(bk6, bk6.dep, bk6.dep), (accR.tiles[1], accR.tiles[1].dep, accR.tiles[1].dep)])
                    ptrA_d, ptrB_d = ptr.dep, [ptr.dep, ptr.dep]
                    state_done = {}

                    def stageA(g):
                        par = g % 2
                        xb, zs, xtk, xbd = xbP[par], zsP[par], xtkP[par], xb_d[par]
                        wb = wq.pop(g)
                        vcopy(Upre.ap(0, [[515, 4], [1, 3]]), halo.ap(g * 12, [[3, 4], [1, 3]]), r=[halo_deps[g]], w=Upre_d)
                        for fi in range(4):
                            for kc in range(8):
                                mm(A0[:, :], wslice(wb, 768, kc, 256 + fi * 128, 256 + (fi + 1) * 128), hT.ap(kc * 512, [[1, 512]]),
                                   start=(kc == 0), stop=(kc == 7), r=[wb, hT], w=[A0])
                            acopy(Upre.ap(fi * 515 + 3, [[1, 512]]), A0[:, :], r=[A0], w=[Upre_d[fi]])
                            yield
                        vcopy(halo.ap(g * 12, [[3, 4], [1, 3]]), Upre.ap(512, [[515, 4], [1, 3]]), r=Upre_d, w=[halo_deps[g]])
                        for fi in range(4):
                            ct = g * 4 + fi
                            cvf = cv.ap(fi * 512, [[1, 512]])
                            act(cvf, Upre.ap(fi * 515, [[1, 512]]), AF.Identity, r=[Upre_d[fi], pc], w=[cv_d[fi]],
                                bias=pc[:, 128 + ct:129 + ct], scale=pc[:, ct * 4:ct * 4 + 1])
                            yield
                            for k in range(1, 4):
                                stt(cvf, Upre.ap(fi * 515 + k, [[1, 512]]), pc[:, ct * 4 + k:ct * 4 + k + 1], cvf, ALU.mult, ALU.add,
                                    r=[Upre_d[fi], pc, cv_d[fi]], w=[cv_d[fi]])
                                yield
                            th = thR.next()
                            act(th[:], cvf, AF.Tanh, r=[cv_d[fi]], w=[th])
                            stt(xb.ap(fi * 512, [[1, 512]]), th[:], 1.0, cvf, ALU.add, ALU.mult, r=[th, cv_d[fi]], w=[xbd[fi]])
                            yield
                        for c in range(4):
                            for kc in range(8):
                                mm(A0[:, 0:256], hT.ap(kc * 512 + c * 128, [[1, 128]]), wslice(wb, 768, kc, 0, 256),
                                   start=(kc == 0), stop=(kc == 7), r=[hT, wb], w=[A0])
                            th = thR.next()
                            act(th[:, 0:256], A0[:, 0:256], AF.Tanh, r=[A0], w=[th], scale=0.5)
                            stt(zs.ap(c * 256, [[1, 256]]), th[:, 0:256], 1.0, A0[:, 0:256], ALU.add, ALU.mult, r=[th, A0], w=[zs])
                            yield
                        for c in range(4):
                            for fi in range(3):
                                tr(ptr.ap(fi * 128, [[1, 128]]), xb.ap(fi * 512 + c * 128, [[1, 128]]), r=[xbd[fi]], w=[ptrA_d])
                            acopy(xtk.ap(c * 384, [[1, 384]]), ptr.ap(0, [[1, 384]]), r=[ptrA_d], w=[xtk])
                            yield

                    def chunkB(g, c):
                        par = g % 2
                        xb, zs, xtk, xbd = xbP[par], zsP[par], xtkP[par], xb_d[par]
                        hsl = c * 32 + g * 4
                        rhsA, CBm, Eseg, MT = rhsAR.next(), CBmR.next(), EsegR.next(), MTR.next()
                        xc, xcd, xsD, t1, yz = xcR.next(), xcdR.next(), xsDR.next(), t1R.next(), yzR.next()
                        smg = smgP[g % 2]
                        seg = segR.next()
                        cbo, cbd = cbR.next()
                        ydo, ydd = ydR.next()
                        sob, stsd, yofd = soR.next()
                        tt(rhsA.ap(0, [[128, 4], [1, 128]]), pk.ap(TRI, [[0, 4], [1, 128]]), a4.ap(hsl, [[1, 4], [0, 128]]),
                           ALU.mult, r=[pk, a4], w=[rhsA], eng="pool")
                        xs3 = xtk.ap(c * 384, [[64, 4], [1, 64]])
                        tt(xc.ap(0, [[64, 4], [1, 64]]), xs3, dt4.ap(hsl, [[1, 4], [0, 64]]), ALU.mult, r=[xtk, dt4], w=[xc], eng="pool")
                        tt(xcd.ap(0, [[64, 4], [1, 64]]), xs3, dtd4.ap(hsl, [[1, 4], [0, 64]]), ALU.mult, r=[xtk, dtd4], w=[xcd], eng="pool")
                        tt(xsD.ap(0, [[64, 4], [1, 64]]), xs3, pb.ap(DSK + g * 4, [[1, 4], [0, 64]]), ALU.mult, r=[xtk, pb], w=[xsD], eng="pool")
                        yield
                        mm(seg[:, :], pk[:, USTR:USTR + 128], rhsA[:, :], r=[pk, rhsA], w=[seg])
                        mm(bk4[:, cbo:cbo + 128], xb.ap(2 * 512 + c * 128, [[1, 128]]), xb.ap(3 * 512 + c * 128, [[1, 128]]),
                           r=[xbd[2], xbd[3]], w=[cbd])
                        tt(CBm[:], bk4[:, cbo:cbo + 128], pk[:, TRI:TRI + 128], ALU.mult, r=[cbd, pk], w=[CBm])
                        act(Eseg[:], seg[:, :], AF.Exp, r=[seg], w=[Eseg])
                        yield
                        tt(MT.ap(0, [[128, 4], [1, 128]]), Eseg.ap(0, [[128, 4], [1, 128]]), CBm.ap(0, [[0, 4], [1, 128]]),
                           ALU.mult, r=[Eseg, CBm], w=[MT])
                        yield
                        while c > 0 and not state_done.get((g, c - 1)):
                            yield
                        mm(sob[:, 256:512], xb.ap(3 * 512 + c * 128, [[1, 128]]), Sb.ap(g * 256, [[1, 256]]),
                           r=[xbd[3], Sb_deps[g]], w=[yofd])
                        for j in range(4):
                            mm(bk5[:, ydo + j * 64:ydo + (j + 1) * 64], MT[:, j * 128:(j + 1) * 128], xc[:, j * 64:(j + 1) * 64],
                               start=True, stop=True, r=[MT, xc], w=[ydd])
                        mm(sob[:, 0:256], xtk.ap(c * 384 + 256, [[1, 128]]), xcd[:], r=[xtk, xcd], w=[stsd])
                        tt(St.ap(g * 256, [[64, 4], [1, 64]]), St.ap(g * 256, [[64, 4], [1, 64]]),
                           cdb4.ap(hsl, [[1, 4], [0, 64]]), ALU.mult, r=[St_deps[g], cdb4], w=[St_deps[g]])
                        tt(St.ap(g * 256, [[1, 256]]), St.ap(g * 256, [[1, 256]]), sob[:, 0:256], ALU.add,
                           r=[St_deps[g], stsd], w=[St_deps[g]])
                        acopy(Sb.ap(g * 256, [[1, 256]]), St.ap(g * 256, [[1, 256]]), r=[St_deps[g]], w=[Sb_deps[g]])
                        state_done[(g, c)] = True
                        tt(t1.ap(0, [[64, 4], [1, 64]]), sob.ap(256, [[64, 4], [1, 64]]), eacs4.ap(hsl, [[1, 4], [0, 64]]),
                           ALU.mult, r=[yofd, eacs4], w=[t1])
                        tt(t1[:], bk5[:, ydo:ydo + 256], t1[:], ALU.add, r=[ydd, t1], w=[t1])
                        tt(t1[:], xsD[:], t1[:], ALU.add, r=[xsD, t1], w=[t1])
                        yield
                        tt(yz[:], t1[:], zs.ap(c * 256, [[1, 256]]), ALU.mult, r=[t1, zs], w=[yz])
                        act(yjk[:], yz[:], AF.Square, r=[yz], w=[yjk, smg], accum=smg[:, c:c + 1])
                        yzs[(g, c)] = yz
                        yield

                    def normB(g):
                        smg = smgP[g % 2]
                        act(smg[:, 4:8], smg[:, 0:4], AF.Sqrt, r=[smg, cc], w=[smg], bias=cc[:, 3:4], scale=1.0 / 256)
                        recip(smg[:, 8:12], smg[:, 4:8], r=[smg], w=[smg])
                        for c in range(4):
                            yz = yzs.pop((g, c))
                            yN = yNR.next()
                            stt(yN[:], yz[:], smg[:, 8 + c:9 + c], pb[:, SNW + g * 256:SNW + (g + 1) * 256], ALU.mult, ALU.mult,
                                r=[yz, smg, pb], w=[yN])
                            for i in range(2):
                                tr(ptr.ap(512 + (c % 2) * 256 + i * 128, [[1, 128]]), yN[:, i * 128:(i + 1) * 128], r=[yN], w=[ptrB_d[c % 2]])
                            acopy(yNT.ap((g * 2) * 512 + c * 128, [[512, 2], [1, 128]]), ptr.ap(512 + (c % 2) * 256, [[128, 2], [1, 128]]),
                                  r=[ptrB_d[c % 2]], w=[yNT])

                    def run_group(g):
                        if g + 2 < 8:
                            wq[g + 2] = load_w(win_v, C_GRP + (g + 2) * 768, 768)
                        pending = [chunkB(g, c) for c in range(4)]
                        active = [pending.pop(0), pending.pop(0)]
                        ag = stageA(g + 1) if g < 7 else None
                        while active or ag is not None:
                            for g_ in list(active):
                                try:
                                    next(g_)
                                except StopIteration:
                                    active.remove(g_)
                                    if pending:
                                        active.append(pending.pop(0))
                            for _rep in range(2):
                                if ag is not None:
                                    try:
                                        next(ag)
                                    except StopIteration:
                                        ag = None

                    wq = {0: pre_w.pop(), 1: load_w(win_v, C_GRP + 768, 768)}
                    for _ in stageA(0):
                        pass
                    for g in range(8):
                        run_group(g)
                        normB(g)
                    if seq == 0 and stl == 0:
                        dump("yNT", yNT, yNT[:], [128, 16, 512])
                    obanks = [accR.tiles[0], accR.tiles[1], bk3, bk7]
                    for half in range(2):
                        for kh in range(2):
                            wb = wring.next()
                            dma("pool", wb.ap(0, [[512, 8], [1, 512]]), wso_v[:, kh * 8:(kh + 1) * 8, half * 512:(half + 1) * 512], w=[wb])
                            for tt_ in range(4):
                                for kc in range(8):
                                    mm(obanks[tt_][:, :], yNT.ap((kh * 8 + kc) * 512 + tt_ * 128, [[1, 128]]), wslice(wb, 512, kc, 0, 512),
                                       start=(kh == 0 and kc == 0), stop=(kh == 1 and kc == 7), r=[yNT, wb], w=[obanks[tt_]])
                        for tt_ in range(4):
                            acopy(yssm.ap(tt_ * 1024 + half * 512, [[1, 512]]), obanks[tt_][:, :], r=[obanks[tt_]], w=[yssm])
                if seq == 0 and stl == 0:
                    dump("yssm", yssm, yssm[:], [128, 4, 1024])

                S.barrier()
                with ExitStack() as ph:
                    QA = T("QA", [128, 16, 512], BF16, stack=ph)
                    QI = T("QI", [128, 8, 512], BF16, stack=ph)
                    zA = T("zA", [128, 4, 1024], BF16, stack=ph)
                    isc = T("isc", [128, SEQ], F32, stack=ph)
                    rlR = Ring([T("rl%d" % i, [128, 512], F32, stack=ph) for i in range(2)])
                    maskb = T("maskb", [128, SEQ], BF16, stack=ph)
                    maskTP = [T("maskT%d" % i, [128, 16, 128], BF16, stack=ph) for i in range(2)]
                    ER = Ring([T("E%d" % i, [128, 512], BF16, stack=ph) for i in range(5)])
                    PR = Ring([T("P%d" % i, [128, 512], BF16, stack=ph) for i in range(5)])
                    og = T("og", [128, 1024], BF16, stack=ph)
                    Lm = T("Lm", [128, 512], F32, stack=ph)
                    OTs = T("OTs", [128, 512], F32, stack=ph)
                    thA = Lm
                    oT = T("oT", [128, 8, 512], BF16, stack=ph)
                    bs = T("bs", [128, 4], F32, stack=ph)
                    rc = T("rc", [128, 4], F32, stack=ph)
                    bu = T("bu", [128, 2], U32, stack=ph)
                    LR = Ring([bk3, accR.tiles[1], bk6, bk7])
                    A0 = accR.tiles[0]
                    A1 = accR.tiles[1]
                    Ob = [bk4, bk5, bk4, bk5]
                    Odeps = [Ob[j].dep for j in range(4)]
                    ptrA_d = ptrB_d = ptr.dep

                    dma("sp", QA.ap(0, [[512, 16], [1, 512]], p0=64, np_=5), qaug_d[:, :, t0:t0 + 512], w=[QA])
                    wb = load_w(win_v, C_QI, 512)
                    for h in range(8):
                        for kc in range(8):
                            mm(A0[0:64, :], wslice(wb, 512, kc, h * 64, (h + 1) * 64), hT.ap(kc * 512, [[1, 512]]),
                               start=(kc == 0), stop=(kc == 7), r=[wb, hT], w=[A0])
                        acopy(QI.ap(h * 512, [[1, 512]], np_=64), A0[0:64, :], r=[A0], w=[QI])

                    def stageI(tt_):
                        qb = stl * 4 + tt_
                        SL = (qb + 1) * 128
                        maskT = maskTP[tt_ % 2]
                        for c4 in range((SL + 511) // 512):
                            w_ = min(512, SL - c4 * 512)
                            for h in range(8):
                                mm(A0[:, 0:w_], QI.ap(h * 512 + tt_ * 128, [[1, 128]], np_=64), KIN.ap(c4 * 512, [[1, w_]], np_=64),
                                   r=[QI, KIN], w=[A0])
                                rl = rlR.next()
                                act(rl[:, 0:w_], A0[:, 0:w_], AF.Relu, r=[A0], w=[rl])
                                wcol = wis.ap(tt_ * 8 + h, [[1, 1]])
                                if h == 0:
                                    ts(isc[:, c4 * 512:c4 * 512 + w_], rl[:, 0:w_], wcol, ALU.mult, r=[rl, wis], w=[isc])
                                else:
                                    stt(isc[:, c4 * 512:c4 * 512 + w_], rl[:, 0:w_], wcol, isc[:, c4 * 512:c4 * 512 + w_],
                                        ALU.mult, ALU.add, r=[rl, wis, isc], w=[isc])
                                yield
                        if qb >= 2:
                            S.op("dve", lambda e, SL=SL, bs=bs, isc=isc: e.tensor_reduce(out=bs[:, 1:2], in_=isc[:, 0:SL], axis=AX.X, op=ALU.max), [isc], [bs])
                            S.op("dve", lambda e, SL=SL, bs=bs, isc=isc: e.tensor_reduce(out=bs[:, 0:1], in_=isc[:, 0:SL], axis=AX.X, op=ALU.min), [isc], [bs])
                            ts(bs[:, 1:2], bs[:, 1:2], 1.0, ALU.add, r=[bs], w=[bs])
                        tt(isc[:, SL - 128:SL], isc[:, SL - 128:SL], pk[:, NEGM:NEGM + 128], ALU.add, r=[isc, pk], w=[isc])
                        yield
                        if qb >= 2:
                            for it in range(NIT):
                                ts(bs[:, 2:3], bs[:, 0:1], bs[:, 1:2], ALU.add, r=[bs], w=[bs], s2=0.5, op1=ALU.mult)
                                ts(maskb[:, 0:SL], isc[:, 0:SL], bs[:, 2:3], ALU.is_ge, r=[isc, bs], w=[maskb, bs],
                                   s2=None, op1=ALU.add, accum=bs[:, 3:4])
                                yield
                                ts(bu[:, 0:1], bs[:, 3:4], TOPK - 0.5, ALU.is_ge, r=[bs], w=[bu])
                                ts(bu[:, 1:2], bs[:, 3:4], TOPK - 0.5, ALU.is_lt, r=[bs], w=[bu])
                                S.op("dve", lambda e, bs=bs, bu=bu: e.copy_predicated(out=bs[:, 0:1], mask=bu[:, 0:1], data=bs[:, 2:3]), [bs, bu], [bs])
                                S.op("dve", lambda e, bs=bs, bu=bu: e.copy_predicated(out=bs[:, 1:2], mask=bu[:, 1:2], data=bs[:, 2:3]), [bs, bu], [bs])
                                yield
                            thr = bs[:, 0:1]
                        else:
                            thr = cc[:, 2:3]
                        ts(maskb[:, 0:SL], isc[:, 0:SL], thr, ALU.is_ge, r=[isc, bs, cc], w=[maskb])
                        if seq == 0 and stl == 0 and tt_ == 3:
                            dump("isc", isc, isc[:, 0:512], [128, 512])
                            dump("bs", bs, bs[:], [128, 4])
                        for k0 in range(0, qb + 1, 4):
                            nk = min(4, qb + 1 - k0)
                            for kb in range(k0, k0 + nk):
                                tr(ptr.ap((kb - k0) * 128, [[1, 128]]), maskb[:, kb * 128:(kb + 1) * 128], r=[maskb], w=[ptrA_d])
                            acopy(maskT.ap(k0 * 128, [[1, nk * 128]]), ptr.ap(0, [[1, nk * 128]]), r=[ptrA_d], w=[maskT])
                            yield

                    def stageAT(tt_):
                        qb = stl * 4 + tt_
                        maskT = maskTP[tt_ % 2]
                        def kb_lo(hg):
                            smin = 2.0 ** (-8.0 * (hg * 4 + 4) / 16.0)
                            dskip = int(np.ceil((60.0 / smin + 127.0) / 128.0))
                            return max(0, qb - dskip + 1)
                        steps = [(hg, kb) for hg in range(4) for kb in range(kb_lo(hg), qb + 1)]
                        Ps = {}

                        def front(i):
                            hg, kb = steps[i]
                            L = LR.next()
                            mm(L[:, :], KA.ap(kb * 128, [[1, 128]], np_=69),
                               QA.ap(hg * 4 * 512 + tt_ * 128, [[512, 4], [1, 128]], np_=69), r=[KA, QA], w=[L])
                            E = ER.next()
                            if kb == qb:
                                tt(Lm.ap(0, [[128, 4], [1, 128]]), L.ap(0, [[128, 4], [1, 128]]),
                                   pk.ap(NEGT, [[0, 4], [1, 128]]), ALU.add, r=[L, pk], w=[Lm])
                                act(E[:], Lm[:], AF.Exp, r=[Lm], w=[E])
                            else:
                                act(E[:], L[:, :], AF.Exp, r=[L], w=[E])
                            P = PR.next()
                            tt(P.ap(0, [[128, 4], [1, 128]]), E.ap(0, [[128, 4], [1, 128]]),
                               maskT.ap(kb * 128, [[0, 4], [1, 128]]), ALU.mult, r=[E, maskT], w=[P],
                               eng=("pool" if i % 2 == 0 else "dve"))
                            Ps[i] = P

                        def back(i):
                            hg, kb = steps[i]
                            P = Ps.pop(i)
                            mm(Ob[hg][0:66, :], VA.ap(kb * 66, [[1, 66]]), P[:, :], start=(kb == kb_lo(hg)), stop=(kb == qb),
                               r=[VA, P], w=[Odeps[hg]])
                            if kb == qb:
                                acopy(OTs[0:66, :], Ob[hg][0:66, :], r=[Odeps[hg]], w=[OTs])
                                for j in range(4):
                                    S.op("pe", lambda e, j=j, hg=hg, OTs=OTs: e.transpose(out=Ob[hg][:, j * 66:(j + 1) * 66],
                                                                                         in_=OTs[0:66, j * 128:(j + 1) * 128],
                                                                                         identity=pk[0:66, IDENT:IDENT + 66]),
                                         [OTs, pk], [Odeps[hg]])
                                for j in range(4):
                                    h = hg * 4 + j
                                    recip(rc[:, j:j + 1], Ob[hg][:, j * 66 + 64:j * 66 + 65], r=[Odeps[hg]], w=[rc])
                                    stt(og[:, h * 64:(h + 1) * 64], Ob[hg][:, j * 66:j * 66 + 64], rc[:, j:j + 1],
                                        zA.ap(tt_ * 1024 + h * 64, [[1, 64]]), ALU.mult, ALU.mult, r=[Odeps[hg], rc, zA], w=[og])

                        LA = 3
                        for i in range(min(LA, len(steps))):
                            front(i)
                        for i in range(len(steps)):
                            if i + LA < len(steps):
                                front(i + LA)
                            back(i)
                            yield
                        for kc in range(8):
                            tr(ptr.ap(512 + (kc % 4) * 128, [[1, 128]]), og[:, kc * 128:(kc + 1) * 128], r=[og], w=[ptrB_d])
                            if kc % 4 == 3:
                                acopy(oT.ap((kc - 3) * 512 + tt_ * 128, [[512, 4], [1, 128]]), ptr.ap(512, [[128, 4], [1, 128]]),
                                      r=[ptrB_d], w=[oT])
                        yield

                    def stageProj():
                        for half in range(2):
                            wb = load_w(win_v, C_Q + half * 512, 512)
                            for hh in range(8):
                                h = half * 8 + hh
                                for kc in range(8):
                                    mm(A1[0:64, :], wslice(wb, 512, kc, hh * 64, (hh + 1) * 64), hT.ap(kc * 512, [[1, 512]]),
                                       start=(kc == 0), stop=(kc == 7), r=[wb, hT], w=[A1])
                                S.op("act", lambda e, h=h, QA=QA, A1=A1: e.mul(QA.ap(h * 512, [[1, 512]], np_=64), A1[0:64, :], 0.125), [A1], [QA])
                                yield
                        for half in range(2):
                            wb = load_w(win_v, C_AZ + half * 512, 512)
                            for tt_ in range(4):
                                for kc in range(8):
                                    mm(A1[:, :], hT.ap(kc * 512 + tt_ * 128, [[1, 128]]), wslice(wb, 512, kc, 0, 512),
                                       start=(kc == 0), stop=(kc == 7), r=[hT, wb], w=[A1])
                                act(thA[:], A1[:, :], AF.Tanh, r=[A1], w=[thA], scale=0.5)
                                stt(zA.ap(tt_ * 1024 + half * 512, [[1, 512]]), thA[:], 1.0, A1[:, :], ALU.add, ALU.mult,
                                    r=[thA, A1], w=[zA])
                                yield

                    interleave(stageI(0), stageProj())
                    if seq == 0 and stl == 0:
                        dump("QA", QA, QA[:], [128, 16, 512])
                        dump("QI", QI, QI[:], [128, 8, 512])
                    for tt_ in range(4):
                        interleave(stageAT(tt_), stageI(tt_ + 1) if tt_ < 3 else iter(()))
                    if seq == 0 and stl == 0:
                        dump("oT", oT, oT[:], [128, 8, 512])
                    for half in range(2):
                        wb = load_w(wao_v, half * 512, 512)
                        for tt_ in range(4):
                            acc = accR.next()
                            for kc in range(8):
                                mm(acc[:, :], oT.ap(kc * 512 + tt_ * 128, [[1, 128]]), wslice(wb, 512, kc, 0, 512),
                                   start=(kc == 0), stop=(kc == 7), r=[oT, wb], w=[acc])
                            acopy(yattn.ap(tt_ * 1024 + half * 512, [[1, 512]]), acc[:, :], r=[acc], w=[yattn])
                if seq == 0 and stl == 0:
                    dump("yattn", yattn, yattn[:], [128, 4, 1024])

                S.barrier()
                with ExitStack() as ph:
                    gtR = Ring([T("gt%d" % i, [128, 512], F32, stack=ph) for i in range(2)])
                    gsR = Ring([T("gs%d" % i, [128, 512], F32, stack=ph) for i in range(2)])
                    mg = T("mg", [128, 4, 1024], F32, stack=ph)
                    mb = T("mb", [128, 1024], BF16, stack=ph)
                    mT = T("mT", [128, 8, 512], BF16, stack=ph)
                    r4 = T("r4", [128, 4, 1024], F32, stack=ph)
                    roR = Ring([T("ro%d" % i, [128, 1024], F32, stack=ph) for i in range(2)])
                    p5 = T("p5", [128, NPB - NRES], F32, stack=ph)
                    dma("sp", p5[:], pb_d[:, NRES:NPB], w=[p5])
                    for u in range(4):
                        wb = load_w(win_v, C_GATE + u * 512, 512)
                        for tt_ in range(4):
                            acc = accR.next()
                            for kc in range(8):
                                mm(acc[:, :], hT.ap(kc * 512 + tt_ * 128, [[1, 128]]), wslice(wb, 512, kc, 0, 512),
                                   start=(kc == 0), stop=(kc == 7), r=[hT, wb], w=[acc])
                            gtt = gtR.next()
                            tt(gtt[:], acc[:, :], p5[:, u * 512:(u + 1) * 512], ALU.add, r=[acc, p5], w=[gtt])
                            gs = gsR.next()
                            act(gs[:], gtt[:], AF.Tanh, r=[gtt], w=[gs], scale=0.5)
                            if u < 2:
                                stt(mg.ap(tt_ * 1024 + u * 512, [[1, 512]]), gs[:], 1.0, yssm.ap(tt_ * 1024 + u * 512, [[1, 512]]),
                                    ALU.add, ALU.mult, r=[gs, yssm], w=[mg])
                            else:
                                stt(gs[:], gs[:], 1.0, yattn.ap(tt_ * 1024 + (u - 2) * 512, [[1, 512]]), ALU.add, ALU.mult,
                                    r=[gs, yattn], w=[gs])
                                tt(mg.ap(tt_ * 1024 + (u - 2) * 512, [[1, 512]]), mg.ap(tt_ * 1024 + (u - 2) * 512, [[1, 512]]),
                                   gs[:], ALU.add, r=[mg, gs], w=[mg])
                    for tt_ in range(4):
                        S.op("act", lambda e, tt_=tt_, mb=mb, mg=mg: e.mul(mb[:], mg.ap(tt_ * 1024, [[1, 1024]]), 0.5), [mg], [mb])
                        for kc in range(8):
                            tr(ptr.ap(kc * 128, [[1, 128]]), mb[:, kc * 128:(kc + 1) * 128], r=[mb], w=[ptr])
                        acopy(mT.ap(tt_ * 128, [[512, 8], [1, 128]]), ptr.ap(0, [[128, 8], [1, 128]]), r=[ptr], w=[mT])
                    for half in range(2):
                        wb = load_w(wout_v, half * 512, 512)
                        for tt_ in range(4):
                            acc = accR.next()
                            for kc in range(8):
                                mm(acc[:, :], mT.ap(kc * 512 + tt_ * 128, [[1, 128]]), wslice(wb, 512, kc, 0, 512),
                                   start=(kc == 0), stop=(kc == 7), r=[mT, wb], w=[acc])
                            acopy(r4.ap(tt_ * 1024 + half * 512, [[1, 512]]), acc[:, :], r=[acc], w=[r4])
                    for tt_ in range(4):
                        xt = xring.next()
                        dma("sp", xt[:], x_d[seq, t0 + tt_ * 128:t0 + (tt_ + 1) * 128, :], w=[xt])
                        r4s = r4.ap(tt_ * 1024, [[1, 1024]])
                        tt(r4s, r4s, xt[:], ALU.add, r=[r4, xt], w=[r4])
                        act(mb[:], r4s, AF.Square, r=[r4], w=[mb, sm], accum=sm[:, 0:1])
                        rstd_col(sm[:, 2:3], sm[:, 0:1], 1.0 / D_MODEL, cc[:, 0:1], sm)
                        ro = roR.next()
                        stt(ro[:], r4s, sm[:, 2:3], p5[:, FNW - NRES:FNW - NRES + 1024], ALU.mult, ALU.mult, r=[r4, sm, p5], w=[ro])
                        dma("sp", out_d[seq, t0 + tt_ * 128:t0 + (tt_ + 1) * 128, :], ro[:], r=[ro], w=[ro])
        S.final_wait("sp")
        S.emit()
    return nc, dbg_outs


def host_prep(inputs):
    f32 = np.float32
    w_in = np.asarray(inputs["w_in"], f32)[0]
    O_Z, O_XBC, O_DT, O_Q, O_K, O_V, O_AZ, O_QI, O_KI, O_WI, O_G = 0, 2048, 6144, 6176, 7200, 7264, 7328, 8352, 8864, 8928, 8936
    cols = []
    cols += list(range(O_DT, O_DT + 32)) + list(range(O_K, O_K + 64)) + list(range(O_V, O_V + 64))
    cols += list(range(O_KI, O_KI + 64)) + list(range(O_WI, O_WI + 8))
    for g in range(8):
        cols += list(range(O_Z + g * 256, O_Z + (g + 1) * 256))
        cols += list(range(O_XBC + g * 256, O_XBC + (g + 1) * 256))
        cols += list(range(O_XBC + 2048 + g * 128, O_XBC + 2048 + (g + 1) * 128))
        cols += list(range(O_XBC + 3072 + g * 128, O_XBC + 3072 + (g + 1) * 128))
    cols += list(range(O_Q, O_Q + 1024)) + list(range(O_QI, O_QI + 512)) + list(range(O_AZ, O_AZ + 1024))
    cols += list(range(O_G, O_G + 2048))
    cols = np.asarray(cols)
    assert cols.shape[0] == IN_TOTAL and np.unique(cols).shape[0] == IN_TOTAL
    win = np.ascontiguousarray(w_in[:, cols])

    pb = np.zeros((128, NPB), f32)

    def put(off, v):
        v = np.asarray(v, f32).reshape(-1)
        pb[:, off:off + v.shape[0]] = v[None, :]
    put(NW, inputs["norm_w"][0])
    put(FNW, inputs["final_norm_w"])
    put(GB, inputs["gate_b"][0])
    put(SNW, inputs["ssm_norm_w"][0])
    put(DTB, inputs["dt_bias"][0])
    put(ALOG, inputs["a_log"][0])
    put(DSK, inputs["d_skip"][0])
    put(KIW, inputs["idx_k_norm_w"][0])
    put(KIB, inputs["idx_k_norm_b"][0])

    conv_w = np.asarray(inputs["conv_w"], f32)[0]
    conv_b = np.asarray(inputs["conv_b"], f32)[0]
    pc = np.zeros((128, 160), f32)
    for g in range(8):
        for fi in range(4):
            ct = g * 4 + fi
            if fi < 2:
                ch0 = g * 256 + fi * 128
            elif fi == 2:
                ch0 = 2048 + g * 128
            else:
                ch0 = 3072 + g * 128
            pc[:, ct * 4:(ct + 1) * 4] = conv_w[:, ch0:ch0 + 128].T
            pc[:, 128 + ct] = conv_b[ch0:ch0 + 128]

    pk = np.zeros((128, NPK), f32)
    i = np.arange(128)
    pk[:, IDENT:IDENT + 128] = np.eye(128, dtype=f32)
    pk[:, TRI:TRI + 128] = (i[:, None] <= i[None, :]).astype(f32)
    pk[:, USTR:USTR + 128] = (i[:, None] > i[None, :]).astype(f32)
    pk[:, NEGM:NEGM + 128] = np.where(i[None, :] > i[:, None], f32(-1e30), f32(0))
    pk[:, ONES:ONES + 128] = 1.0
    pk[:, NEGT:NEGT + 128] = np.where(i[:, None] > i[None, :], f32(-1e30), f32(0))

    bf = ml_dtypes.bfloat16
    slopes = np.exp2(-8.0 * np.arange(1, 17, dtype=np.float64) / 16).astype(f32)
    s_hi = slopes.astype(bf)
    s_lo = (slopes - s_hi.astype(f32)).astype(bf)
    spos = np.arange(SEQ)
    kaug = np.zeros((5, SEQ), f32)
    kaug[0] = 1.0
    kaug[1] = spos % 128
    kaug[2] = (spos // 128) * 128
    kaug[3] = spos % 128
    kaug[4] = (spos // 128) * 128
    kaug = kaug.astype(bf)
    qaug = np.zeros((5, 16, SEQ), f32)
    qaug[0] = -(slopes[:, None].astype(np.float64) * spos[None, :]).astype(f32)
    qaug[1] = s_hi.astype(f32)[:, None]
    qaug[2] = s_hi.astype(f32)[:, None]
    qaug[3] = s_lo.astype(f32)[:, None]
    qaug[4] = s_lo.astype(f32)[:, None]
    qaug = qaug.astype(bf)
    shared = {
        "win": win,
        "wso": np.ascontiguousarray(np.asarray(inputs["w_ssm_out"], f32)[0]),
        "wao": np.ascontiguousarray(np.asarray(inputs["w_attn_out"], f32)[0]),
        "wout": np.ascontiguousarray(np.asarray(inputs["w_out"], f32)[0]),
        "pb": pb, "pc": pc, "pk": pk, "kaug": kaug, "qaug": qaug,
    }
    return shared


_CACHE = {}


def kernel(**inputs):
    x = np.asarray(inputs["x"], np.float32)
    shared = host_prep(inputs)
    if "nc" not in _CACHE:
        _CACHE["nc"] = build_program(2)[0]
    nc = _CACHE["nc"]
    in_maps = []
    for c in range(8):
        m = dict(shared)
        m["x"] = np.ascontiguousarray(x[2 * c:2 * c + 2])
        in_maps.append(m)
    res = run_bass_kernel_spmd(nc, in_maps, core_ids=list(range(8)))
    out = np.concatenate([np.asarray(r["out"], np.float32) for r in res.results], axis=0)
    return out
```

```python
import numpy as np
import ml_dtypes
from contextlib import ExitStack
import concourse.bass as bass
import concourse.mybir as mybir
from concourse.bass_utils import run_bass_kernel_spmd

F32 = mybir.dt.float32
BF16 = mybir.dt.bfloat16
U32 = mybir.dt.uint32
AF = mybir.ActivationFunctionType
ALU = mybir.AluOpType
AX = mybir.AxisListType

D_MODEL = 1024
SEQ = 2048
IN_TOTAL = 10984
NIT = 13
TOPK = 256
EPS = 1e-6
IDX_SCALE = (8 ** -0.5) * (64 ** -0.5)

NW, SNW, DTB, ALOG, DSK, KIW, KIB, NRES, GB, FNW, NPB = 0, 1024, 3072, 3104, 3136, 3168, 3232, 3296, 3296, 5344, 6368
IDENT, TRI, USTR, NEGM, ONES, NEGT, NPK = 0, 128, 256, 384, 512, 640, 768
C_SMALL, C_GRP, C_Q, C_QI, C_AZ, C_GATE = 0, 232, 6376, 7400, 7912, 8936


class Dep:
    __slots__ = ("name", "w", "r")

    def __init__(self, name=""):
        self.name = name
        self.w = None
        self.r = {}


class Sched:
    ENGS = ("pe", "act", "dve", "pool", "sp")

    def __init__(self, nc, stack, n_dma_sems=24):
        self.nc = nc
        self.lists = {e: [] for e in self.ENGS}
        self.sems = {}
        self.cnt = {}
        for e in ("pe", "act", "dve", "pool"):
            self.sems[e] = stack.enter_context(nc.semaphore("s_" + e))
            self.cnt[e] = 0
        self.dma_pool = {}
        for q, n in (("sp", n_dma_sems), ("pool", 8)):
            keys = []
            for i in range(n):
                k = "d_%s_%d" % (q, i)
                self.sems[k] = stack.enter_context(nc.semaphore(k))
                self.cnt[k] = 0
                keys.append(k)
            self.dma_pool[q] = [keys, 0]
        self.seen = {e: {} for e in self.ENGS}
        self.n_ops = 0

    def _needs(self, eng, reads, writes):
        needs = {}

        def add(k, v):
            if v > needs.get(k, 0):
                needs[k] = v
        for d in reads:
            if d.w is not None and not (eng == "pe" and d.w[0] == "pe"):
                add(*d.w)
        for d in writes:
            if d.w is not None and not (eng == "pe" and d.w[0] == "pe"):
                add(*d.w)
            for k, v in d.r.items():
                if not (eng == "pe" and k == "pe"):
                    add(k, v)
        out = []
        seen = self.seen[eng]
        for k, v in needs.items():
            if seen.get(k, 0) >= v:
                continue
            seen[k] = v
            out.append((k, v))
        return out

    def op(self, eng, fn, reads=(), writes=()):
        reads = [getattr(d, "dep", d) for d in reads]
        writes = [getattr(d, "dep", d) for d in writes]
        waits = self._needs(eng, reads, writes)
        self.cnt[eng] += 1
        v = self.cnt[eng]
        self.lists[eng].append((waits, fn, eng, 1))
        for d in reads:
            d.r[eng] = v
        for d in writes:
            d.w = (eng, v)
            d.r = {}
        self.n_ops += 1

    def dma(self, q, fn, reads=(), writes=()):
        reads = [getattr(d, "dep", d) for d in reads]
        writes = [getattr(d, "dep", d) for d in writes]
        keys, idx = self.dma_pool[q]
        k = keys[idx % len(keys)]
        self.dma_pool[q][1] = idx + 1
        waits = self._needs(q, reads, writes)
        prev = self.cnt[k]
        if prev > 0 and self.seen[q].get(k, 0) < prev:
            self.seen[q][k] = prev
            waits.append((k, prev))
        self.cnt[k] += 16
        v = self.cnt[k]
        self.lists[q].append((waits, fn, k, 16))
        for d in reads:
            d.r[k] = v
        for d in writes:
            d.w = (k, v)
            d.r = {}
        self.n_ops += 1

    def barrier(self, engs=("pe", "act", "dve", "sp")):
        for e in engs:
            waits = []
            for k, v in self.cnt.items():
                if k == e or k.startswith("d_pool") or v == 0:
                    continue
                if self.seen[e].get(k, 0) < v:
                    self.seen[e][k] = v
                    waits.append((k, v))
            if waits:
                self.lists[e].append((waits, None, None, 0))

    def final_wait(self, eng):
        waits = []
        for k, v in self.cnt.items():
            if k == eng or v == 0:
                continue
            if self.seen[eng].get(k, 0) < v:
                self.seen[eng][k] = v
                waits.append((k, v))
        self.lists[eng].append((waits, None, None, 0))

    def emit(self):
        nc = self.nc
        sems = self.sems
        lists = self.lists

        def replay(e, lst):
            for waits, fn, k, inc in lst:
                for (wk, wv) in waits:
                    e.wait_ge(sems[wk], wv)
                if fn is not None:
                    fn(e).then_inc(sems[k], inc)

        with nc.Block() as block:
            @block.tensor
            def _(e):
                replay(e, lists["pe"])

            @block.scalar
            def _(e):
                replay(e, lists["act"])

            @block.vector
            def _(e):
                replay(e, lists["dve"])

            @block.gpsimd
            def _(e):
                replay(e, lists["pool"])

            @block.sync
            def _(e):
                replay(e, lists["sp"])


def build_program(n_seq=2, dbg=None):
    nc = bass.Bass("TRN2", target_bir_lowering=False)

    def dram(name, shape, dt=F32, kind="ExternalInput"):
        return nc.dram_tensor(name, shape, dt, kind=kind).ap()

    x_d = dram("x", [n_seq, SEQ, D_MODEL])
    win_d = dram("win", [D_MODEL, IN_TOTAL])
    wso_d = dram("wso", [2048, 1024])
    wao_d = dram("wao", [1024, 1024])
    wout_d = dram("wout", [1024, 1024])
    pb_d = dram("pb", [128, NPB])
    pc_d = dram("pc", [128, 160])
    pk_d = dram("pk", [128, NPK])
    kaug_d = dram("kaug", [5, SEQ], BF16)
    qaug_d = dram("qaug", [5, 16, SEQ], BF16)
    out_d = dram("out", [n_seq, SEQ, D_MODEL], kind="ExternalOutput")
    dbg_outs = {}

    win_v = win_d.rearrange("(kc p) n -> p kc n", p=128)
    wso_v = wso_d.rearrange("(kc p) n -> p kc n", p=128)
    wao_v = wao_d.rearrange("(kc p) n -> p kc n", p=128)
    wout_v = wout_d.rearrange("(kc p) n -> p kc n", p=128)

    with ExitStack() as st0:
        S = Sched(nc, st0)
        uid = [0]

        class T:
            def __init__(self, name, shape, dt, psum=False, stack=st0):
                uid[0] += 1
                nm = "%s_%d" % (name, uid[0])
                alloc = nc.psum_tensor if psum else nc.sbuf_tensor
                self.t = stack.enter_context(alloc(nm, list(shape), dt))
                self.dep = Dep(nm)
                self.row = int(np.prod(shape[1:]))

            def __getitem__(self, k):
                return self.t[k]

            def ap(self, col0, dims, p0=0, np_=128):
                return bass.AP(self.t, p0 * self.row + col0, [[self.row, np_]] + [list(d) for d in dims])

        class Ring:
            def __init__(self, tiles):
                self.tiles = tiles
                self.i = 0

            def next(self):
                t = self.tiles[self.i % len(self.tiles)]
                self.i += 1
                return t

        def mm(out, lhsT, rhs, start=True, stop=True, r=(), w=()):
            S.op("pe", lambda e: e.matmul(out, lhsT=lhsT, rhs=rhs, start=start, stop=stop), r, w)

        def tr(out, in_, r=(), w=()):
            S.op("pe", lambda e: e.transpose(out=out, in_=in_, identity=identb[:]), list(r) + [identb], w)

        def act(out, in_, func, r=(), w=(), bias=None, scale=None, accum=None):
            kw = {}
            if bias is not None:
                kw["bias"] = bias
            if scale is not None:
                kw["scale"] = scale
            if accum is not None:
                kw["accum_out"] = accum
            S.op("act", lambda e: e.activation(out=out, in_=in_, func=func, **kw), r, w)

        def acopy(out, in_, r=(), w=()):
            S.op("act", lambda e: e.copy(out=out, in_=in_), r, w)

        def tt(out, a, b, op, r=(), w=(), eng="dve"):
            S.op(eng, lambda e: e.tensor_tensor(out=out, in0=a, in1=b, op=op), r, w)

        def ts(out, a, s1, op0, r=(), w=(), s2=None, op1=None, accum=None):
            kw = {}
            if op1 is not None:
                kw["op1"] = op1
            if accum is not None:
                kw["accum_out"] = accum
            S.op("dve", lambda e: e.tensor_scalar(out=out, in0=a, scalar1=s1, scalar2=s2, op0=op0, **kw), r, w)

        def stt(out, a, s, b, op0, op1, r=(), w=()):
            S.op("dve", lambda e: e.scalar_tensor_tensor(out=out, in0=a, scalar=s, in1=b, op0=op0, op1=op1), r, w)

        def vcopy(out, in_, r=(), w=()):
            S.op("dve", lambda e: e.tensor_copy(out=out, in_=in_), r, w)

        def memset(ap, val, w=()):
            S.op("dve", lambda e: e.memset(ap, val), (), w)

        def recip(out, in_, r=(), w=()):
            S.op("dve", lambda e: e.reciprocal(out=out, in_=in_), r, w)

        def dma(q, out, in_, r=(), w=()):
            S.dma(q, lambda e: e.dma_start(out=out, in_=in_), r, w)

        def rstd_col(out_col, ssq_col, scale, eps_col, tile):
            act(ssq_col, ssq_col, AF.Sqrt, r=[tile, cc], w=[tile], bias=eps_col, scale=scale)
            recip(out_col, ssq_col, r=[tile], w=[tile])

        def interleave(*gens):
            alive = list(gens)
            while alive:
                for g_ in list(alive):
                    try:
                        next(g_)
                    except StopIteration:
                        alive.remove(g_)

        def dump(name, tile, ap, shape):
            if dbg is None or name not in dbg:
                return
            d = nc.dram_tensor("dbg_" + name, list(shape), tile.t.dtype, kind="ExternalOutput").ap()
            dbg_outs[name] = "dbg_" + name
            dma("sp", d, ap, r=[tile], w=[])

        pb = T("pb", [128, NRES], F32)
        pc = T("pc", [128, 160], F32)
        pk = T("pk", [128, NPK], F32)
        identb = T("identb", [128, 128], BF16)
        cc = T("cc", [128, 8], F32)
        Abc = T("Abc", [128, 32], F32)
        Wsm = T("Wsm", [128, 8, 232], BF16)
        KA = T("KA", [128, SEQ], BF16)
        KIN = T("KIN", [128, SEQ], BF16)
        VA = T("VA", [128, 16, 66], BF16)
        St = T("St", [128, 8, 256], F32)
        Sb = T("Sb", [128, 8, 256], BF16)
        St_deps = [Dep("St%d" % g) for g in range(8)]
        Sb_deps = [Dep("Sb%d" % g) for g in range(8)]
        halo = T("halo", [128, 32, 3], F32)
        halo_deps = [Dep("halo%d" % g) for g in range(8)]
        xring = Ring([T("xt%d" % i, [128, 1024], F32) for i in range(2)])
        hb = T("hb", [128, 1024], BF16)
        hT = T("hT", [128, 8, 512], BF16)
        sm = T("sm", [128, 16], F32)
        dt4 = T("dt4", [128, 4, 32], F32)
        a4 = T("a4", [128, 4, 32], F32)
        eacs4 = T("eacs4", [128, 4, 32], F32)
        cdb4 = T("cdb4", [128, 4, 32], F32)
        dtd4 = T("dtd4", [128, 4, 32], F32)
        wis = T("wis", [128, 4, 8], F32)
        s32 = [T("s32_%d" % i, [128, 32], F32) for i in range(4)]
        kvb = T("kvb", [128, 128], BF16)
        kif = T("kif", [128, 64], F32)
        wring = Ring([T("wb%d" % i, [128, 6144], BF16) for i in range(3)])
        yssm = T("yssm", [128, 4, 1024], BF16)
        yattn = T("yattn", [128, 4, 1024], BF16)

        accR = Ring([T("acc%d" % i, [128, 512], F32, psum=True) for i in range(2)])
        ptr = T("ptr", [128, 1024], BF16, psum=True)
        bk3 = T("bk3", [128, 512], F32, psum=True)
        bk4 = T("bk4", [128, 512], F32, psum=True)
        bk5 = T("bk5", [128, 512], F32, psum=True)
        bk6 = T("bk6", [128, 512], F32, psum=True)
        bk7 = T("bk7", [128, 512], F32, psum=True)
        cb_dep = acs_dep = bk4.dep
        sts_dep = yoff_dep = bk6.dep

        dma("sp", pb[:], pb_d[:, 0:NRES], w=[pb])
        dma("sp", pc[:], pc_d, w=[pc])
        dma("sp", pk[:], pk_d, w=[pk])
        vcopy(identb[:], pk[:, IDENT:IDENT + 128], r=[pk], w=[identb])
        memset(cc[:, 0:1], EPS, w=[cc])
        memset(cc[:, 1:2], 1.0, w=[cc])
        memset(cc[:, 2:3], -1e29, w=[cc])
        memset(cc[:, 3:4], 4.0 * EPS, w=[cc])
        p2 = T("p2", [128, NIT + 1], F32)
        for k_ in range(NIT + 1):
            memset(p2[:, k_:k_ + 1], 2.0 ** -(k_ + 1), w=[p2])
        ts(pc[:], pc[:], 0.5, ALU.mult, r=[pc], w=[pc])
        act(Abc[:], pb[:, ALOG:ALOG + 32], AF.Exp, r=[pb], w=[Abc])
        ts(Abc[:], Abc[:], -1.0, ALU.mult, r=[Abc], w=[Abc])
        memset(KA[:], 0.0, w=[KA])
        dma("sp", KA.ap(0, [[1, SEQ]], p0=64, np_=5), kaug_d, w=[KA])
        memset(VA[:], 2.0, w=[VA])
        dma("pool", Wsm[:], win_v[:, :, C_SMALL:C_SMALL + 232], w=[Wsm])

        def load_w(src_v, c0, n, nkc=8):
            wb = wring.next()
            dma("pool", wb.ap(0, [[n, nkc], [1, n]]), src_v[:, :, c0:c0 + n], w=[wb])
            return wb

        def wslice(wb, n, kc, a, b):
            return wb.ap(kc * n + a, [[1, b - a]])

        for seq in range(n_seq):
            memset(St[:], 0.0, w=St_deps)
            memset(Sb[:], 0.0, w=Sb_deps)
            memset(halo[:], 0.0, w=halo_deps)
            for stl in range(4):
                t0 = stl * 512
                pre_w = [load_w(win_v, C_GRP, 768)]
                for tt_ in range(4):
                    xt = xring.next()
                    dma("sp", xt[:], x_d[seq, t0 + tt_ * 128:t0 + (tt_ + 1) * 128, :], w=[xt])
                    act(hb[:], xt[:], AF.Square, r=[xt], w=[hb, sm], accum=sm[:, 0:1])
                    rstd_col(sm[:, 2:3], sm[:, 0:1], 1.0 / D_MODEL, cc[:, 0:1], sm)
                    stt(hb[:], xt[:], sm[:, 2:3], pb[:, NW:NW + 1024], ALU.mult, ALU.mult, r=[xt, sm, pb], w=[hb])
                    for kc in range(8):
                        tr(ptr.ap(kc * 128, [[1, 128]]), hb[:, kc * 128:(kc + 1) * 128], r=[hb], w=[ptr])
                    acopy(hT.ap(tt_ * 128, [[512, 8], [1, 128]]), ptr.ap(0, [[128, 8], [1, 128]]), r=[ptr], w=[hT])
                if seq == 0 and stl == 0:
                    dump("hT", hT, hT[:], [128, 8, 512])

                for tt_ in range(4):
                    gt = stl * 4 + tt_
                    acc = accR.next()
                    for kc in range(8):
                        mm(acc[:, 0:232], hT.ap(kc * 512 + tt_ * 128, [[1, 128]]), Wsm.ap(kc * 232, [[1, 232]]),
                           start=(kc == 0), stop=(kc == 7), r=[hT, Wsm], w=[acc])
                    x32, e32, acs_sb, dd = s32
                    tt(x32[:], acc[:, 0:32], pb[:, DTB:DTB + 32], ALU.add, r=[acc, pb], w=[x32])
                    act(e32[:], x32[:], AF.Exp, r=[x32], w=[e32])
                    act(dt4.ap(tt_ * 32, [[1, 32]]), e32[:], AF.Ln, r=[e32, cc], w=[dt4], bias=cc[:, 1:2], scale=1.0)
                    tt(a4.ap(tt_ * 32, [[1, 32]]), dt4.ap(tt_ * 32, [[1, 32]]), Abc[:], ALU.mult, r=[dt4, Abc], w=[a4])
                    acopy(kvb[:, 0:64], acc[:, 32:96], r=[acc], w=[kvb])
                    acopy(VA.ap(gt * 66, [[1, 64]]), acc[:, 96:160], r=[acc], w=[VA])
                    S.op("dve", lambda e, acc=acc: e.bn_stats(out=sm[:, 4:10], in_=acc[:, 160:224]), [acc], [sm])
                    S.op("dve", lambda e: e.bn_aggr(out=sm[:, 10:12], in_=sm[:, 4:10]), [sm], [sm])
                    rstd_col(sm[:, 13:14], sm[:, 11:12], 1.0, cc[:, 0:1], sm)
                    ts(kif[:], acc[:, 160:224], sm[:, 10:11], ALU.subtract, r=[acc, sm], w=[kif], s2=sm[:, 13:14], op1=ALU.mult)
                    tt(kif[:], kif[:], pb[:, KIW:KIW + 64], ALU.mult, r=[kif, pb], w=[kif])
                    tt(kvb[:, 64:128], kif[:], pb[:, KIB:KIB + 64], ALU.add, r=[kif, pb], w=[kvb])
                    ts(wis.ap(tt_ * 8, [[1, 8]]), acc[:, 224:232], IDX_SCALE, ALU.mult, r=[acc], w=[wis])
                    tr(ptr.ap(0, [[1, 128]], np_=64), kvb[:, 0:64], r=[kvb], w=[ptr])
                    tr(ptr.ap(128, [[1, 128]], np_=64), kvb[:, 64:128], r=[kvb], w=[ptr])
                    acopy(KA.ap(gt * 128, [[1, 128]], np_=64), ptr.ap(0, [[1, 128]], np_=64), r=[ptr], w=[KA])
                    acopy(KIN.ap(gt * 128, [[1, 128]], np_=64), ptr.ap(128, [[1, 128]], np_=64), r=[ptr], w=[KIN])
                    mm(bk4[:, 128:160], pk[:, TRI:TRI + 128], a4.ap(tt_ * 32, [[1, 32]]), r=[pk, a4], w=[acs_dep])
                    mm(bk4[:, 160:192], pk[:, ONES:ONES + 128], a4.ap(tt_ * 32, [[1, 32]]), r=[pk, a4], w=[acs_dep])
                    acopy(acs_sb[:], bk4[:, 128:160], r=[acs_dep], w=[acs_sb])
                    act(eacs4.ap(tt_ * 32, [[1, 32]]), bk4[:, 128:160], AF.Exp, r=[acs_dep], w=[eacs4])
                    act(cdb4.ap(tt_ * 32, [[1, 32]]), bk4[:, 160:192], AF.Exp, r=[acs_dep], w=[cdb4])
                    tt(dd[:], bk4[:, 160:192], acs_sb[:], ALU.subtract, r=[acs_dep, acs_sb], w=[dd])
                    act(dd[:], dd[:], AF.Exp, r=[dd], w=[dd])
                    tt(dtd4.ap(tt_ * 32, [[1, 32]]), dt4.ap(tt_ * 32, [[1, 32]]), dd[:], ALU.mult, r=[dt4, dd], w=[dtd4])
                if seq == 0 and stl == 0:
                    dump("dt4", dt4, dt4[:], [128, 4, 32])
                    dump("KA", KA, KA[:], [128, SEQ])
                    dump("KIN", KIN, KIN[:], [128, SEQ])
                    dump("VA", VA, VA[:], [128, 16, 66])
                    dump("wis", wis, wis[:], [128, 4, 8])
                    dump("eacs4", eacs4, eacs4[:], [128, 4, 32])
                    dump("dtd4", dtd4, dtd4[:], [128, 4, 32])

                S.barrier(("pe", "act", "dve", "sp", "pool"))
                with ExitStack() as ph:
                    Upre = T("Upre", [128, 4, 515], F32, stack=ph)
                    Upre_d = [Dep("Upre%d" % i) for i in range(4)]
                    cv = T("cv", [128, 4, 512], F32, stack=ph)
                    cv_d = [Dep("cv%d" % i) for i in range(4)]
                    thR = Ring([T("th%d" % i, [128, 512], F32, stack=ph) for i in range(2)])
                    xbP = [T("xb%d" % i, [128, 4, 512], BF16, stack=ph) for i in range(2)]
                    xb_d = [[Dep("xb%d_%d" % (i, f)) for f in range(4)] for i in range(2)]
                    zsP = [T("zs%d" % i, [128, 4, 256], F32, stack=ph) for i in range(2)]
                    xtkP = [T("xtk%d" % i, [128, 4, 384], BF16, stack=ph) for i in range(2)]
                    rhsAR = Ring([T("rhsA%d" % i, [128, 512], F32, stack=ph) for i in range(2)])
                    CBmR = Ring([T("CBm%d" % i, [128, 128], F32, stack=ph) for i in range(2)])
                    EsegR = Ring([T("Eseg%d" % i, [128, 512], BF16, stack=ph) for i in range(2)])
                    MTR = Ring([T("MT%d" % i, [128, 512], BF16, stack=ph) for i in range(2)])
                    xcR = Ring([T("xc%d" % i, [128, 256], BF16, stack=ph) for i in range(2)])
                    xcdR = Ring([T("xcd%d" % i, [128, 256], BF16, stack=ph) for i in range(2)])
                    xsDR = Ring([T("xsD%d" % i, [128, 256], F32, stack=ph) for i in range(2)])
                    t1R = Ring([T("t1%d" % i, [128, 256], F32, stack=ph) for i in range(2)])
                    yzR = Ring([T("yz%d" % i, [128, 256], F32, stack=ph) for i in range(4)])
                    smgP = [T("smg%d" % i, [128, 12], F32, stack=ph) for i in range(2)]
                    yzs = {}
                    yjk = T("yjk", [128, 256], BF16, stack=ph)
                    yNR = Ring([T("yN%d" % i, [128, 256], BF16, stack=ph) for i in range(2)])
                    yNT = T("yNT", [128, 16, 512], BF16, stack=ph)
                    A0 = accR.tiles[0]
                    segR = Ring([bk3, bk7])
                    cbR = Ring([(0, bk4.dep)])
                    ydR = Ring([(0, bk5.dep)])
                    soR = Ring([(bk6, bk6.dep, bk6.dep), (accR.tiles[1], accR.tiles[1].dep, accR.tiles[1].dep)])
                    ptrA_d, ptrB_d = ptr.dep, [ptr.dep, ptr.dep]
                    state_done = {}

                    def stageA(g):
                        par = g % 2
                        xb, zs, xtk, xbd = xbP[par], zsP[par], xtkP[par], xb_d[par]
                        wb = wq.pop(g)
                        vcopy(Upre.ap(0, [[515, 4], [1, 3]]), halo.ap(g * 12, [[3, 4], [1, 3]]), r=[halo_deps[g]], w=Upre_d)
                        for fi in range(4):
                            for kc in range(8):
                                mm(A0[:, :], wslice(wb, 768, kc, 256 + fi * 128, 256 + (fi + 1) * 128), hT.ap(kc * 512, [[1, 512]]),
                                   start=(kc == 0), stop=(kc == 7), r=[wb, hT], w=[A0])
                            acopy(Upre.ap(fi * 515 + 3, [[1, 512]]), A0[:, :], r=[A0], w=[Upre_d[fi]])
                            yield
                        vcopy(halo.ap(g * 12, [[3, 4], [1, 3]]), Upre.ap(512, [[515, 4], [1, 3]]), r=Upre_d, w=[halo_deps[g]])
                        for fi in range(4):
                            ct = g * 4 + fi
                            cvf = cv.ap(fi * 512, [[1, 512]])
                            act(cvf, Upre.ap(fi * 515, [[1, 512]]), AF.Identity, r=[Upre_d[fi], pc], w=[cv_d[fi]],
                                bias=pc[:, 128 + ct:129 + ct], scale=pc[:, ct * 4:ct * 4 + 1])
                            yield
                            for k in range(1, 4):
                                stt(cvf, Upre.ap(fi * 515 + k, [[1, 512]]), pc[:, ct * 4 + k:ct * 4 + k + 1], cvf, ALU.mult, ALU.add,
                                    r=[Upre_d[fi], pc, cv_d[fi]], w=[cv_d[fi]])
                                yield
                            th = thR.next()
                            act(th[:], cvf, AF.Tanh, r=[cv_d[fi]], w=[th])
                            stt(xb.ap(fi * 512, [[1, 512]]), th[:], 1.0, cvf, ALU.add, ALU.mult, r=[th, cv_d[fi]], w=[xbd[fi]])
                            yield
                        for c in range(4):
                            for kc in range(8):
                                mm(A0[:, 0:256], hT.ap(kc * 512 + c * 128, [[1, 128]]), wslice(wb, 768, kc, 0, 256),
                                   start=(kc == 0), stop=(kc == 7), r=[hT, wb], w=[A0])
                            th = thR.next()
                            act(th[:, 0:256], A0[:, 0:256], AF.Tanh, r=[A0], w=[th], scale=0.5)
                            stt(zs.ap(c * 256, [[1, 256]]), th[:, 0:256], 1.0, A0[:, 0:256], ALU.add, ALU.mult, r=[th, A0], w=[zs])
                            yield
                        for c in range(4):
                            for fi in range(3):
                                tr(ptr.ap(fi * 128, [[1, 128]]), xb.ap(fi * 512 + c * 128, [[1, 128]]), r=[xbd[fi]], w=[ptrA_d])
                            acopy(xtk.ap(c * 384, [[1, 384]]), ptr.ap(0, [[1, 384]]), r=[ptrA_d], w=[xtk])
                            yield

                    def chunkB(g, c):
                        par = g % 2
                        xb, zs, xtk, xbd = xbP[par], zsP[par], xtkP[par], xb_d[par]
                        hsl = c * 32 + g * 4
                        rhsA, CBm, Eseg, MT = rhsAR.next(), CBmR.next(), EsegR.next(), MTR.next()
                        xc, xcd, xsD, t1, yz = xcR.next(), xcdR.next(), xsDR.next(), t1R.next(), yzR.next()
                        smg = smgP[g % 2]
                        seg = segR.next()
                        cbo, cbd = cbR.next()
                        ydo, ydd = ydR.next()
                        sob, stsd, yofd = soR.next()
                        tt(rhsA.ap(0, [[128, 4], [1, 128]]), pk.ap(TRI, [[0, 4], [1, 128]]), a4.ap(hsl, [[1, 4], [0, 128]]),
                           ALU.mult, r=[pk, a4], w=[rhsA], eng="pool")
                        xs3 = xtk.ap(c * 384, [[64, 4], [1, 64]])
                        tt(xc.ap(0, [[64, 4], [1, 64]]), xs3, dt4.ap(hsl, [[1, 4], [0, 64]]), ALU.mult, r=[xtk, dt4], w=[xc], eng="pool")
                        tt(xcd.ap(0, [[64, 4], [1, 64]]), xs3, dtd4.ap(hsl, [[1, 4], [0, 64]]), ALU.mult, r=[xtk, dtd4], w=[xcd], eng="pool")
                        tt(xsD.ap(0, [[64, 4], [1, 64]]), xs3, pb.ap(DSK + g * 4, [[1, 4], [0, 64]]), ALU.mult, r=[xtk, pb], w=[xsD], eng="pool")
                        yield
                        mm(seg[:, :], pk[:, USTR:USTR + 128], rhsA[:, :], r=[pk, rhsA], w=[seg])
                        mm(bk4[:, cbo:cbo + 128], xb.ap(2 * 512 + c * 128, [[1, 128]]), xb.ap(3 * 512 + c * 128, [[1, 128]]),
                           r=[xbd[2], xbd[3]], w=[cbd])
                        tt(CBm[:], bk4[:, cbo:cbo + 128], pk[:, TRI:TRI + 128], ALU.mult, r=[cbd, pk], w=[CBm])
                        act(Eseg[:], seg[:, :], AF.Exp, r=[seg], w=[Eseg])
                        yield
                        tt(MT.ap(0, [[128, 4], [1, 128]]), Eseg.ap(0, [[128, 4], [1, 128]]), CBm.ap(0, [[0, 4], [1, 128]]),
                           ALU.mult, r=[Eseg, CBm], w=[MT])
                        yield
                        while c > 0 and not state_done.get((g, c - 1)):
                            yield
                        mm(sob[:, 256:512], xb.ap(3 * 512 + c * 128, [[1, 128]]), Sb.ap(g * 256, [[1, 256]]),
                           r=[xbd[3], Sb_deps[g]], w=[yofd])
                        for j in range(4):
                            mm(bk5[:, ydo + j * 64:ydo + (j + 1) * 64], MT[:, j * 128:(j + 1) * 128], xc[:, j * 64:(j + 1) * 64],
                               start=True, stop=True, r=[MT, xc], w=[ydd])
                        mm(sob[:, 0:256], xtk.ap(c * 384 + 256, [[1, 128]]), xcd[:], r=[xtk, xcd], w=[stsd])
                        tt(St.ap(g * 256, [[64, 4], [1, 64]]), St.ap(g * 256, [[64, 4], [1, 64]]),
                           cdb4.ap(hsl, [[1, 4], [0, 64]]), ALU.mult, r=[St_deps[g], cdb4], w=[St_deps[g]])
                        tt(St.ap(g * 256, [[1, 256]]), St.ap(g * 256, [[1, 256]]), sob[:, 0:256], ALU.add,
                           r=[St_deps[g], stsd], w=[St_deps[g]])
                        acopy(Sb.ap(g * 256, [[1, 256]]), St.ap(g * 256, [[1, 256]]), r=[St_deps[g]], w=[Sb_deps[g]])
                        state_done[(g, c)] = True
                        tt(t1.ap(0, [[64, 4], [1, 64]]), sob.ap(256, [[64, 4], [1, 64]]), eacs4.ap(hsl, [[1, 4], [0, 64]]),
                           ALU.mult, r=[yofd, eacs4], w=[t1])
                        tt(t1[:], bk5[:, ydo:ydo + 256], t1[:], ALU.add, r=[ydd, t1], w=[t1])
                        tt(t1[:], xsD[:], t1[:], ALU.add, r=[xsD, t1], w=[t1])
                        yield
                        tt(yz[:], t1[:], zs.ap(c * 256, [[1, 256]]), ALU.mult, r=[t1, zs], w=[yz])
                        act(yjk[:], yz[:], AF.Square, r=[yz], w=[yjk, smg], accum=smg[:, c:c + 1])
                        yzs[(g, c)] = yz
                        yield

                    def normB(g):
                        smg = smgP[g % 2]
                        act(smg[:, 4:8], smg[:, 0:4], AF.Sqrt, r=[smg, cc], w=[smg], bias=cc[:, 3:4], scale=1.0 / 256)
                        recip(smg[:, 8:12], smg[:, 4:8], r=[smg], w=[smg])
                        for c in range(4):
                            yz = yzs.pop((g, c))
                            yN = yNR.next()
                            stt(yN[:], yz[:], smg[:, 8 + c:9 + c], pb[:, SNW + g * 256:SNW + (g + 1) * 256], ALU.mult, ALU.mult,
                                r=[yz, smg, pb], w=[yN])
                            for i in range(2):
                                tr(ptr.ap(512 + (c % 2) * 256 + i * 128, [[1, 128]]), yN[:, i * 128:(i + 1) * 128], r=[yN], w=[ptrB_d[c % 2]])
                            acopy(yNT.ap((g * 2) * 512 + c * 128, [[512, 2], [1, 128]]), ptr.ap(512 + (c % 2) * 256, [[128, 2], [1, 128]]),
                                  r=[ptrB_d[c % 2]], w=[yNT])

                    def run_group(g):
                        if g + 2 < 8:
                            wq[g + 2] = load_w(win_v, C_GRP + (g + 2) * 768, 768)
                        pending = [chunkB(g, c) for c in range(4)]
                        active = [pending.pop(0), pending.pop(0)]
                        ag = stageA(g + 1) if g < 7 else None
                        while active or ag is not None:
                            for g_ in list(active):
                                try:
                                    next(g_)
                                except StopIteration:
                                    active.remove(g_)
                                    if pending:
                                        active.append(pending.pop(0))
                            for _rep in range(2):
                                if ag is not None:
                                    try:
                                        next(ag)
                                    except StopIteration:
                                        ag = None

                    wq = {0: pre_w.pop(), 1: load_w(win_v, C_GRP + 768, 768)}
                    for _ in stageA(0):
                        pass
                    for g in range(8):
                        run_group(g)
                        normB(g)
                    if seq == 0 and stl == 0:
                        dump("yNT", yNT, yNT[:], [128, 16, 512])
                    obanks = [accR.tiles[0], accR.tiles[1], bk3, bk7]
                    for half in range(2):
                        for kh in range(2):
                            wb = wring.next()
                            dma("pool", wb.ap(0, [[512, 8], [1, 512]]), wso_v[:, kh * 8:(kh + 1) * 8, half * 512:(half + 1) * 512], w=[wb])
                            for tt_ in range(4):
                                for kc in range(8):
                                    mm(obanks[tt_][:, :], yNT.ap((kh * 8 + kc) * 512 + tt_ * 128, [[1, 128]]), wslice(wb, 512, kc, 0, 512),
                                       start=(kh == 0 and kc == 0), stop=(kh == 1 and kc == 7), r=[yNT, wb], w=[obanks[tt_]])
                        for tt_ in range(4):
                            acopy(yssm.ap(tt_ * 1024 + half * 512, [[1, 512]]), obanks[tt_][:, :], r=[obanks[tt_]], w=[yssm])
                if seq == 0 and stl == 0:
                    dump("yssm", yssm, yssm[:], [128, 4, 1024])

                S.barrier()
                with ExitStack() as ph:
                    QA = T("QA", [128, 16, 512], BF16, stack=ph)
                    QI = T("QI", [128, 8, 512], BF16, stack=ph)
                    zA = T("zA", [128, 4, 1024], BF16, stack=ph)
                    isc = T("isc", [128, SEQ], F32, stack=ph)
                    rlR = Ring([T("rl%d" % i, [128, 512], F32, stack=ph) for i in range(2)])
                    maskb = T("maskb", [128, SEQ], BF16, stack=ph)
                    maskTP = [T("maskT%d" % i, [128, 16, 128], BF16, stack=ph) for i in range(2)]
                    ER = Ring([T("E%d" % i, [128, 512], BF16, stack=ph) for i in range(5)])
                    PR = Ring([T("P%d" % i, [128, 512], BF16, stack=ph) for i in range(5)])
                    og = T("og", [128, 1024], BF16, stack=ph)
                    Lm = T("Lm", [128, 512], F32, stack=ph)
                    OTs = T("OTs", [128, 512], F32, stack=ph)
                    thA = Lm
                    oT = T("oT", [128, 8, 512], BF16, stack=ph)
                    bs = T("bs", [128, 8], F32, stack=ph)
                    stp = T("stp", [128, NIT + 1], F32, stack=ph)
                    rc = T("rc", [128, 4], F32, stack=ph)
                    bu = T("bu", [128, 2], U32, stack=ph)
                    LR = Ring([bk3, accR.tiles[1], bk6, bk7])
                    A0 = accR.tiles[0]
                    A1 = accR.tiles[1]
                    Ob = [bk4, bk5, bk4, bk5]
                    Odeps = [Ob[j].dep for j in range(4)]
                    ptrA_d = ptrB_d = ptr.dep

                    dma("sp", QA.ap(0, [[512, 16], [1, 512]], p0=64, np_=5), qaug_d[:, :, t0:t0 + 512], w=[QA])
                    wb = load_w(win_v, C_QI, 512)
                    for h in range(8):
                        for kc in range(8):
                            mm(A0[0:64, :], wslice(wb, 512, kc, h * 64, (h + 1) * 64), hT.ap(kc * 512, [[1, 512]]),
                               start=(kc == 0), stop=(kc == 7), r=[wb, hT], w=[A0])
                        acopy(QI.ap(h * 512, [[1, 512]], np_=64), A0[0:64, :], r=[A0], w=[QI])

                    def stageI(tt_):
                        qb = stl * 4 + tt_
                        SL = (qb + 1) * 128
                        maskT = maskTP[tt_ % 2]
                        for c4 in range((SL + 511) // 512):
                            w_ = min(512, SL - c4 * 512)
                            for h in range(8):
                                mm(A0[:, 0:w_], QI.ap(h * 512 + tt_ * 128, [[1, 128]], np_=64), KIN.ap(c4 * 512, [[1, w_]], np_=64),
                                   r=[QI, KIN], w=[A0])
                                rl = rlR.next()
                                act(rl[:, 0:w_], A0[:, 0:w_], AF.Relu, r=[A0], w=[rl])
                                wcol = wis.ap(tt_ * 8 + h, [[1, 1]])
                                if h == 0:
                                    ts(isc[:, c4 * 512:c4 * 512 + w_], rl[:, 0:w_], wcol, ALU.mult, r=[rl, wis], w=[isc])
                                else:
                                    stt(isc[:, c4 * 512:c4 * 512 + w_], rl[:, 0:w_], wcol, isc[:, c4 * 512:c4 * 512 + w_],
                                        ALU.mult, ALU.add, r=[rl, wis, isc], w=[isc])
                                yield
                        if qb >= 2:
                            S.op("dve", lambda e, SL=SL, bs=bs, isc=isc: e.tensor_reduce(out=bs[:, 1:2], in_=isc[:, 0:SL], axis=AX.X, op=ALU.max), [isc], [bs])
                            S.op("dve", lambda e, SL=SL, bs=bs, isc=isc: e.tensor_reduce(out=bs[:, 0:1], in_=isc[:, 0:SL], axis=AX.X, op=ALU.min), [isc], [bs])
                            ts(bs[:, 5:6], bs[:, 1:2], 1.0, ALU.add, r=[bs], w=[bs], s2=bs[:, 0:1], op1=ALU.subtract)
                            ts(stp[:], p2[:], bs[:, 5:6], ALU.mult, r=[p2, bs], w=[stp])
                            ts(bs[:, 2:3], stp[:, 0:1], bs[:, 0:1], ALU.add, r=[stp, bs], w=[bs])
                        tt(isc[:, SL - 128:SL], isc[:, SL - 128:SL], pk[:, NEGM:NEGM + 128], ALU.add, r=[isc, pk], w=[isc])
                        yield
                        if qb >= 2:
                            for it in range(NIT):
                                ts(maskb[:, 0:SL], isc[:, 0:SL], bs[:, 2:3], ALU.is_ge, r=[isc, bs], w=[maskb, bs],
                                   s2=None, op1=ALU.add, accum=bs[:, 3:4])
                                yield
                                ts(bs[:, 4:5], bs[:, 3:4], TOPK - 0.5, ALU.is_ge, r=[bs, stp], w=[bs], s2=stp[:, it:it + 1], op1=ALU.mult)
                                stt(bs[:, 2:3], bs[:, 4:5], stp[:, it + 1:it + 2], bs[:, 2:3], ALU.subtract, ALU.add, r=[bs, stp], w=[bs])
                                yield
                            tt(bs[:, 6:7], bs[:, 2:3], stp[:, NIT:NIT + 1], ALU.subtract, r=[bs, stp], w=[bs])
                            thr = bs[:, 6:7]
                        else:
                            thr = cc[:, 2:3]
                        ts(maskb[:, 0:SL], isc[:, 0:SL], thr, ALU.is_ge, r=[isc, bs, cc], w=[maskb])
                        if seq == 0 and stl == 0 and tt_ == 3:
                            dump("isc", isc, isc[:, 0:512], [128, 512])
                            dump("bs", bs, bs[:], [128, 8])
                        for k0 in range(0, qb + 1, 4):
                            nk = min(4, qb + 1 - k0)
                            for kb in range(k0, k0 + nk):
                                tr(ptr.ap((kb - k0) * 128, [[1, 128]]), maskb[:, kb * 128:(kb + 1) * 128], r=[maskb], w=[ptrA_d])
                            acopy(maskT.ap(k0 * 128, [[1, nk * 128]]), ptr.ap(0, [[1, nk * 128]]), r=[ptrA_d], w=[maskT])
                            yield

                    def stageAT(tt_):
                        qb = stl * 4 + tt_
                        maskT = maskTP[tt_ % 2]
                        def kb_lo(hg):
                            smin = 2.0 ** (-8.0 * (hg * 4 + 4) / 16.0)
                            dskip = int(np.ceil((60.0 / smin + 127.0) / 128.0))
                            return max(0, qb - dskip + 1)
                        steps = [(hg, kb) for hg in range(4) for kb in range(kb_lo(hg), qb + 1)]
                        Ps = {}

                        def front(i):
                            hg, kb = steps[i]
                            L = LR.next()
                            mm(L[:, :], KA.ap(kb * 128, [[1, 128]], np_=69),
                               QA.ap(hg * 4 * 512 + tt_ * 128, [[512, 4], [1, 128]], np_=69), r=[KA, QA], w=[L])
                            E = ER.next()
                            if kb == qb:
                                tt(Lm.ap(0, [[128, 4], [1, 128]]), L.ap(0, [[128, 4], [1, 128]]),
                                   pk.ap(NEGT, [[0, 4], [1, 128]]), ALU.add, r=[L, pk], w=[Lm])
                                act(E[:], Lm[:], AF.Exp, r=[Lm], w=[E])
                            else:
                                act(E[:], L[:, :], AF.Exp, r=[L], w=[E])
                            P = PR.next()
                            tt(P.ap(0, [[128, 4], [1, 128]]), E.ap(0, [[128, 4], [1, 128]]),
                               maskT.ap(kb * 128, [[0, 4], [1, 128]]), ALU.mult, r=[E, maskT], w=[P],
                               eng=("pool" if i % 2 == 0 else "dve"))
                            Ps[i] = P

                        def back(i):
                            hg, kb = steps[i]
                            P = Ps.pop(i)
                            mm(Ob[hg][0:66, :], VA.ap(kb * 66, [[1, 66]]), P[:, :], start=(kb == kb_lo(hg)), stop=(kb == qb),
                               r=[VA, P], w=[Odeps[hg]])
                            if kb == qb:
                                acopy(OTs[0:66, :], Ob[hg][0:66, :], r=[Odeps[hg]], w=[OTs])
                                for j in range(4):
                                    S.op("pe", lambda e, j=j, hg=hg, OTs=OTs: e.transpose(out=Ob[hg][:, j * 66:(j + 1) * 66],
                                                                                         in_=OTs[0:66, j * 128:(j + 1) * 128],
                                                                                         identity=pk[0:66, IDENT:IDENT + 66]),
                                         [OTs, pk], [Odeps[hg]])
                                for j in range(4):
                                    h = hg * 4 + j
                                    recip(rc[:, j:j + 1], Ob[hg][:, j * 66 + 64:j * 66 + 65], r=[Odeps[hg]], w=[rc])
                                    stt(og[:, h * 64:(h + 1) * 64], Ob[hg][:, j * 66:j * 66 + 64], rc[:, j:j + 1],
                                        zA.ap(tt_ * 1024 + h * 64, [[1, 64]]), ALU.mult, ALU.mult, r=[Odeps[hg], rc, zA], w=[og])

                        LA = 3
                        for i in range(min(LA, len(steps))):
                            front(i)
                        for i in range(len(steps)):
                            if i + LA < len(steps):
                                front(i + LA)
                            back(i)
                            yield
                        for kc in range(8):
                            tr(ptr.ap(512 + (kc % 4) * 128, [[1, 128]]), og[:, kc * 128:(kc + 1) * 128], r=[og], w=[ptrB_d])
                            if kc % 4 == 3:
                                acopy(oT.ap((kc - 3) * 512 + tt_ * 128, [[512, 4], [1, 128]]), ptr.ap(512, [[128, 4], [1, 128]]),
                                      r=[ptrB_d], w=[oT])
                        yield

                    def stageProj():
                        for half in range(2):
                            wb = load_w(win_v, C_Q + half * 512, 512)
                            for hh in range(8):
                                h = half * 8 + hh
                                for kc in range(8):
                                    mm(A1[0:64, :], wslice(wb, 512, kc, hh * 64, (hh + 1) * 64), hT.ap(kc * 512, [[1, 512]]),
                                       start=(kc == 0), stop=(kc == 7), r=[wb, hT], w=[A1])
                                S.op("act", lambda e, h=h, QA=QA, A1=A1: e.mul(QA.ap(h * 512, [[1, 512]], np_=64), A1[0:64, :], 0.125), [A1], [QA])
                                yield
                        for half in range(2):
                            wb = load_w(win_v, C_AZ + half * 512, 512)
                            for tt_ in range(4):
                                for kc in range(8):
                                    mm(A1[:, :], hT.ap(kc * 512 + tt_ * 128, [[1, 128]]), wslice(wb, 512, kc, 0, 512),
                                       start=(kc == 0), stop=(kc == 7), r=[hT, wb], w=[A1])
                                act(thA[:], A1[:, :], AF.Tanh, r=[A1], w=[thA], scale=0.5)
                                stt(zA.ap(tt_ * 1024 + half * 512, [[1, 512]]), thA[:], 1.0, A1[:, :], ALU.add, ALU.mult,
                                    r=[thA, A1], w=[zA])
                                yield

                    interleave(stageI(0), stageProj())
                    if seq == 0 and stl == 0:
                        dump("QA", QA, QA[:], [128, 16, 512])
                        dump("QI", QI, QI[:], [128, 8, 512])
                    for tt_ in range(4):
                        interleave(stageAT(tt_), stageI(tt_ + 1) if tt_ < 3 else iter(()))
                    if seq == 0 and stl == 0:
                        dump("oT", oT, oT[:], [128, 8, 512])
                    for half in range(2):
                        wb = load_w(wao_v, half * 512, 512)
                        for tt_ in range(4):
                            acc = accR.next()
                            for kc in range(8):
                                mm(acc[:, :], oT.ap(kc * 512 + tt_ * 128, [[1, 128]]), wslice(wb, 512, kc, 0, 512),
                                   start=(kc == 0), stop=(kc == 7), r=[oT, wb], w=[acc])
                            acopy(yattn.ap(tt_ * 1024 + half * 512, [[1, 512]]), acc[:, :], r=[acc], w=[yattn])
                if seq == 0 and stl == 0:
                    dump("yattn", yattn, yattn[:], [128, 4, 1024])

                S.barrier()
                with ExitStack() as ph:
                    gtR = Ring([T("gt%d" % i, [128, 512], F32, stack=ph) for i in range(2)])
                    gsR = Ring([T("gs%d" % i, [128, 512], F32, stack=ph) for i in range(2)])
                    mg = T("mg", [128, 4, 1024], F32, stack=ph)
                    mb = T("mb", [128, 1024], BF16, stack=ph)
                    mT = T("mT", [128, 8, 512], BF16, stack=ph)
                    r4 = T("r4", [128, 4, 1024], F32, stack=ph)
                    roR = Ring([T("ro%d" % i, [128, 1024], F32, stack=ph) for i in range(2)])
                    p5 = T("p5", [128, NPB - NRES], F32, stack=ph)
                    dma("sp", p5[:], pb_d[:, NRES:NPB], w=[p5])
                    for u in range(4):
                        wb = load_w(win_v, C_GATE + u * 512, 512)
                        for tt_ in range(4):
                            acc = accR.next()
                            for kc in range(8):
                                mm(acc[:, :], hT.ap(kc * 512 + tt_ * 128, [[1, 128]]), wslice(wb, 512, kc, 0, 512),
                                   start=(kc == 0), stop=(kc == 7), r=[hT, wb], w=[acc])
                            gtt = gtR.next()
                            tt(gtt[:], acc[:, :], p5[:, u * 512:(u + 1) * 512], ALU.add, r=[acc, p5], w=[gtt])
                            gs = gsR.next()
                            act(gs[:], gtt[:], AF.Tanh, r=[gtt], w=[gs], scale=0.5)
                            if u < 2:
                                stt(mg.ap(tt_ * 1024 + u * 512, [[1, 512]]), gs[:], 1.0, yssm.ap(tt_ * 1024 + u * 512, [[1, 512]]),
                                    ALU.add, ALU.mult, r=[gs, yssm], w=[mg])
                            else:
                                stt(gs[:], gs[:], 1.0, yattn.ap(tt_ * 1024 + (u - 2) * 512, [[1, 512]]), ALU.add, ALU.mult,
                                    r=[gs, yattn], w=[gs])
                                tt(mg.ap(tt_ * 1024 + (u - 2) * 512, [[1, 512]]), mg.ap(tt_ * 1024 + (u - 2) * 512, [[1, 512]]),
                                   gs[:], ALU.add, r=[mg, gs], w=[mg])
                    for tt_ in range(4):
                        S.op("act", lambda e, tt_=tt_, mb=mb, mg=mg: e.mul(mb[:], mg.ap(tt_ * 1024, [[1, 1024]]), 0.5), [mg], [mb])
                        for kc in range(8):
                            tr(ptr.ap(kc * 128, [[1, 128]]), mb[:, kc * 128:(kc + 1) * 128], r=[mb], w=[ptr])
                        acopy(mT.ap(tt_ * 128, [[512, 8], [1, 128]]), ptr.ap(0, [[128, 8], [1, 128]]), r=[ptr], w=[mT])
                    for half in range(2):
                        wb = load_w(wout_v, half * 512, 512)
                        for tt_ in range(4):
                            acc = accR.next()
                            for kc in range(8):
                                mm(acc[:, :], mT.ap(kc * 512 + tt_ * 128, [[1, 128]]), wslice(wb, 512, kc, 0, 512),
                                   start=(kc == 0), stop=(kc == 7), r=[mT, wb], w=[acc])
                            acopy(r4.ap(tt_ * 1024 + half * 512, [[1, 512]]), acc[:, :], r=[acc], w=[r4])
                    for tt_ in range(4):
                        xt = xring.next()
                        dma("sp", xt[:], x_d[seq, t0 + tt_ * 128:t0 + (tt_ + 1) * 128, :], w=[xt])
                        r4s = r4.ap(tt_ * 1024, [[1, 1024]])
                        tt(r4s, r4s, xt[:], ALU.add, r=[r4, xt], w=[r4])
                        act(mb[:], r4s, AF.Square, r=[r4], w=[mb, sm], accum=sm[:, 0:1])
                        rstd_col(sm[:, 2:3], sm[:, 0:1], 1.0 / D_MODEL, cc[:, 0:1], sm)
                        ro = roR.next()
                        stt(ro[:], r4s, sm[:, 2:3], p5[:, FNW - NRES:FNW - NRES + 1024], ALU.mult, ALU.mult, r=[r4, sm, p5], w=[ro])
                        dma("sp", out_d[seq, t0 + tt_ * 128:t0 + (tt_ + 1) * 128, :], ro[:], r=[ro], w=[ro])
        S.final_wait("sp")
        S.emit()
    return nc, dbg_outs


def host_prep(inputs):
    f32 = np.float32
    w_in = np.asarray(inputs["w_in"], f32)[0]
    O_Z, O_XBC, O_DT, O_Q, O_K, O_V, O_AZ, O_QI, O_KI, O_WI, O_G = 0, 2048, 6144, 6176, 7200, 7264, 7328, 8352, 8864, 8928, 8936
    cols = []
    cols += list(range(O_DT, O_DT + 32)) + list(range(O_K, O_K + 64)) + list(range(O_V, O_V + 64))
    cols += list(range(O_KI, O_KI + 64)) + list(range(O_WI, O_WI + 8))
    for g in range(8):
        cols += list(range(O_Z + g * 256, O_Z + (g + 1) * 256))
        cols += list(range(O_XBC + g * 256, O_XBC + (g + 1) * 256))
        cols += list(range(O_XBC + 2048 + g * 128, O_XBC + 2048 + (g + 1) * 128))
        cols += list(range(O_XBC + 3072 + g * 128, O_XBC + 3072 + (g + 1) * 128))
    cols += list(range(O_Q, O_Q + 1024)) + list(range(O_QI, O_QI + 512)) + list(range(O_AZ, O_AZ + 1024))
    cols += list(range(O_G, O_G + 2048))
    cols = np.asarray(cols)
    assert cols.shape[0] == IN_TOTAL and np.unique(cols).shape[0] == IN_TOTAL
    win = np.ascontiguousarray(w_in[:, cols])

    pb = np.zeros((128, NPB), f32)

    def put(off, v):
        v = np.asarray(v, f32).reshape(-1)
        pb[:, off:off + v.shape[0]] = v[None, :]
    put(NW, inputs["norm_w"][0])
    put(FNW, inputs["final_norm_w"])
    put(GB, inputs["gate_b"][0])
    put(SNW, inputs["ssm_norm_w"][0])
    put(DTB, inputs["dt_bias"][0])
    put(ALOG, inputs["a_log"][0])
    put(DSK, inputs["d_skip"][0])
    put(KIW, inputs["idx_k_norm_w"][0])
    put(KIB, inputs["idx_k_norm_b"][0])

    conv_w = np.asarray(inputs["conv_w"], f32)[0]
    conv_b = np.asarray(inputs["conv_b"], f32)[0]
    pc = np.zeros((128, 160), f32)
    for g in range(8):
        for fi in range(4):
            ct = g * 4 + fi
            if fi < 2:
                ch0 = g * 256 + fi * 128
            elif fi == 2:
                ch0 = 2048 + g * 128
            else:
                ch0 = 3072 + g * 128
            pc[:, ct * 4:(ct + 1) * 4] = conv_w[:, ch0:ch0 + 128].T
            pc[:, 128 + ct] = conv_b[ch0:ch0 + 128]

    pk = np.zeros((128, NPK), f32)
    i = np.arange(128)
    pk[:, IDENT:IDENT + 128] = np.eye(128, dtype=f32)
    pk[:, TRI:TRI + 128] = (i[:, None] <= i[None, :]).astype(f32)
    pk[:, USTR:USTR + 128] = (i[:, None] > i[None, :]).astype(f32)
    pk[:, NEGM:NEGM + 128] = np.where(i[None, :] > i[:, None], f32(-1e30), f32(0))
    pk[:, ONES:ONES + 128] = 1.0
    pk[:, NEGT:NEGT + 128] = np.where(i[:, None] > i[None, :], f32(-1e30), f32(0))

    bf = ml_dtypes.bfloat16
    slopes = np.exp2(-8.0 * np.arange(1, 17, dtype=np.float64) / 16).astype(f32)
    s_hi = slopes.astype(bf)
    s_lo = (slopes - s_hi.astype(f32)).astype(bf)
    spos = np.arange(SEQ)
    kaug = np.zeros((5, SEQ), f32)
    kaug[0] = 1.0
    kaug[1] = spos % 128
    kaug[2] = (spos // 128) * 128
    kaug[3] = spos % 128
    kaug[4] = (spos // 128) * 128
    kaug = kaug.astype(bf)
    qaug = np.zeros((5, 16, SEQ), f32)
    qaug[0] = -(slopes[:, None].astype(np.float64) * spos[None, :]).astype(f32)
    qaug[1] = s_hi.astype(f32)[:, None]
    qaug[2] = s_hi.astype(f32)[:, None]
    qaug[3] = s_lo.astype(f32)[:, None]
    qaug[4] = s_lo.astype(f32)[:, None]
    qaug = qaug.astype(bf)
    shared = {
        "win": win,
        "wso": np.ascontiguousarray(np.asarray(inputs["w_ssm_out"], f32)[0]),
        "wao": np.ascontiguousarray(np.asarray(inputs["w_attn_out"], f32)[0]),
        "wout": np.ascontiguousarray(np.asarray(inputs["w_out"], f32)[0]),
        "pb": pb, "pc": pc, "pk": pk, "kaug": kaug, "qaug": qaug,
    }
    return shared


_CACHE = {}


def kernel(**inputs):
    x = np.asarray(inputs["x"], np.float32)
    shared = host_prep(inputs)
    if "nc" not in _CACHE:
        _CACHE["nc"] = build_program(2)[0]
    nc = _CACHE["nc"]
    in_maps = []
    for c in range(8):
        m = dict(shared)
        m["x"] = np.ascontiguousarray(x[2 * c:2 * c + 2])
        in_maps.append(m)
    res = run_bass_kernel_spmd(nc, in_maps, core_ids=list(range(8)))
    out = np.concatenate([np.asarray(r["out"], np.float32) for r in res.results], axis=0)
    return out
```

```python
import numpy as np
import ml_dtypes
from contextlib import ExitStack
import concourse.bass as bass
import concourse.mybir as mybir
from concourse.bass_utils import run_bass_kernel_spmd

F32 = mybir.dt.float32
BF16 = mybir.dt.bfloat16
U32 = mybir.dt.uint32
AF = mybir.ActivationFunctionType
ALU = mybir.AluOpType
AX = mybir.AxisListType

D_MODEL = 1024
SEQ = 2048
IN_TOTAL = 10984
NIT = 13
TOPK = 256
EPS = 1e-6
IDX_SCALE = (8 ** -0.5) * (64 ** -0.5)

NW, SNW, DTB, ALOG, DSK, KIW, KIB, NRES, GB, FNW, NPB = 0, 1024, 3072, 3104, 3136, 3168, 3232, 3296, 3296, 5344, 6368
IDENT, TRI, USTR, NEGM, ONES, NEGT, NPK = 0, 128, 256, 384, 512, 640, 768
C_SMALL, C_GRP, C_Q, C_QI, C_AZ, C_GATE = 0, 232, 6376, 7400, 7912, 8936


class Dep:
    __slots__ = ("name", "w", "r")

    def __init__(self, name=""):
        self.name = name
        self.w = None
        self.r = {}


class Sched:
    ENGS = ("pe", "act", "dve", "pool", "sp")

    def __init__(self, nc, stack, n_dma_sems=24):
        self.nc = nc
        self.lists = {e: [] for e in self.ENGS}
        self.sems = {}
        self.cnt = {}
        for e in ("pe", "act", "dve", "pool"):
            self.sems[e] = stack.enter_context(nc.semaphore("s_" + e))
            self.cnt[e] = 0
        self.dma_pool = {}
        for q, n in (("sp", n_dma_sems), ("pool", 8)):
            keys = []
            for i in range(n):
                k = "d_%s_%d" % (q, i)
                self.sems[k] = stack.enter_context(nc.semaphore(k))
                self.cnt[k] = 0
                keys.append(k)
            self.dma_pool[q] = [keys, 0]
        self.seen = {e: {} for e in self.ENGS}
        self.n_ops = 0

    def _needs(self, eng, reads, writes):
        needs = {}

        def add(k, v):
            if v > needs.get(k, 0):
                needs[k] = v
        for d in reads:
            if d.w is not None and not (eng == "pe" and d.w[0] == "pe"):
                add(*d.w)
        for d in writes:
            if d.w is not None and not (eng == "pe" and d.w[0] == "pe"):
                add(*d.w)
            for k, v in d.r.items():
                if not (eng == "pe" and k == "pe"):
                    add(k, v)
        out = []
        seen = self.seen[eng]
        for k, v in needs.items():
            if seen.get(k, 0) >= v:
                continue
            seen[k] = v
            out.append((k, v))
        return out

    def op(self, eng, fn, reads=(), writes=()):
        reads = [getattr(d, "dep", d) for d in reads]
        writes = [getattr(d, "dep", d) for d in writes]
        waits = self._needs(eng, reads, writes)
        self.cnt[eng] += 1
        v = self.cnt[eng]
        self.lists[eng].append((waits, fn, eng, 1))
        for d in reads:
            d.r[eng] = v
        for d in writes:
            d.w = (eng, v)
            d.r = {}
        self.n_ops += 1

    def dma(self, q, fn, reads=(), writes=()):
        reads = [getattr(d, "dep", d) for d in reads]
        writes = [getattr(d, "dep", d) for d in writes]
        keys, idx = self.dma_pool[q]
        k = keys[idx % len(keys)]
        self.dma_pool[q][1] = idx + 1
        waits = self._needs(q, reads, writes)
        prev = self.cnt[k]
        if prev > 0 and self.seen[q].get(k, 0) < prev:
            self.seen[q][k] = prev
            waits.append((k, prev))
        self.cnt[k] += 16
        v = self.cnt[k]
        self.lists[q].append((waits, fn, k, 16))
        for d in reads:
            d.r[k] = v
        for d in writes:
            d.w = (k, v)
            d.r = {}
        self.n_ops += 1

    def barrier(self, engs=("pe", "act", "dve", "sp")):
        for e in engs:
            waits = []
            for k, v in self.cnt.items():
                if k == e or k.startswith("d_pool") or v == 0:
                    continue
                if self.seen[e].get(k, 0) < v:
                    self.seen[e][k] = v
                    waits.append((k, v))
            if waits:
                self.lists[e].append((waits, None, None, 0))

    def final_wait(self, eng):
        waits = []
        for k, v in self.cnt.items():
            if k == eng or v == 0:
                continue
            if self.seen[eng].get(k, 0) < v:
                self.seen[eng][k] = v
                waits.append((k, v))
        self.lists[eng].append((waits, None, None, 0))

    def emit(self):
        nc = self.nc
        sems = self.sems
        lists = self.lists

        def replay(e, lst):
            for waits, fn, k, inc in lst:
                for (wk, wv) in waits:
                    e.wait_ge(sems[wk], wv)
                if fn is not None:
                    fn(e).then_inc(sems[k], inc)

        with nc.Block() as block:
            @block.tensor
            def _(e):
                replay(e, lists["pe"])

            @block.scalar
            def _(e):
                replay(e, lists["act"])

            @block.vector
            def _(e):
                replay(e, lists["dve"])

            @block.gpsimd
            def _(e):
                replay(e, lists["pool"])

            @block.sync
            def _(e):
                replay(e, lists["sp"])


def build_program(n_seq=2, dbg=None):
    nc = bass.Bass("TRN2", target_bir_lowering=False)

    def dram(name, shape, dt=F32, kind="ExternalInput"):
        return nc.dram_tensor(name, shape, dt, kind=kind).ap()

    x_d = dram("x", [n_seq, SEQ, D_MODEL])
    win_d = dram("win", [D_MODEL, IN_TOTAL])
    wso_d = dram("wso", [2048, 1024])
    wao_d = dram("wao", [1024, 1024])
    wout_d = dram("wout", [1024, 1024])
    pb_d = dram("pb", [128, NPB])
    pc_d = dram("pc", [128, 160])
    pk_d = dram("pk", [128, NPK])
    kaug_d = dram("kaug", [5, SEQ], BF16)
    qaug_d = dram("qaug", [5, 16, SEQ], BF16)
    out_d = dram("out", [n_seq, SEQ, D_MODEL], kind="ExternalOutput")
    dbg_outs = {}

    win_v = win_d.rearrange("(kc p) n -> p kc n", p=128)
    wso_v = wso_d.rearrange("(kc p) n -> p kc n", p=128)
    wao_v = wao_d.rearrange("(kc p) n -> p kc n", p=128)
    wout_v = wout_d.rearrange("(kc p) n -> p kc n", p=128)

    with ExitStack() as st0:
        S = Sched(nc, st0)
        uid = [0]

        class T:
            def __init__(self, name, shape, dt, psum=False, stack=st0):
                uid[0] += 1
                nm = "%s_%d" % (name, uid[0])
                alloc = nc.psum_tensor if psum else nc.sbuf_tensor
                self.t = stack.enter_context(alloc(nm, list(shape), dt))
                self.dep = Dep(nm)
                self.row = int(np.prod(shape[1:]))

            def __getitem__(self, k):
                return self.t[k]

            def ap(self, col0, dims, p0=0, np_=128):
                return bass.AP(self.t, p0 * self.row + col0, [[self.row, np_]] + [list(d) for d in dims])

        class Ring:
            def __init__(self, tiles):
                self.tiles = tiles
                self.i = 0

            def next(self):
                t = self.tiles[self.i % len(self.tiles)]
                self.i += 1
                return t

        def mm(out, lhsT, rhs, start=True, stop=True, r=(), w=()):
            S.op("pe", lambda e: e.matmul(out, lhsT=lhsT, rhs=rhs, start=start, stop=stop), r, w)

        def tr(out, in_, r=(), w=()):
            S.op("pe", lambda e: e.transpose(out=out, in_=in_, identity=identb[:]), list(r) + [identb], w)

        def act(out, in_, func, r=(), w=(), bias=None, scale=None, accum=None):
            kw = {}
            if bias is not None:
                kw["bias"] = bias
            if scale is not None:
                kw["scale"] = scale
            if accum is not None:
                kw["accum_out"] = accum
            S.op("act", lambda e: e.activation(out=out, in_=in_, func=func, **kw), r, w)

        def acopy(out, in_, r=(), w=()):
            S.op("act", lambda e: e.copy(out=out, in_=in_), r, w)

        def tt(out, a, b, op, r=(), w=(), eng="dve"):
            S.op(eng, lambda e: e.tensor_tensor(out=out, in0=a, in1=b, op=op), r, w)

        def ts(out, a, s1, op0, r=(), w=(), s2=None, op1=None, accum=None):
            kw = {}
            if op1 is not None:
                kw["op1"] = op1
            if accum is not None:
                kw["accum_out"] = accum
            S.op("dve", lambda e: e.tensor_scalar(out=out, in0=a, scalar1=s1, scalar2=s2, op0=op0, **kw), r, w)

        def stt(out, a, s, b, op0, op1, r=(), w=()):
            S.op("dve", lambda e: e.scalar_tensor_tensor(out=out, in0=a, scalar=s, in1=b, op0=op0, op1=op1), r, w)

        def vcopy(out, in_, r=(), w=()):
            S.op("dve", lambda e: e.tensor_copy(out=out, in_=in_), r, w)

        def memset(ap, val, w=()):
            S.op("dve", lambda e: e.memset(ap, val), (), w)

        def recip(out, in_, r=(), w=()):
            S.op("dve", lambda e: e.reciprocal(out=out, in_=in_), r, w)

        def dma(q, out, in_, r=(), w=()):
            S.dma(q, lambda e: e.dma_start(out=out, in_=in_), r, w)

        def rstd_col(out_col, ssq_col, scale, eps_col, tile):
            act(ssq_col, ssq_col, AF.Sqrt, r=[tile, cc], w=[tile], bias=eps_col, scale=scale)
            recip(out_col, ssq_col, r=[tile], w=[tile])

        def interleave(*gens):
            alive = list(gens)
            while alive:
                for g_ in list(alive):
                    try:
                        next(g_)
                    except StopIteration:
                        alive.remove(g_)

        def dump(name, tile, ap, shape):
            if dbg is None or name not in dbg:
                return
            d = nc.dram_tensor("dbg_" + name, list(shape), tile.t.dtype, kind="ExternalOutput").ap()
            dbg_outs[name] = "dbg_" + name
            dma("sp", d, ap, r=[tile], w=[])

        pb = T("pb", [128, NRES], F32)
        pc = T("pc", [128, 160], F32)
        pk = T("pk", [128, NPK], F32)
        identb = T("identb", [128, 128], BF16)
        cc = T("cc", [128, 8], F32)
        Abc = T("Abc", [128, 32], F32)
        Wsm = T("Wsm", [128, 8, 232], BF16)
        KA = T("KA", [128, SEQ], BF16)
        KIN = T("KIN", [128, SEQ], BF16)
        VA = T("VA", [128, 16, 66], BF16)
        St = T("St", [128, 8, 256], F32)
        Sb = T("Sb", [128, 8, 256], BF16)
        St_deps = [Dep("St%d" % g) for g in range(8)]
        Sb_deps = [Dep("Sb%d" % g) for g in range(8)]
        halo = T("halo", [128, 32, 3], F32)
        halo_deps = [Dep("halo%d" % g) for g in range(8)]
        xring = Ring([T("xt%d" % i, [128, 1024], F32) for i in range(2)])
        hb = T("hb", [128, 1024], BF16)
        hT = T("hT", [128, 8, 512], BF16)
        sm = T("sm", [128, 16], F32)
        dt4 = T("dt4", [128, 4, 32], F32)
        a4 = T("a4", [128, 4, 32], F32)
        eacs4 = T("eacs4", [128, 4, 32], F32)
        cdb4 = T("cdb4", [128, 4, 32], F32)
        dtd4 = T("dtd4", [128, 4, 32], F32)
        wis = T("wis", [128, 4, 8], F32)
        s32 = [T("s32_%d" % i, [128, 32], F32) for i in range(4)]
        kvb = T("kvb", [128, 128], BF16)
        kif = T("kif", [128, 64], F32)
        wring = Ring([T("wb%d" % i, [128, 6144], BF16) for i in range(3)])
        yssm = T("yssm", [128, 4, 1024], BF16)
        yattn = T("yattn", [128, 4, 1024], BF16)

        accR = Ring([T("acc%d" % i, [128, 512], F32, psum=True) for i in range(2)])
        ptr = T("ptr", [128, 1024], BF16, psum=True)
        bk3 = T("bk3", [128, 512], F32, psum=True)
        bk4 = T("bk4", [128, 512], F32, psum=True)
        bk5 = T("bk5", [128, 512], F32, psum=True)
        bk6 = T("bk6", [128, 512], F32, psum=True)
        bk7 = T("bk7", [128, 512], F32, psum=True)
        cb_dep = acs_dep = bk4.dep
        sts_dep = yoff_dep = bk6.dep

        dma("sp", pb[:], pb_d[:, 0:NRES], w=[pb])
        dma("sp", pc[:], pc_d, w=[pc])
        dma("sp", pk[:], pk_d, w=[pk])
        vcopy(identb[:], pk[:, IDENT:IDENT + 128], r=[pk], w=[identb])
        memset(cc[:, 0:1], EPS, w=[cc])
        memset(cc[:, 1:2], 1.0, w=[cc])
        memset(cc[:, 2:3], -1e29, w=[cc])
        memset(cc[:, 3:4], 4.0 * EPS, w=[cc])
        p2 = T("p2", [128, NIT + 1], F32)
        for k_ in range(NIT + 1):
            memset(p2[:, k_:k_ + 1], 2.0 ** -(k_ + 1), w=[p2])
        ts(pc[:], pc[:], 0.5, ALU.mult, r=[pc], w=[pc])
        act(Abc[:], pb[:, ALOG:ALOG + 32], AF.Exp, r=[pb], w=[Abc])
        ts(Abc[:], Abc[:], -1.0, ALU.mult, r=[Abc], w=[Abc])
        memset(KA[:], 0.0, w=[KA])
        dma("sp", KA.ap(0, [[1, SEQ]], p0=64, np_=5), kaug_d, w=[KA])
        memset(VA[:], 2.0, w=[VA])
        dma("pool", Wsm[:], win_v[:, :, C_SMALL:C_SMALL + 232], w=[Wsm])

        def load_w(src_v, c0, n, nkc=8):
            wb = wring.next()
            dma("pool", wb.ap(0, [[n, nkc], [1, n]]), src_v[:, :, c0:c0 + n], w=[wb])
            return wb

        def wslice(wb, n, kc, a, b):
            return wb.ap(kc * n + a, [[1, b - a]])

        for seq in range(n_seq):
            memset(St[:], 0.0, w=St_deps)
            memset(Sb[:], 0.0, w=Sb_deps)
            memset(halo[:], 0.0, w=halo_deps)
            for stl in range(4):
                t0 = stl * 512
                pre_w = [load_w(win_v, C_GRP, 768)]
                for tt_ in range(4):
                    xt = xring.next()
                    dma("sp", xt[:], x_d[seq, t0 + tt_ * 128:t0 + (tt_ + 1) * 128, :], w=[xt])
                    act(hb[:], xt[:], AF.Square, r=[xt], w=[hb, sm], accum=sm[:, 0:1])
                    rstd_col(sm[:, 2:3], sm[:, 0:1], 1.0 / D_MODEL, cc[:, 0:1], sm)
                    stt(hb[:], xt[:], sm[:, 2:3], pb[:, NW:NW + 1024], ALU.mult, ALU.mult, r=[xt, sm, pb], w=[hb])
                    for kc in range(8):
                        tr(ptr.ap(kc * 128, [[1, 128]]), hb[:, kc * 128:(kc + 1) * 128], r=[hb], w=[ptr])
                    acopy(hT.ap(tt_ * 128, [[512, 8], [1, 128]]), ptr.ap(0, [[128, 8], [1, 128]]), r=[ptr], w=[hT])
                if seq == 0 and stl == 0:
                    dump("hT", hT, hT[:], [128, 8, 512])

                for tt_ in range(4):
                    gt = stl * 4 + tt_
                    acc = accR.next()
                    for kc in range(8):
                        mm(acc[:, 0:232], hT.ap(kc * 512 + tt_ * 128, [[1, 128]]), Wsm.ap(kc * 232, [[1, 232]]),
                           start=(kc == 0), stop=(kc == 7), r=[hT, Wsm], w=[acc])
                    x32, e32, acs_sb, dd = s32
                    tt(x32[:], acc[:, 0:32], pb[:, DTB:DTB + 32], ALU.add, r=[acc, pb], w=[x32])
                    act(e32[:], x32[:], AF.Exp, r=[x32], w=[e32])
                    act(dt4.ap(tt_ * 32, [[1, 32]]), e32[:], AF.Ln, r=[e32, cc], w=[dt4], bias=cc[:, 1:2], scale=1.0)
                    tt(a4.ap(tt_ * 32, [[1, 32]]), dt4.ap(tt_ * 32, [[1, 32]]), Abc[:], ALU.mult, r=[dt4, Abc], w=[a4])
                    acopy(kvb[:, 0:64], acc[:, 32:96], r=[acc], w=[kvb])
                    acopy(VA.ap(gt * 66, [[1, 64]]), acc[:, 96:160], r=[acc], w=[VA])
                    S.op("dve", lambda e, acc=acc: e.bn_stats(out=sm[:, 4:10], in_=acc[:, 160:224]), [acc], [sm])
                    S.op("dve", lambda e: e.bn_aggr(out=sm[:, 10:12], in_=sm[:, 4:10]), [sm], [sm])
                    rstd_col(sm[:, 13:14], sm[:, 11:12], 1.0, cc[:, 0:1], sm)
                    ts(kif[:], acc[:, 160:224], sm[:, 10:11], ALU.subtract, r=[acc, sm], w=[kif], s2=sm[:, 13:14], op1=ALU.mult)
                    tt(kif[:], kif[:], pb[:, KIW:KIW + 64], ALU.mult, r=[kif, pb], w=[kif])
                    tt(kvb[:, 64:128], kif[:], pb[:, KIB:KIB + 64], ALU.add, r=[kif, pb], w=[kvb])
                    ts(wis.ap(tt_ * 8, [[1, 8]]), acc[:, 224:232], IDX_SCALE, ALU.mult, r=[acc], w=[wis])
                    tr(ptr.ap(0, [[1, 128]], np_=64), kvb[:, 0:64], r=[kvb], w=[ptr])
                    tr(ptr.ap(128, [[1, 128]], np_=64), kvb[:, 64:128], r=[kvb], w=[ptr])
                    acopy(KA.ap(gt * 128, [[1, 128]], np_=64), ptr.ap(0, [[1, 128]], np_=64), r=[ptr], w=[KA])
                    acopy(KIN.ap(gt * 128, [[1, 128]], np_=64), ptr.ap(128, [[1, 128]], np_=64), r=[ptr], w=[KIN])
                    mm(bk4[:, 128:160], pk[:, TRI:TRI + 128], a4.ap(tt_ * 32, [[1, 32]]), r=[pk, a4], w=[acs_dep])
                    mm(bk4[:, 160:192], pk[:, ONES:ONES + 128], a4.ap(tt_ * 32, [[1, 32]]), r=[pk, a4], w=[acs_dep])
                    acopy(acs_sb[:], bk4[:, 128:160], r=[acs_dep], w=[acs_sb])
                    act(eacs4.ap(tt_ * 32, [[1, 32]]), bk4[:, 128:160], AF.Exp, r=[acs_dep], w=[eacs4])
                    act(cdb4.ap(tt_ * 32, [[1, 32]]), bk4[:, 160:192], AF.Exp, r=[acs_dep], w=[cdb4])
                    tt(dd[:], bk4[:, 160:192], acs_sb[:], ALU.subtract, r=[acs_dep, acs_sb], w=[dd])
                    act(dd[:], dd[:], AF.Exp, r=[dd], w=[dd])
                    tt(dtd4.ap(tt_ * 32, [[1, 32]]), dt4.ap(tt_ * 32, [[1, 32]]), dd[:], ALU.mult, r=[dt4, dd], w=[dtd4])
                if seq == 0 and stl == 0:
                    dump("dt4", dt4, dt4[:], [128, 4, 32])
                    dump("KA", KA, KA[:], [128, SEQ])
                    dump("KIN", KIN, KIN[:], [128, SEQ])
                    dump("VA", VA, VA[:], [128, 16, 66])
                    dump("wis", wis, wis[:], [128, 4, 8])
                    dump("eacs4", eacs4, eacs4[:], [128, 4, 32])
                    dump("dtd4", dtd4, dtd4[:], [128, 4, 32])

                S.barrier(("pe", "act", "dve", "sp", "pool"))
                with ExitStack() as ph:
                    Upre = T("Upre", [128, 4, 515], F32, stack=ph)
                    Upre_d = [Dep("Upre%d" % i) for i in range(4)]
                    cv = T("cv", [128, 4, 512], F32, stack=ph)
                    cv_d = [Dep("cv%d" % i) for i in range(4)]
                    thR = Ring([T("th%d" % i, [128, 512], F32, stack=ph) for i in range(2)])
                    xbP = [T("xb%d" % i, [128, 4, 512], BF16, stack=ph) for i in range(2)]
                    xb_d = [[Dep("xb%d_%d" % (i, f)) for f in range(4)] for i in range(2)]
                    zsP = [T("zs%d" % i, [128, 4, 256], F32, stack=ph) for i in range(2)]
                    xtkP = [T("xtk%d" % i, [128, 4, 384], BF16, stack=ph) for i in range(2)]
                    rhsAR = Ring([T("rhsA%d" % i, [128, 512], F32, stack=ph) for i in range(2)])
                    CBmR = Ring([T("CBm%d" % i, [128, 128], F32, stack=ph) for i in range(2)])
                    EsegR = Ring([T("Eseg%d" % i, [128, 512], BF16, stack=ph) for i in range(2)])
                    MTR = Ring([T("MT%d" % i, [128, 512], BF16, stack=ph) for i in range(2)])
                    xcR = Ring([T("xc%d" % i, [128, 256], BF16, stack=ph) for i in range(2)])
                    xcdR = Ring([T("xcd%d" % i, [128, 256], BF16, stack=ph) for i in range(2)])
                    xsDR = Ring([T("xsD%d" % i, [128, 256], F32, stack=ph) for i in range(2)])
                    t1R = Ring([T("t1%d" % i, [128, 256], F32, stack=ph) for i in range(2)])
                    yzR = Ring([T("yz%d" % i, [128, 256], F32, stack=ph) for i in range(4)])
                    smgP = [T("smg%d" % i, [128, 12], F32, stack=ph) for i in range(2)]
                    yzs = {}
                    yjk = T("yjk", [128, 256], BF16, stack=ph)
                    yNR = Ring([T("yN%d" % i, [128, 256], BF16, stack=ph) for i in range(2)])
                    yNT = T("yNT", [128, 16, 512], BF16, stack=ph)
                    A0 = accR.tiles[0]
                    segR = Ring([bk3, bk7])
                    cbR = Ring([(256, bk5.dep)])
                    saR = Ring([accR.tiles[0], bk4])
                    ydR = Ring([(0, bk5.dep)])
                    soR = Ring([(bk6, bk6.dep, bk6.dep), (accR.tiles[1], accR.tiles[1].dep, accR.tiles[1].dep)])
                    ptrA_d, ptrB_d = ptr.dep, [ptr.dep, ptr.dep]
                    state_done = {}

                    def stageA(g):
                        par = g % 2
                        xb, zs, xtk, xbd = xbP[par], zsP[par], xtkP[par], xb_d[par]
                        wb = wq.pop(g)
                        vcopy(Upre.ap(0, [[515, 4], [1, 3]]), halo.ap(g * 12, [[3, 4], [1, 3]]), r=[halo_deps[g]], w=Upre_d)
                        for fi in range(4):
                            pa = saR.next()
                            for kc in range(8):
                                mm(pa[:, :], wslice(wb, 768, kc, 256 + fi * 128, 256 + (fi + 1) * 128), hT.ap(kc * 512, [[1, 512]]),
                                   start=(kc == 0), stop=(kc == 7), r=[wb, hT], w=[pa])
                            acopy(Upre.ap(fi * 515 + 3, [[1, 512]]), pa[:, :], r=[pa], w=[Upre_d[fi]])
                            yield
                        vcopy(halo.ap(g * 12, [[3, 4], [1, 3]]), Upre.ap(512, [[515, 4], [1, 3]]), r=Upre_d, w=[halo_deps[g]])
                        for fi in range(4):
                            ct = g * 4 + fi
                            cvf = cv.ap(fi * 512, [[1, 512]])
                            act(cvf, Upre.ap(fi * 515, [[1, 512]]), AF.Identity, r=[Upre_d[fi], pc], w=[cv_d[fi]],
                                bias=pc[:, 128 + ct:129 + ct], scale=pc[:, ct * 4:ct * 4 + 1])
                            yield
                            for k in range(1, 4):
                                stt(cvf, Upre.ap(fi * 515 + k, [[1, 512]]), pc[:, ct * 4 + k:ct * 4 + k + 1], cvf, ALU.mult, ALU.add,
                                    r=[Upre_d[fi], pc, cv_d[fi]], w=[cv_d[fi]])
                                yield
                            th = thR.next()
                            act(th[:], cvf, AF.Tanh, r=[cv_d[fi]], w=[th])
                            stt(xb.ap(fi * 512, [[1, 512]]), th[:], 1.0, cvf, ALU.add, ALU.mult, r=[th, cv_d[fi]], w=[xbd[fi]])
                            yield
                        for c in range(4):
                            pa = saR.next()
                            for kc in range(8):
                                mm(pa[:, 0:256], hT.ap(kc * 512 + c * 128, [[1, 128]]), wslice(wb, 768, kc, 0, 256),
                                   start=(kc == 0), stop=(kc == 7), r=[hT, wb], w=[pa])
                            th = thR.next()
                            act(th[:, 0:256], pa[:, 0:256], AF.Tanh, r=[pa], w=[th], scale=0.5)
                            stt(zs.ap(c * 256, [[1, 256]]), th[:, 0:256], 1.0, pa[:, 0:256], ALU.add, ALU.mult, r=[th, pa], w=[zs])
                            yield
                        for c in range(4):
                            for fi in range(3):
                                tr(ptr.ap(fi * 128, [[1, 128]]), xb.ap(fi * 512 + c * 128, [[1, 128]]), r=[xbd[fi]], w=[ptrA_d])
                            acopy(xtk.ap(c * 384, [[1, 384]]), ptr.ap(0, [[1, 384]]), r=[ptrA_d], w=[xtk])
                            yield

                    def chunkB(g, c):
                        par = g % 2
                        xb, zs, xtk, xbd = xbP[par], zsP[par], xtkP[par], xb_d[par]
                        hsl = c * 32 + g * 4
                        rhsA, CBm, Eseg, MT = rhsAR.next(), CBmR.next(), EsegR.next(), MTR.next()
                        xc, xcd, xsD, t1, yz = xcR.next(), xcdR.next(), xsDR.next(), t1R.next(), yzR.next()
                        smg = smgP[g % 2]
                        seg = segR.next()
                        cbo, cbd = cbR.next()
                        ydo, ydd = ydR.next()
                        sob, stsd, yofd = soR.next()
                        tt(rhsA.ap(0, [[128, 4], [1, 128]]), pk.ap(TRI, [[0, 4], [1, 128]]), a4.ap(hsl, [[1, 4], [0, 128]]),
                           ALU.mult, r=[pk, a4], w=[rhsA], eng="pool")
                        xs3 = xtk.ap(c * 384, [[64, 4], [1, 64]])
                        tt(xc.ap(0, [[64, 4], [1, 64]]), xs3, dt4.ap(hsl, [[1, 4], [0, 64]]), ALU.mult, r=[xtk, dt4], w=[xc], eng="pool")
                        tt(xcd.ap(0, [[64, 4], [1, 64]]), xs3, dtd4.ap(hsl, [[1, 4], [0, 64]]), ALU.mult, r=[xtk, dtd4], w=[xcd], eng="pool")
                        tt(xsD.ap(0, [[64, 4], [1, 64]]), xs3, pb.ap(DSK + g * 4, [[1, 4], [0, 64]]), ALU.mult, r=[xtk, pb], w=[xsD], eng="pool")
                        yield
                        mm(seg[:, :], pk[:, USTR:USTR + 128], rhsA[:, :], r=[pk, rhsA], w=[seg])
                        mm(bk5[:, cbo:cbo + 128], xb.ap(2 * 512 + c * 128, [[1, 128]]), xb.ap(3 * 512 + c * 128, [[1, 128]]),
                           r=[xbd[2], xbd[3]], w=[cbd])
                        tt(CBm[:], bk5[:, cbo:cbo + 128], pk[:, TRI:TRI + 128], ALU.mult, r=[cbd, pk], w=[CBm])
                        act(Eseg[:], seg[:, :], AF.Exp, r=[seg], w=[Eseg])
                        yield
                        tt(MT.ap(0, [[128, 4], [1, 128]]), Eseg.ap(0, [[128, 4], [1, 128]]), CBm.ap(0, [[0, 4], [1, 128]]),
                           ALU.mult, r=[Eseg, CBm], w=[MT])
                        yield
                        while c > 0 and not state_done.get((g, c - 1)):
                            yield
                        mm(sob[:, 256:512], xb.ap(3 * 512 + c * 128, [[1, 128]]), Sb.ap(g * 256, [[1, 256]]),
                           r=[xbd[3], Sb_deps[g]], w=[yofd])
                        for j in range(4):
                            mm(bk5[:, ydo + j * 64:ydo + (j + 1) * 64], MT[:, j * 128:(j + 1) * 128], xc[:, j * 64:(j + 1) * 64],
                               start=True, stop=True, r=[MT, xc], w=[ydd])
                        mm(sob[:, 0:256], xtk.ap(c * 384 + 256, [[1, 128]]), xcd[:], r=[xtk, xcd], w=[stsd])
                        tt(St.ap(g * 256, [[64, 4], [1, 64]]), St.ap(g * 256, [[64, 4], [1, 64]]),
                           cdb4.ap(hsl, [[1, 4], [0, 64]]), ALU.mult, r=[St_deps[g], cdb4], w=[St_deps[g]])
                        tt(St.ap(g * 256, [[1, 256]]), St.ap(g * 256, [[1, 256]]), sob[:, 0:256], ALU.add,
                           r=[St_deps[g], stsd], w=[St_deps[g]])
                        acopy(Sb.ap(g * 256, [[1, 256]]), St.ap(g * 256, [[1, 256]]), r=[St_deps[g]], w=[Sb_deps[g]])
                        state_done[(g, c)] = True
                        tt(t1.ap(0, [[64, 4], [1, 64]]), sob.ap(256, [[64, 4], [1, 64]]), eacs4.ap(hsl, [[1, 4], [0, 64]]),
                           ALU.mult, r=[yofd, eacs4], w=[t1])
                        tt(t1[:], bk5[:, ydo:ydo + 256], t1[:], ALU.add, r=[ydd, t1], w=[t1])
                        tt(t1[:], xsD[:], t1[:], ALU.add, r=[xsD, t1], w=[t1])
                        yield
                        tt(yz[:], t1[:], zs.ap(c * 256, [[1, 256]]), ALU.mult, r=[t1, zs], w=[yz])
                        act(yjk[:], yz[:], AF.Square, r=[yz], w=[yjk, smg], accum=smg[:, c:c + 1])
                        yzs[(g, c)] = yz
                        yield

                    def normB(g):
                        smg = smgP[g % 2]
                        act(smg[:, 4:8], smg[:, 0:4], AF.Sqrt, r=[smg, cc], w=[smg], bias=cc[:, 3:4], scale=1.0 / 256)
                        recip(smg[:, 8:12], smg[:, 4:8], r=[smg], w=[smg])
                        for c in range(4):
                            yz = yzs.pop((g, c))
                            yN = yNR.next()
                            stt(yN[:], yz[:], smg[:, 8 + c:9 + c], pb[:, SNW + g * 256:SNW + (g + 1) * 256], ALU.mult, ALU.mult,
                                r=[yz, smg, pb], w=[yN])
                            for i in range(2):
                                tr(ptr.ap(512 + (c % 2) * 256 + i * 128, [[1, 128]]), yN[:, i * 128:(i + 1) * 128], r=[yN], w=[ptrB_d[c % 2]])
                            acopy(yNT.ap((g * 2) * 512 + c * 128, [[512, 2], [1, 128]]), ptr.ap(512 + (c % 2) * 256, [[128, 2], [1, 128]]),
                                  r=[ptrB_d[c % 2]], w=[yNT])

                    def run_group(g):
                        if g + 2 < 8:
                            wq[g + 2] = load_w(win_v, C_GRP + (g + 2) * 768, 768)
                        pending = [chunkB(g, c) for c in range(4)]
                        active = [pending.pop(0), pending.pop(0)]
                        ag = stageA(g + 1) if g < 7 else None
                        while active or ag is not None:
                            for g_ in list(active):
                                try:
                                    next(g_)
                                except StopIteration:
                                    active.remove(g_)
                                    if pending:
                                        active.append(pending.pop(0))
                            for _rep in range(2):
                                if ag is not None:
                                    try:
                                        next(ag)
                                    except StopIteration:
                                        ag = None

                    wq = {0: pre_w.pop(), 1: load_w(win_v, C_GRP + 768, 768)}
                    for _ in stageA(0):
                        pass
                    for g in range(8):
                        run_group(g)
                        normB(g)
                    if seq == 0 and stl == 0:
                        dump("yNT", yNT, yNT[:], [128, 16, 512])
                    obanks = [accR.tiles[0], accR.tiles[1], bk3, bk7]
                    for half in range(2):
                        for kh in range(2):
                            wb = wring.next()
                            dma("pool", wb.ap(0, [[512, 8], [1, 512]]), wso_v[:, kh * 8:(kh + 1) * 8, half * 512:(half + 1) * 512], w=[wb])
                            for tt_ in range(4):
                                for kc in range(8):
                                    mm(obanks[tt_][:, :], yNT.ap((kh * 8 + kc) * 512 + tt_ * 128, [[1, 128]]), wslice(wb, 512, kc, 0, 512),
                                       start=(kh == 0 and kc == 0), stop=(kh == 1 and kc == 7), r=[yNT, wb], w=[obanks[tt_]])
                        for tt_ in range(4):
                            acopy(yssm.ap(tt_ * 1024 + half * 512, [[1, 512]]), obanks[tt_][:, :], r=[obanks[tt_]], w=[yssm])
                if seq == 0 and stl == 0:
                    dump("yssm", yssm, yssm[:], [128, 4, 1024])

                S.barrier()
                with ExitStack() as ph:
                    QA = T("QA", [128, 16, 512], BF16, stack=ph)
                    QI = T("QI", [128, 8, 512], BF16, stack=ph)
                    zA = T("zA", [128, 4, 1024], BF16, stack=ph)
                    isc = T("isc", [128, SEQ], F32, stack=ph)
                    rlR = Ring([T("rl%d" % i, [128, 512], F32, stack=ph) for i in range(2)])
                    maskb = T("maskb", [128, SEQ], BF16, stack=ph)
                    maskTP = [T("maskT%d" % i, [128, 16, 128], BF16, stack=ph) for i in range(2)]
                    ER = Ring([T("E%d" % i, [128, 512], BF16, stack=ph) for i in range(5)])
                    PR = Ring([T("P%d" % i, [128, 512], BF16, stack=ph) for i in range(5)])
                    og = T("og", [128, 1024], BF16, stack=ph)
                    Lm = T("Lm", [128, 512], F32, stack=ph)
                    OTs = T("OTs", [128, 512], F32, stack=ph)
                    thA = Lm
                    oT = T("oT", [128, 8, 512], BF16, stack=ph)
                    bs = T("bs", [128, 8], F32, stack=ph)
                    stp = T("stp", [128, NIT + 1], F32, stack=ph)
                    rc = T("rc", [128, 4], F32, stack=ph)
                    bu = T("bu", [128, 2], U32, stack=ph)
                    LR = Ring([bk3, accR.tiles[1], bk6, bk7])
                    A0 = accR.tiles[0]
                    A1 = accR.tiles[1]
                    Ob = [bk4, bk5, bk4, bk5]
                    Odeps = [Ob[j].dep for j in range(4)]
                    ptrA_d = ptrB_d = ptr.dep

                    dma("sp", QA.ap(0, [[512, 16], [1, 512]], p0=64, np_=5), qaug_d[:, :, t0:t0 + 512], w=[QA])
                    wb = load_w(win_v, C_QI, 512)
                    for h in range(8):
                        for kc in range(8):
                            mm(A0[0:64, :], wslice(wb, 512, kc, h * 64, (h + 1) * 64), hT.ap(kc * 512, [[1, 512]]),
                               start=(kc == 0), stop=(kc == 7), r=[wb, hT], w=[A0])
                        acopy(QI.ap(h * 512, [[1, 512]], np_=64), A0[0:64, :], r=[A0], w=[QI])

                    def stageI(tt_):
                        qb = stl * 4 + tt_
                        SL = (qb + 1) * 128
                        maskT = maskTP[tt_ % 2]
                        for c4 in range((SL + 511) // 512):
                            w_ = min(512, SL - c4 * 512)
                            for h in range(8):
                                mm(A0[:, 0:w_], QI.ap(h * 512 + tt_ * 128, [[1, 128]], np_=64), KIN.ap(c4 * 512, [[1, w_]], np_=64),
                                   r=[QI, KIN], w=[A0])
                                rl = rlR.next()
                                act(rl[:, 0:w_], A0[:, 0:w_], AF.Relu, r=[A0], w=[rl])
                                wcol = wis.ap(tt_ * 8 + h, [[1, 1]])
                                if h == 0:
                                    ts(isc[:, c4 * 512:c4 * 512 + w_], rl[:, 0:w_], wcol, ALU.mult, r=[rl, wis], w=[isc])
                                else:
                                    stt(isc[:, c4 * 512:c4 * 512 + w_], rl[:, 0:w_], wcol, isc[:, c4 * 512:c4 * 512 + w_],
                                        ALU.mult, ALU.add, r=[rl, wis, isc], w=[isc])
                                yield
                        if qb >= 2:
                            S.op("dve", lambda e, SL=SL, bs=bs, isc=isc: e.tensor_reduce(out=bs[:, 1:2], in_=isc[:, 0:SL], axis=AX.X, op=ALU.max), [isc], [bs])
                            S.op("dve", lambda e, SL=SL, bs=bs, isc=isc: e.tensor_reduce(out=bs[:, 0:1], in_=isc[:, 0:SL], axis=AX.X, op=ALU.min), [isc], [bs])
                            ts(bs[:, 5:6], bs[:, 1:2], 1.0, ALU.add, r=[bs], w=[bs], s2=bs[:, 0:1], op1=ALU.subtract)
                            ts(stp[:], p2[:], bs[:, 5:6], ALU.mult, r=[p2, bs], w=[stp])
                            ts(bs[:, 2:3], stp[:, 0:1], bs[:, 0:1], ALU.add, r=[stp, bs], w=[bs])
                        tt(isc[:, SL - 128:SL], isc[:, SL - 128:SL], pk[:, NEGM:NEGM + 128], ALU.add, r=[isc, pk], w=[isc])
                        yield
                        if qb >= 2:
                            for it in range(NIT):
                                ts(maskb[:, 0:SL], isc[:, 0:SL], bs[:, 2:3], ALU.is_ge, r=[isc, bs], w=[maskb, bs],
                                   s2=None, op1=ALU.add, accum=bs[:, 3:4])
                                yield
                                ts(bs[:, 4:5], bs[:, 3:4], TOPK - 0.5, ALU.is_ge, r=[bs, stp], w=[bs], s2=stp[:, it:it + 1], op1=ALU.mult)
                                stt(bs[:, 2:3], bs[:, 4:5], stp[:, it + 1:it + 2], bs[:, 2:3], ALU.subtract, ALU.add, r=[bs, stp], w=[bs])
                                yield
                            tt(bs[:, 6:7], bs[:, 2:3], stp[:, NIT:NIT + 1], ALU.subtract, r=[bs, stp], w=[bs])
                            thr = bs[:, 6:7]
                        else:
                            thr = cc[:, 2:3]
                        ts(maskb[:, 0:SL], isc[:, 0:SL], thr, ALU.is_ge, r=[isc, bs, cc], w=[maskb])
                        if seq == 0 and stl == 0 and tt_ == 3:
                            dump("isc", isc, isc[:, 0:512], [128, 512])
                            dump("bs", bs, bs[:], [128, 8])
                        for k0 in range(0, qb + 1, 4):
                            nk = min(4, qb + 1 - k0)
                            for kb in range(k0, k0 + nk):
                                tr(ptr.ap((kb - k0) * 128, [[1, 128]]), maskb[:, kb * 128:(kb + 1) * 128], r=[maskb], w=[ptrA_d])
                            acopy(maskT.ap(k0 * 128, [[1, nk * 128]]), ptr.ap(0, [[1, nk * 128]]), r=[ptrA_d], w=[maskT])
                            yield

                    def stageAT(tt_):
                        qb = stl * 4 + tt_
                        maskT = maskTP[tt_ % 2]
                        def kb_lo(hg):
                            smin = 2.0 ** (-8.0 * (hg * 4 + 4) / 16.0)
                            dskip = int(np.ceil((60.0 / smin + 127.0) / 128.0))
                            return max(0, qb - dskip + 1)
                        steps = [(hg, kb) for hg in range(4) for kb in range(kb_lo(hg), qb + 1)]
                        Ps = {}

                        def front(i):
                            hg, kb = steps[i]
                            L = LR.next()
                            mm(L[:, :], KA.ap(kb * 128, [[1, 128]], np_=69),
                               QA.ap(hg * 4 * 512 + tt_ * 128, [[512, 4], [1, 128]], np_=69), r=[KA, QA], w=[L])
                            E = ER.next()
                            if kb == qb:
                                tt(Lm.ap(0, [[128, 4], [1, 128]]), L.ap(0, [[128, 4], [1, 128]]),
                                   pk.ap(NEGT, [[0, 4], [1, 128]]), ALU.add, r=[L, pk], w=[Lm])
                                act(E[:], Lm[:], AF.Exp, r=[Lm], w=[E])
                            else:
                                act(E[:], L[:, :], AF.Exp, r=[L], w=[E])
                            P = PR.next()
                            tt(P.ap(0, [[128, 4], [1, 128]]), E.ap(0, [[128, 4], [1, 128]]),
                               maskT.ap(kb * 128, [[0, 4], [1, 128]]), ALU.mult, r=[E, maskT], w=[P],
                               eng=("pool" if i % 2 == 0 else "dve"))
                            Ps[i] = P

                        def back(i):
                            hg, kb = steps[i]
                            P = Ps.pop(i)
                            mm(Ob[hg][0:66, :], VA.ap(kb * 66, [[1, 66]]), P[:, :], start=(kb == kb_lo(hg)), stop=(kb == qb),
                               r=[VA, P], w=[Odeps[hg]])
                            if kb == qb:
                                acopy(OTs[0:66, :], Ob[hg][0:66, :], r=[Odeps[hg]], w=[OTs])
                                for j in range(4):
                                    S.op("pe", lambda e, j=j, hg=hg, OTs=OTs: e.transpose(out=Ob[hg][:, j * 66:(j + 1) * 66],
                                                                                         in_=OTs[0:66, j * 128:(j + 1) * 128],
                                                                                         identity=pk[0:66, IDENT:IDENT + 66]),
                                         [OTs, pk], [Odeps[hg]])
                                for j in range(4):
                                    h = hg * 4 + j
                                    recip(rc[:, j:j + 1], Ob[hg][:, j * 66 + 64:j * 66 + 65], r=[Odeps[hg]], w=[rc])
                                    stt(og[:, h * 64:(h + 1) * 64], Ob[hg][:, j * 66:j * 66 + 64], rc[:, j:j + 1],
                                        zA.ap(tt_ * 1024 + h * 64, [[1, 64]]), ALU.mult, ALU.mult, r=[Odeps[hg], rc, zA], w=[og])

                        LA = 3
                        for i in range(min(LA, len(steps))):
                            front(i)
                        for i in range(len(steps)):
                            if i + LA < len(steps):
                                front(i + LA)
                            back(i)
                            yield
                        for kc in range(8):
                            tr(ptr.ap(512 + (kc % 4) * 128, [[1, 128]]), og[:, kc * 128:(kc + 1) * 128], r=[og], w=[ptrB_d])
                            if kc % 4 == 3:
                                acopy(oT.ap((kc - 3) * 512 + tt_ * 128, [[512, 4], [1, 128]]), ptr.ap(512, [[128, 4], [1, 128]]),
                                      r=[ptrB_d], w=[oT])
                        yield

                    def stageProj():
                        for half in range(2):
                            wb = load_w(win_v, C_Q + half * 512, 512)
                            for hh in range(8):
                                h = half * 8 + hh
                                for kc in range(8):
                                    mm(A1[0:64, :], wslice(wb, 512, kc, hh * 64, (hh + 1) * 64), hT.ap(kc * 512, [[1, 512]]),
                                       start=(kc == 0), stop=(kc == 7), r=[wb, hT], w=[A1])
                                S.op("act", lambda e, h=h, QA=QA, A1=A1: e.mul(QA.ap(h * 512, [[1, 512]], np_=64), A1[0:64, :], 0.125), [A1], [QA])
                                yield
                        for half in range(2):
                            wb = load_w(win_v, C_AZ + half * 512, 512)
                            for tt_ in range(4):
                                for kc in range(8):
                                    mm(A1[:, :], hT.ap(kc * 512 + tt_ * 128, [[1, 128]]), wslice(wb, 512, kc, 0, 512),
                                       start=(kc == 0), stop=(kc == 7), r=[hT, wb], w=[A1])
                                act(thA[:], A1[:, :], AF.Tanh, r=[A1], w=[thA], scale=0.5)
                                stt(zA.ap(tt_ * 1024 + half * 512, [[1, 512]]), thA[:], 1.0, A1[:, :], ALU.add, ALU.mult,
                                    r=[thA, A1], w=[zA])
                                yield

                    interleave(stageI(0), stageProj())
                    if seq == 0 and stl == 0:
                        dump("QA", QA, QA[:], [128, 16, 512])
                        dump("QI", QI, QI[:], [128, 8, 512])
                    for tt_ in range(4):
                        interleave(stageAT(tt_), stageI(tt_ + 1) if tt_ < 3 else iter(()))
                    if seq == 0 and stl == 0:
                        dump("oT", oT, oT[:], [128, 8, 512])
                    for half in range(2):
                        wb = load_w(wao_v, half * 512, 512)
                        for tt_ in range(4):
                            acc = accR.next()
                            for kc in range(8):
                                mm(acc[:, :], oT.ap(kc * 512 + tt_ * 128, [[1, 128]]), wslice(wb, 512, kc, 0, 512),
                                   start=(kc == 0), stop=(kc == 7), r=[oT, wb], w=[acc])
                            acopy(yattn.ap(tt_ * 1024 + half * 512, [[1, 512]]), acc[:, :], r=[acc], w=[yattn])
                if seq == 0 and stl == 0:
                    dump("yattn", yattn, yattn[:], [128, 4, 1024])

                S.barrier()
                with ExitStack() as ph:
                    gtR = Ring([T("gt%d" % i, [128, 512], F32, stack=ph) for i in range(2)])
                    gsR = Ring([T("gs%d" % i, [128, 512], F32, stack=ph) for i in range(2)])
                    mg = T("mg", [128, 4, 1024], F32, stack=ph)
                    mb = T("mb", [128, 1024], BF16, stack=ph)
                    mT = T("mT", [128, 8, 512], BF16, stack=ph)
                    r4 = T("r4", [128, 4, 1024], F32, stack=ph)
                    roR = Ring([T("ro%d" % i, [128, 1024], F32, stack=ph) for i in range(2)])
                    p5 = T("p5", [128, NPB - NRES], F32, stack=ph)
                    dma("sp", p5[:], pb_d[:, NRES:NPB], w=[p5])
                    for u in range(4):
                        wb = load_w(win_v, C_GATE + u * 512, 512)
                        for tt_ in range(4):
                            acc = accR.next()
                            for kc in range(8):
                                mm(acc[:, :], hT.ap(kc * 512 + tt_ * 128, [[1, 128]]), wslice(wb, 512, kc, 0, 512),
                                   start=(kc == 0), stop=(kc == 7), r=[hT, wb], w=[acc])
                            gtt = gtR.next()
                            tt(gtt[:], acc[:, :], p5[:, u * 512:(u + 1) * 512], ALU.add, r=[acc, p5], w=[gtt])
                            gs = gsR.next()
                            act(gs[:], gtt[:], AF.Tanh, r=[gtt], w=[gs], scale=0.5)
                            if u < 2:
                                stt(mg.ap(tt_ * 1024 + u * 512, [[1, 512]]), gs[:], 1.0, yssm.ap(tt_ * 1024 + u * 512, [[1, 512]]),
                                    ALU.add, ALU.mult, r=[gs, yssm], w=[mg])
                            else:
                                stt(gs[:], gs[:], 1.0, yattn.ap(tt_ * 1024 + (u - 2) * 512, [[1, 512]]), ALU.add, ALU.mult,
                                    r=[gs, yattn], w=[gs])
                                tt(mg.ap(tt_ * 1024 + (u - 2) * 512, [[1, 512]]), mg.ap(tt_ * 1024 + (u - 2) * 512, [[1, 512]]),
                                   gs[:], ALU.add, r=[mg, gs], w=[mg])
                    for tt_ in range(4):
                        S.op("act", lambda e, tt_=tt_, mb=mb, mg=mg: e.mul(mb[:], mg.ap(tt_ * 1024, [[1, 1024]]), 0.5), [mg], [mb])
                        for kc in range(8):
                            tr(ptr.ap(kc * 128, [[1, 128]]), mb[:, kc * 128:(kc + 1) * 128], r=[mb], w=[ptr])
                        acopy(mT.ap(tt_ * 128, [[512, 8], [1, 128]]), ptr.ap(0, [[128, 8], [1, 128]]), r=[ptr], w=[mT])
                    for half in range(2):
                        wb = load_w(wout_v, half * 512, 512)
                        for tt_ in range(4):
                            acc = accR.next()
                            for kc in range(8):
                                mm(acc[:, :], mT.ap(kc * 512 + tt_ * 128, [[1, 128]]), wslice(wb, 512, kc, 0, 512),
                                   start=(kc == 0), stop=(kc == 7), r=[mT, wb], w=[acc])
                            acopy(r4.ap(tt_ * 1024 + half * 512, [[1, 512]]), acc[:, :], r=[acc], w=[r4])
                    for tt_ in range(4):
                        xt = xring.next()
                        dma("sp", xt[:], x_d[seq, t0 + tt_ * 128:t0 + (tt_ + 1) * 128, :], w=[xt])
                        r4s = r4.ap(tt_ * 1024, [[1, 1024]])
                        tt(r4s, r4s, xt[:], ALU.add, r=[r4, xt], w=[r4])
                        act(mb[:], r4s, AF.Square, r=[r4], w=[mb, sm], accum=sm[:, 0:1])
                        rstd_col(sm[:, 2:3], sm[:, 0:1], 1.0 / D_MODEL, cc[:, 0:1], sm)
                        ro = roR.next()
                        stt(ro[:], r4s, sm[:, 2:3], p5[:, FNW - NRES:FNW - NRES + 1024], ALU.mult, ALU.mult, r=[r4, sm, p5], w=[ro])
                        dma("sp", out_d[seq, t0 + tt_ * 128:t0 + (tt_ + 1) * 128, :], ro[:], r=[ro], w=[ro])
        S.final_wait("sp")
        S.emit()
    return nc, dbg_outs


def host_prep(inputs):
    f32 = np.float32
    w_in = np.asarray(inputs["w_in"], f32)[0]
    O_Z, O_XBC, O_DT, O_Q, O_K, O_V, O_AZ, O_QI, O_KI, O_WI, O_G = 0, 2048, 6144, 6176, 7200, 7264, 7328, 8352, 8864, 8928, 8936
    cols = []
    cols += list(range(O_DT, O_DT + 32)) + list(range(O_K, O_K + 64)) + list(range(O_V, O_V + 64))
    cols += list(range(O_KI, O_KI + 64)) + list(range(O_WI, O_WI + 8))
    for g in range(8):
        cols += list(range(O_Z + g * 256, O_Z + (g + 1) * 256))
        cols += list(range(O_XBC + g * 256, O_XBC + (g + 1) * 256))
        cols += list(range(O_XBC + 2048 + g * 128, O_XBC + 2048 + (g + 1) * 128))
        cols += list(range(O_XBC + 3072 + g * 128, O_XBC + 3072 + (g + 1) * 128))
    cols += list(range(O_Q, O_Q + 1024)) + list(range(O_QI, O_QI + 512)) + list(range(O_AZ, O_AZ + 1024))
    cols += list(range(O_G, O_G + 2048))
    cols = np.asarray(cols)
    assert cols.shape[0] == IN_TOTAL and np.unique(cols).shape[0] == IN_TOTAL
    win = np.ascontiguousarray(w_in[:, cols])

    pb = np.zeros((128, NPB), f32)

    def put(off, v):
        v = np.asarray(v, f32).reshape(-1)
        pb[:, off:off + v.shape[0]] = v[None, :]
    put(NW, inputs["norm_w"][0])
    put(FNW, inputs["final_norm_w"])
    put(GB, inputs["gate_b"][0])
    put(SNW, inputs["ssm_norm_w"][0])
    put(DTB, inputs["dt_bias"][0])
    put(ALOG, inputs["a_log"][0])
    put(DSK, inputs["d_skip"][0])
    put(KIW, inputs["idx_k_norm_w"][0])
    put(KIB, inputs["idx_k_norm_b"][0])

    conv_w = np.asarray(inputs["conv_w"], f32)[0]
    conv_b = np.asarray(inputs["conv_b"], f32)[0]
    pc = np.zeros((128, 160), f32)
    for g in range(8):
        for fi in range(4):
            ct = g * 4 + fi
            if fi < 2:
                ch0 = g * 256 + fi * 128
            elif fi == 2:
                ch0 = 2048 + g * 128
            else:
                ch0 = 3072 + g * 128
            pc[:, ct * 4:(ct + 1) * 4] = conv_w[:, ch0:ch0 + 128].T
            pc[:, 128 + ct] = conv_b[ch0:ch0 + 128]

    pk = np.zeros((128, NPK), f32)
    i = np.arange(128)
    pk[:, IDENT:IDENT + 128] = np.eye(128, dtype=f32)
    pk[:, TRI:TRI + 128] = (i[:, None] <= i[None, :]).astype(f32)
    pk[:, USTR:USTR + 128] = (i[:, None] > i[None, :]).astype(f32)
    pk[:, NEGM:NEGM + 128] = np.where(i[None, :] > i[:, None], f32(-1e30), f32(0))
    pk[:, ONES:ONES + 128] = 1.0
    pk[:, NEGT:NEGT + 128] = np.where(i[:, None] > i[None, :], f32(-1e30), f32(0))

    bf = ml_dtypes.bfloat16
    slopes = np.exp2(-8.0 * np.arange(1, 17, dtype=np.float64) / 16).astype(f32)
    s_hi = slopes.astype(bf)
    s_lo = (slopes - s_hi.astype(f32)).astype(bf)
    spos = np.arange(SEQ)
    kaug = np.zeros((5, SEQ), f32)
    kaug[0] = 1.0
    kaug[1] = spos % 128
    kaug[2] = (spos // 128) * 128
    kaug[3] = spos % 128
    kaug[4] = (spos // 128) * 128
    kaug = kaug.astype(bf)
    qaug = np.zeros((5, 16, SEQ), f32)
    qaug[0] = -(slopes[:, None].astype(np.float64) * spos[None, :]).astype(f32)
    qaug[1] = s_hi.astype(f32)[:, None]
    qaug[2] = s_hi.astype(f32)[:, None]
    qaug[3] = s_lo.astype(f32)[:, None]
    qaug[4] = s_lo.astype(f32)[:, None]
    qaug = qaug.astype(bf)
    shared = {
        "win": win,
        "wso": np.ascontiguousarray(np.asarray(inputs["w_ssm_out"], f32)[0]),
        "wao": np.ascontiguousarray(np.asarray(inputs["w_attn_out"], f32)[0]),
        "wout": np.ascontiguousarray(np.asarray(inputs["w_out"], f32)[0]),
        "pb": pb, "pc": pc, "pk": pk, "kaug": kaug, "qaug": qaug,
    }
    return shared


_CACHE = {}


def kernel(**inputs):
    x = np.asarray(inputs["x"], np.float32)
    shared = host_prep(inputs)
    if "nc" not in _CACHE:
        _CACHE["nc"] = build_program(2)[0]
    nc = _CACHE["nc"]
    in_maps = []
    for c in range(8):
        m = dict(shared)
        m["x"] = np.ascontiguousarray(x[2 * c:2 * c + 2])
        in_maps.append(m)
    res = run_bass_kernel_spmd(nc, in_maps, core_ids=list(range(8)))
    out = np.concatenate([np.asarray(r["out"], np.float32) for r in res.results], axis=0)
    return out
```
